# Optimizing a Trainium2 kernel written in Bass

```python
import math
import jax, jax.numpy as jnp
from jax import lax
import numpy as np

D_MODEL = 1024
BATCH = 16
SEQ = 2048
DEPTH = 2

HG_HEADS = 4
HG_DIM = 64
HG_WIDTH = HG_HEADS * HG_DIM
HG_CHUNK = 64

ATT_HEADS = 4
ATT_DIM = 64
ATT_WIDTH = ATT_HEADS * ATT_DIM
IDX_HEADS = 8
IDX_DIM = 64
TOPK_MAX = 256
Q_BLOCK = 128
ROPE_THETA = 10000.0

SSM_HEADS = 8
SSM_HEADDIM = 64
SSM_WIDTH = SSM_HEADS * SSM_HEADDIM
SSM_GROUPS = 2
SSM_STATE = 128
SSM_CONV = 4
SSM_CHUNK = 128
CONV_CH = SSM_WIDTH + 2 * SSM_GROUPS * SSM_STATE

MIX_WIDTH = HG_WIDTH + ATT_WIDTH + SSM_WIDTH
IN_SPLITS = (HG_WIDTH, HG_WIDTH, HG_WIDTH, HG_WIDTH,
             ATT_WIDTH, ATT_DIM, ATT_DIM, IDX_HEADS * IDX_DIM, IDX_DIM, IDX_HEADS,
             SSM_WIDTH, CONV_CH, SSM_HEADS)
IN_WIDTH = (4 * HG_WIDTH + ATT_WIDTH + 2 * ATT_DIM + IDX_HEADS * IDX_DIM + IDX_DIM + IDX_HEADS
            + SSM_WIDTH + CONV_CH + SSM_HEADS)

D_FF = 2816
EPS = 1e-6
NEG_BIG = -1e30

kernel_name = "hybrid_hgrn2_dsa_ssd_macaron_adaln"


def rms_norm(x, g):
    xf = x.astype(jnp.float32)
    y = xf * lax.rsqrt(jnp.mean(xf * xf, axis=-1, keepdims=True) + EPS)
    return (y * g.astype(jnp.float32)).astype(x.dtype)


def layer_norm(x, g, b):
    xf = x.astype(jnp.float32)
    xc = xf - jnp.mean(xf, axis=-1, keepdims=True)
    y = xc * lax.rsqrt(jnp.mean(xc * xc, axis=-1, keepdims=True) + EPS)
    return (y * g.astype(jnp.float32) + b.astype(jnp.float32)).astype(x.dtype)


def modulate(h, shift, scale):
    return h * (1 + scale[:, None, :]) + shift[:, None, :]


def swiglu(h, w_gu, w_down):
    gate, up = jnp.split(h @ w_gu, 2, axis=-1)
    return (jax.nn.silu(gate) * up) @ w_down


def rope_tables(seq, dim):
    inv_freq = 1.0 / (ROPE_THETA ** (jnp.arange(0, dim, 2, dtype=jnp.float32) / dim))
    ang = jnp.arange(seq, dtype=jnp.float32)[:, None] * inv_freq[None, :]
    return jnp.cos(ang), jnp.sin(ang)


def apply_rope(x, cos, sin):
    half = x.shape[-1] // 2
    bshape = (x.shape[1],) + (1,) * (x.ndim - 3) + (half,)
    cos = cos.reshape(bshape)
    sin = sin.reshape(bshape)
    xf = x.astype(jnp.float32)
    x1, x2 = xf[..., :half], xf[..., half:]
    return jnp.concatenate([x1 * cos - x2 * sin, x2 * cos + x1 * sin], axis=-1).astype(x.dtype)


def segsum_exp(cs):
    n = cs.shape[-1]
    mask = jnp.tril(jnp.ones((n, n), dtype=bool))
    return jnp.exp(jnp.where(mask, cs[..., :, None] - cs[..., None, :], NEG_BIG))


def hgrn2_mixer(q, f_logit, i, g, lb, norm_g):
    bsz, seq, _ = q.shape
    nc = seq // HG_CHUNK
    f32 = jnp.float32
    fl = f_logit.astype(f32)
    lb = lb.astype(f32)
    f = lb + (1 - lb) * jax.nn.sigmoid(fl)
    log_f = jnp.log(jnp.maximum(f, 1e-30))
    k = (1 - lb) * jax.nn.sigmoid(-fl)

    def chunks(t):
        return t.astype(f32).reshape(bsz, nc, HG_CHUNK, HG_HEADS, HG_DIM).transpose(1, 0, 3, 2, 4)

    causal = jnp.tril(jnp.ones((HG_CHUNK, HG_CHUNK), dtype=bool))[:, :, None]

    def step(state, inp):
        qc, kc, vc, lfc = inp
        b = jnp.cumsum(lfc, axis=2)
        o_inter = jnp.einsum('bhtk,bhkv->bhtv', qc * jnp.exp(b), state)
        diff = b[:, :, :, None, :] - b[:, :, None, :, :]
        decay = jnp.exp(jnp.where(causal, diff, NEG_BIG))
        scores = jnp.einsum('bhtk,bhtsk,bhsk->bhts', qc, decay, kc)
        o = o_inter + jnp.einsum('bhts,bhsv->bhtv', scores, vc)
        b_end = b[:, :, -1:, :]
        state = (state * jnp.exp(b_end[:, :, 0, :, None])
                 + jnp.einsum('bhsk,bhsv->bhkv', kc * jnp.exp(b_end - b), vc))
        return state, o

    s0 = jnp.zeros((bsz, HG_HEADS, HG_DIM, HG_DIM), f32)
    _, o = lax.scan(step, s0, (chunks(q), chunks(k), chunks(i), chunks(log_f)))
    o = o.transpose(1, 0, 3, 2, 4).reshape(bsz, seq, HG_HEADS, HG_DIM)
    o = rms_norm(o, norm_g.reshape(HG_HEADS, HG_DIM)).reshape(bsz, seq, HG_WIDTH)
    return (o * jax.nn.silu(g.astype(f32))).astype(q.dtype)


def dsa_mixer(q, k, v, q_idx, k_idx, w_idx, kn_g, kn_b):
    bsz, seq, _ = q.shape
    n_keep = min(TOPK_MAX, seq // 4)
    cos_a, sin_a = rope_tables(seq, ATT_DIM)
    cos_i, sin_i = rope_tables(seq, IDX_DIM)
    q = apply_rope(q.reshape(bsz, seq, ATT_HEADS, ATT_DIM), cos_a, sin_a)
    k = apply_rope(k, cos_a, sin_a)
    q_idx = apply_rope(q_idx.reshape(bsz, seq, IDX_HEADS, IDX_DIM), cos_i, sin_i)
    k_idx = apply_rope(layer_norm(k_idx, kn_g, kn_b), cos_i, sin_i)
    w_idx = w_idx * (IDX_HEADS ** -0.5 * IDX_DIM ** -0.5)
    key_pos = jnp.arange(seq)

    def block(j):
        t0 = j * Q_BLOCK
        qb = lax.dynamic_slice_in_dim(q, t0, Q_BLOCK, axis=1)
        qib = lax.dynamic_slice_in_dim(q_idx, t0, Q_BLOCK, axis=1)
        wib = lax.dynamic_slice_in_dim(w_idx, t0, Q_BLOCK, axis=1)
        q_pos = t0 + jnp.arange(Q_BLOCK)
        logits = jax.nn.relu(jnp.einsum('bthd,bsd->bths', qib, k_idx))
        score = jnp.einsum('bths,bth->bts', logits, wib).astype(jnp.float32)
        score = jnp.where((key_pos[None, :] <= q_pos[:, None])[None], score, NEG_BIG)
        _, sel = lax.top_k(score, n_keep)
        k_sel = jax.vmap(lambda kk, ii: kk[ii])(k, sel)
        v_sel = jax.vmap(lambda vv, ii: vv[ii])(v, sel)
        s = jnp.einsum('bthd,btkd->bhtk', qb, k_sel).astype(jnp.float32) * ATT_DIM ** -0.5
        s = jnp.where((sel <= q_pos[None, :, None])[:, None], s, NEG_BIG)
        p = jax.nn.softmax(s, axis=-1).astype(v.dtype)
        return jnp.einsum('bhtk,btkd->bthd', p, v_sel)

    out = lax.map(block, jnp.arange(seq // Q_BLOCK))
    return out.transpose(1, 0, 2, 3, 4).reshape(bsz, seq, ATT_WIDTH)


def mamba2_mixer(z, xbc, dt_raw, conv_w, conv_b, dt_bias, a_log, d_skip, norm_g):
    bsz, seq, _ = z.shape
    nc = seq // SSM_CHUNK
    R = SSM_HEADS // SSM_GROUPS
    f32 = jnp.float32
    xbc = lax.conv_general_dilated(xbc, conv_w[:, None, :].astype(xbc.dtype), (1,), [(SSM_CONV - 1, 0)],
                                   dimension_numbers=('NWC', 'WIO', 'NWC'), feature_group_count=CONV_CH)
    xbc = jax.nn.silu(xbc.astype(f32) + conv_b.astype(f32))
    xs, bm, cm = jnp.split(xbc, [SSM_WIDTH, SSM_WIDTH + SSM_GROUPS * SSM_STATE], axis=-1)
    dt = jax.nn.softplus(dt_raw.astype(f32) + dt_bias.astype(f32))
    a = -jnp.exp(a_log.astype(f32))
    x6 = xs.reshape(bsz, nc, SSM_CHUNK, SSM_GROUPS, R, SSM_HEADDIM)
    dt5 = dt.reshape(bsz, nc, SSM_CHUNK, SSM_GROUPS, R)
    bm = bm.reshape(bsz, nc, SSM_CHUNK, SSM_GROUPS, SSM_STATE)
    cm = cm.reshape(bsz, nc, SSM_CHUNK, SSM_GROUPS, SSM_STATE)
    xdt = x6 * dt5[..., None]
    a_cs = jnp.cumsum((dt5 * a.reshape(SSM_GROUPS, R)).transpose(0, 3, 4, 1, 2), axis=-1)
    decay_in = segsum_exp(a_cs)
    cb = jnp.einsum('bclgn,bcsgn->bgcls', cm, bm)
    y_diag = jnp.einsum('bgrcls,bcsgrp->bclgrp', cb[:, :, None] * decay_in, xdt)
    states = jnp.einsum('bclgn,bgrcl,bclgrp->bcgrpn', bm, jnp.exp(a_cs[..., -1:] - a_cs), xdt)
    chunk_cs = jnp.cumsum(jnp.pad(a_cs[..., -1], ((0, 0), (0, 0), (0, 0), (1, 0))), axis=-1)
    decay_chunk = segsum_exp(chunk_cs)
    states = jnp.concatenate([jnp.zeros_like(states[:, :1]), states], axis=1)
    prev = jnp.einsum('bgrzc,bcgrpn->bzgrpn', decay_chunk, states)[:, :-1]
    y_off = jnp.einsum('bclgn,bcgrpn,bgrcl->bclgrp', cm, prev, jnp.exp(a_cs))
    y = (y_diag + y_off).reshape(bsz, seq, SSM_WIDTH) + xs * jnp.repeat(d_skip.astype(f32), SSM_HEADDIM)
    y = y * jax.nn.silu(z.astype(f32))
    y = rms_norm(y.reshape(bsz, seq, SSM_GROUPS, SSM_WIDTH // SSM_GROUPS),
                 norm_g.reshape(SSM_GROUPS, SSM_WIDTH // SSM_GROUPS))
    return y.reshape(bsz, seq, SSM_WIDTH).astype(z.dtype)


def token_mixing(h, w_in, w_out, lb, hg_norm_g, kn_g, kn_b, conv_w, conv_b, dt_bias, a_log, d_skip, ssm_norm_g):
    offsets = [int(o) for o in np.cumsum(IN_SPLITS)[:-1]]
    (hq, hf, hi, hg, aq, ak, av, iq, ik, iw, sz, sxbc, sdt) = jnp.split(h @ w_in, offsets, axis=-1)
    o_a = hgrn2_mixer(hq, hf, hi, hg, lb, hg_norm_g)
    o_b = dsa_mixer(aq, ak, av, iq, ik, iw, kn_g, kn_b).astype(h.dtype)
    o_c = mamba2_mixer(sz, sxbc, sdt, conv_w, conv_b, dt_bias, a_log, d_skip, ssm_norm_g)
    return jnp.concatenate([o_a, o_b, o_c], axis=-1) @ w_out


def setup_inputs(seed: int = 0) -> dict:
    key = jax.random.key(seed)
    ks = jax.random.split(key, 24)
    f32 = jnp.float32

    def nrm(k, shape, s):
        return jax.random.normal(k, shape, f32) * s

    x = nrm(ks[0], (BATCH, SEQ, D_MODEL), 1.0)
    c = nrm(ks[1], (BATCH, D_MODEL), 1.0)
    w_ada = nrm(ks[2], (DEPTH, D_MODEL, 9 * D_MODEL), 0.5 * D_MODEL ** -0.5)
    b_ada = nrm(ks[3], (DEPTH, 9 * D_MODEL), 0.02)
    norm_g = 1.0 + nrm(ks[4], (DEPTH, 3, D_MODEL), 0.05)
    w_ffn_gu = nrm(ks[5], (DEPTH, 2, D_MODEL, 2 * D_FF), D_MODEL ** -0.5)
    w_ffn_down = nrm(ks[6], (DEPTH, 2, D_FF, D_MODEL), D_FF ** -0.5)
    w_in = nrm(ks[7], (DEPTH, D_MODEL, IN_WIDTH), D_MODEL ** -0.5)
    w_out = nrm(ks[8], (DEPTH, MIX_WIDTH, D_MODEL), MIX_WIDTH ** -0.5)
    lb_logits = nrm(ks[9], (DEPTH, HG_WIDTH), 1.0)
    hg_norm_g = 1.0 + nrm(ks[10], (DEPTH, HG_WIDTH), 0.05)
    idx_k_norm_g = 1.0 + nrm(ks[11], (DEPTH, IDX_DIM), 0.05)
    idx_k_norm_b = nrm(ks[12], (DEPTH, IDX_DIM), 0.01)
    conv_w = nrm(ks[13], (DEPTH, SSM_CONV, CONV_CH), SSM_CONV ** -0.5)
    conv_b = nrm(ks[14], (DEPTH, CONV_CH), 0.01)
    dt0 = jnp.exp(jax.random.uniform(ks[15], (DEPTH, SSM_HEADS), f32, math.log(1e-3), math.log(1e-1)))
    dt_bias = dt0 + jnp.log(-jnp.expm1(-dt0))
    a_log = jnp.log(jax.random.uniform(ks[16], (DEPTH, SSM_HEADS), f32, 1.0, 16.0))
    d_skip = 1.0 + nrm(ks[17], (DEPTH, SSM_HEADS), 0.1)
    ssm_norm_g = 1.0 + nrm(ks[18], (DEPTH, SSM_WIDTH), 0.05)
    final_norm_g = 1.0 + nrm(ks[19], (D_MODEL,), 0.05)
    return {"x": x, "c": c, "w_ada": w_ada, "b_ada": b_ada, "norm_g": norm_g,
            "w_ffn_gu": w_ffn_gu, "w_ffn_down": w_ffn_down, "w_in": w_in, "w_out": w_out,
            "lb_logits": lb_logits, "hg_norm_g": hg_norm_g, "idx_k_norm_g": idx_k_norm_g,
            "idx_k_norm_b": idx_k_norm_b, "conv_w": conv_w, "conv_b": conv_b, "dt_bias": dt_bias,
            "a_log": a_log, "d_skip": d_skip, "ssm_norm_g": ssm_norm_g, "final_norm_g": final_norm_g}


def reference(x, c, w_ada, b_ada, norm_g, w_ffn_gu, w_ffn_down, w_in, w_out, lb_logits, hg_norm_g,
              idx_k_norm_g, idx_k_norm_b, conv_w, conv_b, dt_bias, a_log, d_skip, ssm_norm_g, final_norm_g):
    sm = jax.nn.softmax(lb_logits.astype(jnp.float32), axis=0)
    lower_bounds = jnp.cumsum(sm, axis=0) - sm[:1]
    cond = jax.nn.silu(c)
    for l in range(DEPTH):
        mod = cond @ w_ada[l] + b_ada[l]
        sh1, sc1, g1, sh2, sc2, g2, sh3, sc3, g3 = jnp.split(mod, 9, axis=-1)
        h = modulate(rms_norm(x, norm_g[l, 0]), sh1, sc1)
        x = x + 0.5 * g1[:, None, :] * swiglu(h, w_ffn_gu[l, 0], w_ffn_down[l, 0])
        h = modulate(rms_norm(x, norm_g[l, 1]), sh2, sc2)
        x = x + g2[:, None, :] * token_mixing(h, w_in[l], w_out[l], lower_bounds[l], hg_norm_g[l],
                                              idx_k_norm_g[l], idx_k_norm_b[l], conv_w[l], conv_b[l],
                                              dt_bias[l], a_log[l], d_skip[l], ssm_norm_g[l])
        h = modulate(rms_norm(x, norm_g[l, 2]), sh3, sc3)
        x = x + 0.5 * g3[:, None, :] * swiglu(h, w_ffn_gu[l, 1], w_ffn_down[l, 1])
    return rms_norm(x, final_norm_g)
```

```python
import numpy as np
from contextlib import ExitStack
import concourse.bass as bass
import concourse.mybir as mybir
from concourse.bass_utils import run_bass_kernel_spmd

F32 = mybir.dt.float32
BF16 = mybir.dt.bfloat16
AF = mybir.ActivationFunctionType
ALU = mybir.AluOpType
AX = mybir.AxisListType

D = 1024
S = 2048
DEPTH = 2
DFF = 2816
NSEQ = 2
NT = S // 128
EPS = 1e-6
NEG = -1e30


class Buf:
    __slots__ = ("w", "r", "name")

    def __init__(self, name="", w=None):
        self.w = dict(w) if w else {}
        self.r = {}
        self.name = name


class V:
    __slots__ = ("ap", "buf")

    def __init__(self, ap, buf):
        self.ap = ap
        self.buf = buf

    def __getitem__(self, k):
        return V(self.ap[k], self.buf)


class Prog:
    def __init__(self, nc, es):
        self.nc = nc
        self.es = es
        self.E = dict(pe=nc.tensor, act=nc.scalar, dve=nc.vector, pool=nc.gpsimd, sp=nc.sync)
        self.sem = {}
        self.cnt = {}
        for e in ("pe", "act", "dve", "pool"):
            self.sem[e] = es.enter_context(nc.semaphore("s_" + e))
            self.cnt[e] = 0
        self.waited = {e: {} for e in self.E}
        self.ninstr = 0

    def dma_sem(self, name):
        key = "d_" + name
        self.sem[key] = self.es.enter_context(self.nc.semaphore("s_" + key))
        self.cnt[key] = 0
        return key

    def snapshot(self):
        return {k: v for k, v in self.cnt.items() if v > 0}

    def buf(self, name="", fresh=True):
        return Buf(name, self.snapshot() if fresh else None)

    def _emit_waits(self, eng, reads, writes, skipkey=None):
        need = {}
        for b in reads:
            for k, v in b.w.items():
                if v > need.get(k, 0):
                    need[k] = v
        for b in writes:
            for k, v in b.w.items():
                if k == skipkey:
                    continue
                if v > need.get(k, 0):
                    need[k] = v
            for k, v in b.r.items():
                if v > need.get(k, 0):
                    need[k] = v
        e = self.E[eng]
        wd = self.waited[eng]
        for k, v in need.items():
            if k == "pe" and eng == "pe":
                continue
            if wd.get(k, 0) >= v:
                continue
            wd[k] = v
            e.wait_ge(self.sem[k], v)
            self.ninstr += 1

    def op(self, eng, fn, reads=(), writes=(), inc=True):
        reads = [x.buf if isinstance(x, V) else x for x in reads]
        writes = [x.buf if isinstance(x, V) else x for x in writes]
        self._emit_waits(eng, reads, writes)
        ins = fn(self.E[eng])
        self.ninstr += 1
        if inc:
            self.cnt[eng] += 1
            ins.then_inc(self.sem[eng], 1)
            t = self.cnt[eng]
        else:
            t = self.cnt[eng] + 1
        for b in reads:
            if t > b.r.get(eng, 0):
                b.r[eng] = t
        for b in writes:
            b.w = {eng: t}
            b.r = {}
        return ins

    def dma(self, q, out, in_, semkey, reads=(), writes=(), **kw):
        reads = [x.buf if isinstance(x, V) else x for x in reads]
        writes = [x.buf if isinstance(x, V) else x for x in writes]
        self._emit_waits(q, reads, writes, skipkey=semkey)
        ins = self.E[q].dma_start(out=out, in_=in_, **kw)
        self.ninstr += 1
        self.cnt[semkey] += 16
        ins.then_inc(self.sem[semkey], 16)
        t = self.cnt[semkey]
        for b in reads:
            if t > b.r.get(semkey, 0):
                b.r[semkey] = t
        for b in writes:
            if semkey in b.w and len(b.w) == 1:
                b.w[semkey] = t
            else:
                b.w = {semkey: t}
            b.r = {}
        return ins

    def wait_all(self, eng):
        e = self.E[eng]
        for k, v in self.snapshot().items():
            if self.waited[eng].get(k, 0) >= v:
                continue
            self.waited[eng][k] = v
            e.wait_ge(self.sem[k], v)


O_HQ, O_HF, O_HI, O_HG, O_AQ, O_AK, O_AV, O_IQ, O_IK, O_IW, O_SZ, O_SX, O_SDT = (
    0, 256, 512, 768, 1024, 1280, 1344, 1408, 1920, 1984, 1992, 2504, 3528)
IWSCALE = float(8 ** -0.5 * 64 ** -0.5)
NIT = 16


def _win2_layout():
    off = {}
    ncol = {}
    cols = []

    def add(name, idx):
        idx = list(idx)
        off[name] = len(cols)
        ncol[name] = len(idx)
        cols.extend(idx)

    def sw(base, n):
        out = []
        for h in range(n // 64):
            b = base + h * 64
            out += list(range(b + 32, b + 64)) + list(range(b, b + 32))
        return out

    for s in range(2):
        add(f"HQ{s}", range(O_HQ + s * 128, O_HQ + (s + 1) * 128))
        add(f"HF{s}", range(O_HF + s * 128, O_HF + (s + 1) * 128))
        add(f"HG{s}", range(O_HG + s * 128, O_HG + (s + 1) * 128))
        add(f"HI{s}", range(O_HI + s * 128, O_HI + (s + 1) * 128))
    for s in range(2):
        add(f"AQ{s}", range(O_AQ + s * 128, O_AQ + (s + 1) * 128))
        add(f"AQW{s}", sw(O_AQ + s * 128, 128))
    add("AK2", list(range(O_AK, O_AK + 64)) * 2)
    add("AKW2", sw(O_AK, 64) * 2)
    for s in range(4):
        add(f"IQ{s}", range(O_IQ + s * 128, O_IQ + (s + 1) * 128))
        add(f"IQW{s}", sw(O_IQ + s * 128, 128))
    add("IK2", list(range(O_IK, O_IK + 64)) * 2)
    add("IKW2", sw(O_IK, 64) * 2)
    add("AVIW", list(range(O_AV, O_AV + 64)) + list(range(O_IW, O_IW + 8)))
    for g in range(2):
        add(f"Z{g}0", range(O_SZ + (2 * g) * 128, O_SZ + (2 * g + 1) * 128))
        add(f"Z{g}1", range(O_SZ + (2 * g + 1) * 128, O_SZ + (2 * g + 2) * 128))
        add(f"XS{g}0", range(O_SX + (2 * g) * 128, O_SX + (2 * g + 1) * 128))
        add(f"XS{g}1", range(O_SX + (2 * g + 1) * 128, O_SX + (2 * g + 2) * 128))
        add(f"B{g}", range(O_SX + 512 + g * 128, O_SX + 512 + (g + 1) * 128))
        add(f"C{g}", range(O_SX + 768 + g * 128, O_SX + 768 + (g + 1) * 128))
    add("SDT", range(O_SDT, O_SDT + 8))
    return np.array(cols, dtype=np.int64), off, ncol


W2COLS, W2OFF, W2N = _win2_layout()
NC2 = len(W2COLS)


def _consts():
    c = {}
    p = np.arange(128)
    c["ident"] = np.eye(128, dtype=np.float32)
    c["ones_d"] = np.full((128, 128), 1.0 / 1024.0, np.float32)
    c["ones_256"] = np.full((128, 128), 1.0 / 256.0, np.float32)
    c["ones_f"] = np.ones((128, 128), np.float32)
    o64 = np.zeros((128, 128), np.float32)
    o64[:64, :64] = 1.0 / 64.0
    o64[64:, 64:] = 1.0 / 64.0
    c["ones_64b"] = o64
    s_, t_ = p[:, None], p[None, :]
    c["bdmask"] = ((s_ // 64 == t_ // 64) & (t_ >= s_)).astype(np.float32)
    c["causT"] = (t_ >= s_).astype(np.float32)
    same = (s_ // 64 == t_ // 64)
    c["maskD2"] = (same & (t_ >= s_) & (s_ % 64 >= 32) & (t_ % 64 >= 32)).astype(np.float32)
    c["maskX"] = (same & (s_ % 64 < 32) & (t_ % 64 >= 32)).astype(np.float32)
    c["causbias"] = np.where(t_ <= s_, 0.0, NEG).astype(np.float32)
    c["Lgt"] = (s_ > t_).astype(np.float32)
    c["Uincl"] = (s_ <= t_).astype(np.float32)
    tt = np.arange(S)
    c["rmask"] = np.broadcast_to((tt % 64 != 0).astype(np.float32)[None, :], (128, S)).copy()
    inv_freq = 1.0 / (10000.0 ** (np.arange(0, 64, 2, dtype=np.float32) / 64.0))
    ang = tt.astype(np.float32)[:, None] * inv_freq[None, :]
    cos = np.cos(ang).astype(np.float32).T
    sin = np.sin(ang).astype(np.float32).T
    c["cosT"] = np.concatenate([cos, cos, cos, cos], axis=0)
    c["sinT"] = np.concatenate([-sin, sin, -sin, sin], axis=0)
    c["pow2"] = np.broadcast_to((2.0 ** -(np.arange(NIT + 1, dtype=np.float32) + 1.0))[None, :], (128, NIT + 1)).copy()
    return c


CONST_SHAPES = dict(ident=[128, 128], ones_d=[128, 128], ones_256=[128, 128], ones_f=[128, 128], ones_64b=[128, 128],
                    bdmask=[128, 128], maskD2=[128, 128], maskX=[128, 128], causT=[128, 128], causbias=[128, 128], Lgt=[128, 128], Uincl=[128, 128],
                    rmask=[128, S], cosT=[128, S], sinT=[128, S], pow2=[128, NIT + 1])

FULL_CFG = dict(nseq=2, layers=(0, 1), ffn1=True, mix=("hg", "dsa", "ssd"), ffn2=True, hostmod=False)


def _fm(v, nchunk):
    v = np.asarray(v, np.float32)
    lead = v.shape[:-1]
    r = v.reshape(lead + (nchunk, 128))
    r = np.moveaxis(r, -1, 0)
    return np.ascontiguousarray(r)


def _shared_inputs(inputs, cfg=FULL_CFG):
    m = {}
    if not cfg["hostmod"]:
        m["w_ada"] = np.ascontiguousarray(inputs["w_ada"])
        m["b_adaT"] = _fm(inputs["b_ada"], 72)
    m["norm_gT"] = _fm(inputs["norm_g"].reshape(DEPTH, 3 * D), 24)
    m["fnorm_gT"] = _fm(inputs["final_norm_g"], 8)
    if cfg["ffn1"] or cfg["ffn2"]:
        m["w_ffn_gu"] = np.ascontiguousarray(inputs["w_ffn_gu"])
        m["w_ffn_down"] = np.ascontiguousarray(inputs["w_ffn_down"])
    if cfg["mix"]:
        m["win2"] = np.ascontiguousarray(inputs["w_in"][:, :, W2COLS])
        m["w_out"] = np.ascontiguousarray(inputs["w_out"])
        m["lblT"] = _fm(inputs["lb_logits"], 2)
        m["hgngT"] = _fm(inputs["hg_norm_g"], 2)
        g64 = inputs["idx_k_norm_g"]
        b64 = inputs["idx_k_norm_b"]
        swp = np.concatenate([np.arange(32, 64), np.arange(0, 32)])
        kn = np.stack([np.tile(g64, (1, 2)), np.tile(b64, (1, 2)), np.tile(g64[:, swp], (1, 2)), np.tile(b64[:, swp], (1, 2))], axis=1)
        m["knT"] = np.ascontiguousarray(kn.transpose(2, 0, 1))
        m["convwT"] = np.ascontiguousarray(inputs["conv_w"].reshape(DEPTH, 4, 8, 128).transpose(3, 0, 2, 1))
        m["convbT"] = _fm(inputs["conv_b"], 8)
        m["ssmgT"] = _fm(inputs["ssm_norm_g"], 4)
        dsk = np.repeat(inputs["d_skip"], 64, axis=1)
        m["dskT"] = _fm(dsk, 4)
        m["dtbB"] = np.ascontiguousarray(np.broadcast_to(inputs["dt_bias"][None], (128, DEPTH, 8)))
        m["alogB"] = np.ascontiguousarray(np.broadcast_to(inputs["a_log"][None], (128, DEPTH, 8)))
    cc = _consts()
    for k in CONST_SHAPES:
        m[k] = cc[k]
    return m


def _host_inputs(inputs, core, cfg=FULL_CFG):
    ns = cfg["nseq"]
    b0 = core * NSEQ
    m = {"x": np.ascontiguousarray(inputs["x"][b0:b0 + ns])}
    if not cfg["hostmod"]:
        c = inputs["c"][b0:b0 + ns]
        m["cT"] = np.ascontiguousarray(c.reshape(ns, 8, 128).transpose(2, 1, 0))
    return m


def build_program(cfg=None):
    cfg = dict(FULL_CFG if cfg is None else cfg)
    NSQ = cfg["nseq"]
    LAYERS = list(cfg["layers"])
    MIX = tuple(cfg["mix"])
    nc = bass.Bass("TRN2", target_bir_lowering=False)

    def din(name, shape, dtype=F32):
        return nc.dram_tensor(name, list(shape), dtype, kind="ExternalInput").ap()

    x_d = din("x", [NSQ, S, D])
    if cfg["hostmod"]:
        modin_d = din("modT_in", [128, DEPTH, 72, NSQ])
    else:
        cT_d = din("cT", [128, 8, NSQ])
        wada_d = din("w_ada", [DEPTH, D, 9 * D])
        badaT_d = din("b_adaT", [128, DEPTH, 72])
    normgT_d = din("norm_gT", [128, DEPTH, 24])
    fngT_d = din("fnorm_gT", [128, 8])
    if cfg["ffn1"] or cfg["ffn2"]:
        wgu_d = din("w_ffn_gu", [DEPTH, 2, D, 2 * DFF])
        wdn_d = din("w_ffn_down", [DEPTH, 2, DFF, D])
    if MIX:
        win2_d = din("win2", [DEPTH, D, NC2])
        wout_d = din("w_out", [DEPTH, D, D])
        lblT_d = din("lblT", [128, DEPTH, 2])
        hgngT_d = din("hgngT", [128, DEPTH, 2])
        knT_d = din("knT", [128, DEPTH, 4])
        convwT_d = din("convwT", [128, DEPTH, 8, 4])
        convbT_d = din("convbT", [128, DEPTH, 8])
        ssmgT_d = din("ssmgT", [128, DEPTH, 4])
        dskT_d = din("dskT", [128, DEPTH, 4])
        dtbB_d = din("dtbB", [128, DEPTH, 8])
        alogB_d = din("alogB", [128, DEPTH, 8])
    cd = {k: din(k, shp) for k, shp in CONST_SHAPES.items()}
    out_d = nc.dram_tensor("out", [NSQ, S, D], F32, kind="ExternalOutput").ap()

    es = ExitStack()
    with es:
        P = Prog(nc, es)
        uid = [0]

        def sb(name, shape, dtype, st=es):
            uid[0] += 1
            return st.enter_context(nc.sbuf_tensor(f"sb{uid[0]}_{name}", list(shape), dtype))

        xT_t = sb("xT", [128, 8, S], F32)
        hT_t = sb("hT", [128, 8, S], BF16)
        RING = 3
        ring_t = [sb(f"ring{i}", [128, 6144], BF16) for i in range(RING)]
        ring_b = [Buf(f"ring{i}") for i in range(RING)]
        ring_sem = [P.dma_sem(f"ring{i}") for i in range(RING)]
        modT_t = sb("modT", [128, DEPTH, 72, NSQ], F32)
        AG_t = sb("AG", [128, DEPTH, NSQ, 6, 8], F32)
        normg_t = sb("normg", [128, DEPTH, 24], F32)
        fng_t = sb("fng", [128, 8], F32)
        eps_t = sb("eps", [128, 1], F32)
        one_t = sb("one", [128, 1], F32)
        ident_t = sb("ident", [128, 128], F32)
        identb_t = sb("identb", [128, 128], BF16)
        onesd_t = sb("onesd", [128, 128], BF16)

        cst = Buf("consts")
        modb = Buf("mod")
        csem = P.dma_sem("const")
        csemp = P.dma_sem("constp")
        misc_sem = P.dma_sem("miscp")
        P.dma("sp", ident_t[:], cd["ident"], csem, writes=[cst])
        P.dma("pool", identb_t[:], cd["ident"], csemp, writes=[cst])
        P.dma("pool", onesd_t[:], cd["ones_d"], csemp, writes=[cst])
        P.dma("sp", normg_t[:], normgT_d, csem, writes=[cst])
        P.dma("sp", fng_t[:], fngT_d, csem, writes=[cst])
        if MIX:
            ones256_t = sb("ones256", [128, 128], BF16)
            ones64b_t = sb("ones64b", [128, 128], BF16)
            onesf_t = sb("onesf", [128, 128], F32)
            bdmask_t = sb("bdmask", [128, 128], BF16)
            maskD2_t = sb("maskD2", [128, 128], BF16)
            maskX_t = sb("maskX", [128, 128], BF16)
            causT_t = sb("causT", [128, 128], BF16)
            causb_t = sb("causb", [128, 128], F32)
            Lgt_t = sb("Lgt", [128, 128], F32)
            Uincl_t = sb("Uincl", [128, 128], F32)
            pow2_t = sb("pow2", [128, NIT + 1], F32)
            lbl_t = sb("lbl", [128, DEPTH, 2], F32)
            lbv_t = sb("lbv", [128, DEPTH, 2], F32)
            oml_t = sb("oml", [128, DEPTH, 2], F32)
            hgng_t = sb("hgng", [128, DEPTH, 2], F32)
            kn_t = sb("kn", [128, DEPTH, 4], F32)
            cw_t = sb("cw", [128, DEPTH, 8, 4], F32)
            cb_t = sb("cb", [128, DEPTH, 8], F32)
            ssmg_t = sb("ssmg", [128, DEPTH, 4], F32)
            dsk_t = sb("dsk", [128, DEPTH, 4], F32)
            dtb_t = sb("dtbias", [128, DEPTH, 8], F32)
            nega_t = sb("nega", [128, DEPTH, 8], F32)
            negthr_t = sb("negthr", [128, 1], F32)
            for tname, tl, q in (("ones_256", ones256_t, "pool"), ("ones_64b", ones64b_t, "pool"), ("ones_f", onesf_t, "sp"),
                                 ("bdmask", bdmask_t, "pool"), ("maskD2", maskD2_t, "pool"), ("maskX", maskX_t, "pool"), ("causT", causT_t, "pool"), ("causbias", causb_t, "sp"),
                                 ("Lgt", Lgt_t, "sp"), ("Uincl", Uincl_t, "sp"), ("pow2", pow2_t, "sp")):
                P.dma(q, tl[:], cd[tname], csemp if q == "pool" else csem, writes=[cst])
            for dsrc, tl in ((lblT_d, lbl_t), (hgngT_d, hgng_t), (knT_d, kn_t), (convwT_d, cw_t), (convbT_d, cb_t),
                             (ssmgT_d, ssmg_t), (dskT_d, dsk_t), (dtbB_d, dtb_t), (alogB_d, nega_t)):
                P.dma("sp", tl[:], dsrc, csem, writes=[cst])
        P.op("dve", lambda e: e.memset(eps_t[:], EPS), writes=[cst])
        P.op("dve", lambda e: e.memset(one_t[:], 1.0), writes=[cst])
        cst.w = P.snapshot()
        if MIX:
            P.op("dve", lambda e: e.memset(negthr_t[:], -1e29), reads=[cst], writes=[cst])
            P.op("dve", lambda e: e.memset(lbv_t[:], 0.0), reads=[cst], writes=[cst])
            P.op("dve", lambda e: e.tensor_tensor(out=lbv_t[:, 1, :], in0=lbl_t[:, 1, :], in1=lbl_t[:, 0, :], op=ALU.subtract),
                 reads=[cst], writes=[cst])
            P.op("act", lambda e: e.activation(out=lbv_t[:, 1, :], in_=lbv_t[:, 1, :], func=AF.Sigmoid), reads=[cst], writes=[cst])
            P.op("dve", lambda e: e.tensor_scalar(out=oml_t[:], in0=lbv_t[:], scalar1=-1.0, scalar2=1.0, op0=ALU.mult, op1=ALU.add),
                 reads=[cst], writes=[cst])
            P.op("act", lambda e: e.activation(out=nega_t[:], in_=nega_t[:], func=AF.Exp), reads=[cst], writes=[cst])
            P.op("dve", lambda e: e.tensor_scalar(out=nega_t[:], in0=nega_t[:], scalar1=-1.0, scalar2=None, op0=ALU.mult),
                 reads=[cst], writes=[cst])

        ps_t = [es.enter_context(nc.psum_tensor(f"ps{i}", [128, 512], F32)) for i in range(8)]
        ps_b = [Buf(f"ps{i}") for i in range(8)]
        ps_rr = [0]

        ps_held = set()

        def psum(hold=False):
            while True:
                i = ps_rr[0] % 8
                ps_rr[0] += 1
                if i not in ps_held:
                    break
            if hold:
                ps_held.add(i)
            return V(ps_t[i][:], ps_b[i])

        def psum_release(v):
            ps_held.discard(ps_b.index(v.buf))

        def bf(pm):
            return pm.ap.bitcast(BF16)

        xb = [[Buf(f"x{c}_{t}") for t in range(4)] for c in range(8)]
        hb = [[Buf(f"h{c}_{t}") for t in range(4)] for c in range(8)]

        def xv(c, t):
            return V(xT_t[:, c, t * 512:(t + 1) * 512], xb[c][t])

        def hv(c, t):
            return V(hT_t[:, c, t * 512:(t + 1) * 512], hb[c][t])

        ring_rr = [0]

        def ring_next():
            i = ring_rr[0] % RING
            ring_rr[0] += 1
            return i

        def load_wset(l, names):
            i = ring_next()
            views = {}
            pos = 0
            for nm in names:
                n = W2N[nm]
                dst = ring_t[i][:, pos * 8:(pos + n) * 8].rearrange("p (k c) -> p k c", c=n)
                src = win2_d[l][:, W2OFF[nm]:W2OFF[nm] + n].rearrange("(k p) c -> p k c", p=128)
                P.dma("pool", dst, src, ring_sem[i], writes=[ring_b[i]])
                views[nm] = dst
                pos += n
            return i, views

        def load_wout(l, row0, nj):
            i = ring_next()
            dst = ring_t[i][:, 0:nj * 1024].rearrange("p (j m) -> p j m", m=1024)
            P.dma("pool", dst, wout_d[l][row0:row0 + nj * 128, :].rearrange("(j p) m -> p j m", p=128), ring_sem[i],
                  writes=[ring_b[i]])
            return i, dst

        def proj_fm(wview, r, t):
            pm = psum()
            for k in range(8):
                P.op("pe", lambda e, k=k: e.matmul(pm.ap, lhsT=wview[:, k, :], rhs=hT_t[:, k, t * 512:(t + 1) * 512],
                                                   start=(k == 0), stop=(k == 7)),
                     reads=[ring_b[r], hb[k][t]], writes=[pm], inc=(k == 7))
            return pm

        def proj_tm(wview, rbuf, tt, n):
            pm = psum()
            for k in range(8):
                P.op("pe", lambda e, k=k: e.matmul(pm.ap[:, 0:n], lhsT=hT_t[:, k, tt * 128:(tt + 1) * 128], rhs=wview[:, k, :],
                                                   start=(k == 0), stop=(k == 7)),
                     reads=[rbuf, hb[k][tt // 4]], writes=[pm], inc=(k == 7))
            return pm

        if cfg["hostmod"]:
            P.dma("sp", modT_t[:], modin_d, P.dma_sem("modin"), writes=[modb])
        else:
            with ExitStack() as ph:
                wa_t = [sb(f"wada{i}", [128, 8, 768], F32, ph) for i in range(2)]
                wa_b = [P.buf(f"wada{i}") for i in range(2)]
                wa_sem = [P.dma_sem(f"wada{i}") for i in range(2)]
                cT_t = sb("cT", [128, 8, NSQ], F32, ph)
                bada_t = sb("badaT", [128, DEPTH, 72], F32, ph)
                condb = P.buf("cond")
                cond_sem = P.dma_sem("cond")
                P.dma("sp", cT_t[:], cT_d, cond_sem, writes=[condb])
                P.dma("sp", bada_t[:], badaT_d, cond_sem, writes=[condb])
                P.op("act", lambda e: e.activation(out=cT_t[:], in_=cT_t[:], func=AF.Silu), reads=[condb], writes=[condb])
                npiece = 12
                for l in LAYERS:
                    pm = psum()
                    for pc in range(npiece):
                        i = pc % 2
                        P.dma("sp", wa_t[i][:], wada_d[l][:, pc * 768:(pc + 1) * 768].rearrange("(k p) c -> p k c", p=128),
                              wa_sem[i], writes=[wa_b[i]])
                        for m in range(6):
                            mg = pc * 6 + m
                            for k in range(8):
                                P.op("pe", lambda e, i=i, m=m, k=k, mg=mg: e.matmul(
                                    pm.ap[:, mg * NSQ:(mg + 1) * NSQ], lhsT=wa_t[i][:, k, m * 128:(m + 1) * 128], rhs=cT_t[:, k, :],
                                    start=(k == 0), stop=(k == 7)),
                                    reads=[wa_b[i], condb], writes=[pm], inc=(k == 7))
                    P.op("dve", lambda e, l=l: e.tensor_tensor(
                        out=modT_t[:, l, :, :], in0=pm.ap[:, 0:72 * NSQ].rearrange("p (m s) -> p m s", s=NSQ),
                        in1=bada_t[:, l, :].unsqueeze(2).to_broadcast([128, 72, NSQ]), op=ALU.add),
                        reads=[pm, condb], writes=[modb])
        for l in LAYERS:
            for s in range(NSQ):
                for j in range(3):
                    P.op("dve", lambda e, l=l, s=s, j=j: e.scalar_tensor_tensor(
                        out=AG_t[:, l, s, j, :], in0=modT_t[:, l, (3 * j + 1) * 8:(3 * j + 2) * 8, s], scalar=1.0,
                        in1=normg_t[:, l, j * 8:(j + 1) * 8], op0=ALU.add, op1=ALU.mult),
                        reads=[modb, cst], writes=[modb])
                    P.op("dve", lambda e, l=l, s=s, j=j: e.tensor_scalar(
                        out=AG_t[:, l, s, 3 + j, :], in0=modT_t[:, l, (3 * j + 2) * 8:(3 * j + 3) * 8, s],
                        scalar1=(1.0 if j == 1 else 0.5), scalar2=None, op0=ALU.mult),
                        reads=[modb], writes=[modb])

        def Avec(l, s, j, c):
            return AG_t[:, l, s, j, c:c + 1]

        def Gvec(l, s, j, c):
            return AG_t[:, l, s, 3 + j, c:c + 1]

        def Bvec(l, s, j, c):
            return modT_t[:, l, 3 * j * 8 + c, s:s + 1]

        def rms_state(ph, tag, C, ones_t):
            return dict(C=C, T=512, ones=ones_t,
                        sq=sb(f"sq_{tag}", [128, C, 512], BF16, ph), rs=sb(f"rs_{tag}", [128, 512], F32, ph),
                        tm=sb(f"tm_{tag}", [128, 2, 512], F32, ph),
                        sqb=P.buf("sq"), rsb=P.buf("rs"), tmb=[P.buf("tm0"), P.buf("tm1")])

        def rms_run(st, srcs, outs, scale_fn, bias_fn):
            C = st["C"]
            T = st["T"]
            sqv = V(st["sq"][:], st["sqb"])
            for c in range(C):
                P.op("act", lambda e, c=c: e.activation(out=st["sq"][:, c, :], in_=srcs[c].ap, func=AF.Square),
                     reads=[srcs[c]], writes=[sqv])
            pm = psum()
            for c in range(C):
                P.op("pe", lambda e, c=c: e.matmul(pm.ap[:, 0:T], lhsT=st["ones"][:], rhs=st["sq"][:, c, :],
                                                   start=(c == 0), stop=(c == C - 1)),
                     reads=[sqv, cst], writes=[pm], inc=(c == C - 1))
            rsv = V(st["rs"][:], st["rsb"])
            P.op("act", lambda e: e.activation(out=st["rs"][:], in_=pm.ap[:, 0:T], func=AF.Sqrt, bias=eps_t[:], scale=1.0),
                 reads=[pm, cst], writes=[rsv])
            P.op("dve", lambda e: e.reciprocal(out=st["rs"][:], in_=st["rs"][:]), reads=[rsv], writes=[rsv])
            for c in range(C):
                bias = bias_fn(c) if bias_fn is not None else None
                if bias is None:
                    P.op("dve", lambda e, c=c: e.scalar_tensor_tensor(
                        out=outs[c].ap, in0=srcs[c].ap, scalar=scale_fn(c), in1=st["rs"][:], op0=ALU.mult, op1=ALU.mult),
                        reads=[srcs[c], rsv, modb, cst], writes=[outs[c]])
                else:
                    tb = st["tmb"][c % 2]
                    P.op("dve", lambda e, c=c: e.scalar_tensor_tensor(
                        out=st["tm"][:, c % 2, :], in0=srcs[c].ap, scalar=scale_fn(c), in1=st["rs"][:], op0=ALU.mult, op1=ALU.mult),
                        reads=[srcs[c], rsv, modb, cst], writes=[tb])
                    P.op("act", lambda e, c=c, bias=bias: e.activation(
                        out=outs[c].ap, in_=st["tm"][:, c % 2, :], func=AF.Identity, bias=bias, scale=1.0),
                        reads=[tb, modb], writes=[outs[c]])

        def norm_mod(l, s, j):
            with ExitStack() as ph:
                st = rms_state(ph, "nm", 8, onesd_t)
                for t in range(4):
                    rms_run(st, [xv(c, t) for c in range(8)], [hv(c, t) for c in range(8)],
                            lambda c: Avec(l, s, j, c), lambda c: Bvec(l, s, j, c))

        def add_wout(l, s, r2, wo, nj, rhs_fn, rhs_bufs, t0, ntok):
            for m in range(8):
                pw = psum()
                for jj in range(nj):
                    P.op("pe", lambda e, jj=jj, m=m: e.matmul(pw.ap[:, 0:ntok], lhsT=wo[:, jj, m * 128:(m + 1) * 128], rhs=rhs_fn(jj),
                                                             start=(jj == 0), stop=(jj == nj - 1)),
                         reads=[ring_b[r2]] + list(rhs_bufs), writes=[pw], inc=(jj == nj - 1))
                xbuf = xb[m][t0 // 512]
                P.op("dve", lambda e, m=m: e.scalar_tensor_tensor(
                    out=xT_t[:, m, t0:t0 + ntok], in0=pw.ap[:, 0:ntok], scalar=Gvec(l, s, 1, m), in1=xT_t[:, m, t0:t0 + ntok],
                    op0=ALU.mult, op1=ALU.add), reads=[pw, xbuf, modb], writes=[xbuf])

        def ffn(l, s, i):
            j = 0 if i == 0 else 2
            norm_mod(l, s, j)
            with ExitStack() as ph:
                a_t = [sb(f"a{q}", [128, 2, 512], BF16, ph) for q in range(3)]
                a_b = [P.buf(f"a{q}") for q in range(3)]
                sg_t = [sb(f"sg{q}", [128, 512], F32, ph) for q in range(2)]
                sg_b = [P.buf(f"sg{q}") for q in range(2)]
                NG = 11
                wgu = wgu_d[l, i].rearrange("(k p) c -> p k c", p=128)
                wdn = wdn_d[l, i]

                def load(g):
                    r = ring_next()
                    rt = ring_t[r]
                    P.dma("pool", rt[:, 0:2048].rearrange("p (k c) -> p k c", c=256), wgu[:, :, g * 256:(g + 1) * 256],
                          ring_sem[r], writes=[ring_b[r]])
                    P.dma("pool", rt[:, 2048:4096].rearrange("p (k c) -> p k c", c=256),
                          wgu[:, :, DFF + g * 256:DFF + (g + 1) * 256], ring_sem[r], writes=[ring_b[r]])
                    P.dma("pool", rt[:, 4096:6144].rearrange("p (j m) -> p j m", m=1024),
                          wdn[g * 256:(g + 1) * 256, :].rearrange("(j p) m -> p j m", p=128), ring_sem[r], writes=[ring_b[r]])
                    return r
                slots = {0: load(0)}
                slots[1] = load(1)
                items = [(g, t) for g in range(NG) for t in range(4)]
                sgq = [0]

                def emit_gu(n):
                    g, t = items[n]
                    r = slots[g]
                    wg = ring_t[r][:, 0:2048].rearrange("p (k c) -> p k c", c=256)
                    wu = ring_t[r][:, 2048:4096].rearrange("p (k c) -> p k c", c=256)
                    av = a_b[n % 3]
                    for jj in range(2):
                        pg = psum()
                        pu = psum()
                        for k in range(8):
                            P.op("pe", lambda e, k=k, jj=jj: e.matmul(pg.ap, lhsT=wg[:, k, jj * 128:(jj + 1) * 128], rhs=hv(k, t).ap,
                                                                     start=(k == 0), stop=(k == 7)),
                                 reads=[ring_b[r], hv(k, t)], writes=[pg], inc=(k == 7))
                        for k in range(8):
                            P.op("pe", lambda e, k=k, jj=jj: e.matmul(pu.ap, lhsT=wu[:, k, jj * 128:(jj + 1) * 128], rhs=hv(k, t).ap,
                                                                     start=(k == 0), stop=(k == 7)),
                                 reads=[ring_b[r], hv(k, t)], writes=[pu], inc=(k == 7))
                        q = sgq[0] % 2
                        sgq[0] += 1
                        P.op("act", lambda e, q=q: e.activation(out=sg_t[q][:], in_=pg.ap, func=AF.Silu),
                             reads=[pg], writes=[sg_b[q]])
                        P.op("dve", lambda e, q=q, jj=jj: e.tensor_tensor(out=a_t[n % 3][:, jj, :], in0=pu.ap, in1=sg_t[q][:], op=ALU.mult),
                             reads=[pu, sg_b[q]], writes=[av])

                def emit_down(n):
                    g, t = items[n]
                    r = slots[g]
                    wd = ring_t[r][:, 4096:6144].rearrange("p (j m) -> p j m", m=1024)
                    for m in range(8):
                        po = psum()
                        for jj in range(2):
                            P.op("pe", lambda e, jj=jj, m=m: e.matmul(po.ap, lhsT=wd[:, jj, m * 128:(m + 1) * 128], rhs=a_t[n % 3][:, jj, :],
                                                                     start=(jj == 0), stop=(jj == 1)),
                                 reads=[ring_b[r], a_b[n % 3]], writes=[po], inc=(jj == 1))
                        P.op("dve", lambda e, m=m: e.scalar_tensor_tensor(
                            out=xv(m, t).ap, in0=po.ap, scalar=Gvec(l, s, j, m), in1=xv(m, t).ap, op0=ALU.mult, op1=ALU.add),
                            reads=[po, xv(m, t), modb], writes=[xv(m, t)])

                for n in range(len(items)):
                    g, t = items[n]
                    emit_gu(n)
                    if n > 0:
                        emit_down(n - 1)
                    if t == 0 and g + 2 < NG:
                        slots[g + 2] = load(g + 2)
                emit_down(len(items) - 1)

        def mixer_hg(l, s, slot):
            with ExitStack() as ph:
                qt = sb("hg_qt", [128, S], BF16, ph)
                kt = sb("hg_kt", [128, S], BF16, ph)
                ktok = sb("hg_ktok", [128, NT, 128], BF16, ph)
                vtok = sb("hg_vtok", [128, NT, 128], BF16, ph)
                e123 = sb("hg_e", [128, 3, 32], F32, ph)
                bmid = sb("hg_bmid", [128, 32], F32, ph)
                qB = sb("hg_qB", [128, S], BF16, ph)
                kB = sb("hg_kB", [128, S], BF16, ph)
                bh = sb("hg_bh", [128, 64], F32, ph)
                qtb, ktb, ktokb, vtokb, eb = P.buf("qt"), P.buf("kt"), P.buf("ktok"), P.buf("vtok"), P.buf("e")
                qBb, kBb = P.buf("qB"), P.buf("kB")
                r, wv = load_wset(l, [f"HQ{slot}", f"HF{slot}", f"HG{slot}", f"HI{slot}"])
                rb = ring_b[r]
                lbp = lbv_t[:, l, slot:slot + 1]
                omlp = oml_t[:, l, slot:slot + 1]
                with ExitStack() as pa:
                    bb = sb("hg_bb", [128, S], F32, pa)
                    bb2 = sb("hg_bb2", [128, S], F32, pa)
                    bb2b = P.buf("bb2")
                    tA = [sb(f"hg_tA{i}", [128, 512], F32, pa) for i in range(2)]
                    rmask = sb("hg_rmask", [128, S], BF16, pa)
                    bbb, tAb, rmb = P.buf("bb"), [P.buf("tA0"), P.buf("tA1")], P.buf("rmask")
                    P.dma("pool", rmask[:], cd["rmask"], misc_sem, writes=[rmb])
                    for t in range(4):
                        sl = slice(t * 512, (t + 1) * 512)
                        pf = proj_fm(wv[f"HF{slot}"], r, t)
                        P.op("act", lambda e: e.activation(out=tA[0][:], in_=pf.ap, func=AF.Sigmoid), reads=[pf], writes=[tAb[0]])
                        P.op("dve", lambda e: e.tensor_scalar(out=tA[0][:], in0=tA[0][:], scalar1=omlp, scalar2=lbp, op0=ALU.mult, op1=ALU.add),
                             reads=[tAb[0], cst], writes=[tAb[0]])
                        P.op("act", lambda e: e.activation(out=bb[:, sl], in_=tA[0][:], func=AF.Ln), reads=[tAb[0]], writes=[bbb])
                        P.op("dve", lambda e: e.tensor_scalar(out=kt[:, sl], in0=tA[0][:], scalar1=-1.0, scalar2=1.0, op0=ALU.mult, op1=ALU.add),
                             reads=[tAb[0]], writes=[ktb])
                        pq = proj_fm(wv[f"HQ{slot}"], r, t)
                        P.op("act", lambda e: e.activation(out=qt[:, sl], in_=pq.ap, func=AF.Copy), reads=[pq], writes=[qtb])
                        for tt in range(t * 4, t * 4 + 4):
                            pv = proj_tm(wv[f"HI{slot}"], rb, tt, 128)
                            P.op("dve", lambda e, tt=tt: e.tensor_copy(out=vtok[:, tt, :], in_=pv.ap[:, 0:128]), reads=[pv], writes=[vtokb])
                    P.op("dve", lambda e: e.tensor_tensor_scan(out=bb[:], data0=rmask[:], data1=bb[:], initial=0.0, op0=ALU.mult, op1=ALU.add),
                         reads=[bbb, rmb], writes=[bbb])
                    bb3 = bb[:].rearrange("p (c j) -> p c j", j=64)
                    bb4 = bb[:].rearrange("p (c j) -> p c j", j=32)
                    P.op("dve", lambda e: e.tensor_copy(out=bh[:], in_=bb4[:, :, 15]), reads=[bbb], writes=[eb])
                    P.op("dve", lambda e: e.tensor_tensor(out=bb2[:].rearrange("p (c j) -> p c j", j=32), in0=bb4,
                                                          in1=bh[:].unsqueeze(2).to_broadcast([128, 64, 32]), op=ALU.subtract),
                         reads=[bbb, eb], writes=[bb2b])
                    for t in range(4):
                        sl = slice(t * 512, (t + 1) * 512)
                        P.op("act", lambda e: e.activation(out=tA[0][:], in_=bb2[:, sl], func=AF.Exp), reads=[bb2b], writes=[tAb[0]])
                        P.op("dve", lambda e: e.tensor_tensor(out=qB[:, sl], in0=qt[:, sl], in1=tA[0][:], op=ALU.mult), reads=[qtb, tAb[0]], writes=[qBb])
                        P.op("act", lambda e: e.activation(out=tA[1][:], in_=bb2[:, sl], func=AF.Exp, scale=-1.0), reads=[bb2b], writes=[tAb[1]])
                        P.op("dve", lambda e: e.tensor_tensor(out=kB[:, sl], in0=kt[:, sl], in1=tA[1][:], op=ALU.mult), reads=[ktb, tAb[1]], writes=[kBb])
                    P.op("act", lambda e: e.activation(out=e123[:, 0, :], in_=bb3[:, :, 31], func=AF.Exp), reads=[bbb], writes=[eb])
                    P.op("act", lambda e: e.activation(out=e123[:, 2, :], in_=bb3[:, :, 63], func=AF.Exp), reads=[bbb], writes=[eb])
                    P.op("dve", lambda e: e.tensor_copy(out=bmid[:], in_=bb3[:, :, 31]), reads=[bbb], writes=[eb])
                    P.op("dve", lambda e: e.tensor_tensor(out=bb3, in0=bb3, in1=bmid[:].unsqueeze(2).to_broadcast([128, 32, 64]), op=ALU.subtract),
                         reads=[bbb, eb], writes=[bbb])
                    P.op("act", lambda e: e.activation(out=e123[:, 1, :], in_=bb3[:, :, 63], func=AF.Exp), reads=[bbb], writes=[eb])
                    for t in range(4):
                        sl = slice(t * 512, (t + 1) * 512)
                        P.op("act", lambda e: e.activation(out=tA[0][:], in_=bb[:, sl], func=AF.Exp), reads=[bbb], writes=[tAb[0]])
                        P.op("dve", lambda e: e.tensor_tensor(out=qt[:, sl], in0=qt[:, sl], in1=tA[0][:], op=ALU.mult), reads=[qtb, tAb[0]], writes=[qtb])
                        P.op("act", lambda e: e.activation(out=tA[1][:], in_=bb[:, sl], func=AF.Exp, scale=-1.0), reads=[bbb], writes=[tAb[1]])
                        P.op("dve", lambda e: e.tensor_tensor(out=kt[:, sl], in0=kt[:, sl], in1=tA[1][:], op=ALU.mult), reads=[ktb, tAb[1]], writes=[ktb])
                for t4 in range(4):
                    pm = psum()
                    pmb = bf(pm)
                    for i in range(4):
                        tt = t4 * 4 + i
                        P.op("pe", lambda e, i=i, tt=tt: e.transpose(pmb[:, i * 128:(i + 1) * 128], kt[:, tt * 128:(tt + 1) * 128], identb_t[:]),
                             reads=[ktb, cst], writes=[pm], inc=(i == 3))
                    P.op("dve", lambda e, t4=t4: e.tensor_copy(out=ktok[:, t4 * 4:(t4 + 1) * 4, :],
                                                               in_=pmb[:, 0:512].rearrange("p (a b) -> p a b", b=128)),
                         reads=[pm], writes=[ktokb])
                with ExitStack() as pb:
                    kvs = sb("hg_kvs", [128, 64, 32], F32, pb)
                    e3bc = sb("hg_e3bc", [128, 64, 32], F32, pb)
                    smid = sb("hg_smid", [128, 32, 64], BF16, pb)
                    scm = [sb(f"hg_scm{i}", [128, 2, 128], BF16, pb) for i in range(2)]
                    sctmp = sb("hg_sctmp", [128, 2, 32], BF16, pb)
                    sctb = P.buf("sctmp")
                    osb = sb("hg_osb", [128, 512], F32, pb)
                    sgl = sb("hg_sgl", [128, 512], F32, pb)
                    oA = [sb(f"hg_oA{i}", [128, 512], BF16, pb) for i in range(2)]
                    kvsb, e3b, smb, scb, osbb, sglb = P.buf("kvs"), P.buf("e3bc"), P.buf("smid"), [P.buf("scm0"), P.buf("scm1")], P.buf("osb"), P.buf("sgl")
                    oAb = [P.buf("oA0"), P.buf("oA1")]
                    st = rms_state(pb, "hg", 1, ones64b_t)
                    for q_ in range(2):
                        P.op("dve", lambda e, q_=q_: e.memset(scm[q_][:], 0.0), writes=[scb[q_]])
                    for c0 in range(0, 32, 8):
                        pmh = [psum(), psum()]
                        for half in range(2):
                            pm = pmh[half]
                            for idx in range(4):
                                c = c0 + 2 * idx + half
                                tt = c // 2
                                for part in range(2):
                                    last = (idx == 3 and part == 1)
                                    P.op("pe", lambda e, pm=pm, idx=idx, tt=tt, half=half, part=part: e.matmul(
                                        pm.ap[part * 64:(part + 1) * 64, idx * 64:(idx + 1) * 64],
                                        lhsT=ktok[half * 64:(half + 1) * 64, tt, part * 64:(part + 1) * 64],
                                        rhs=vtok[half * 64:(half + 1) * 64, tt, part * 64:(part + 1) * 64], start=True, stop=True),
                                        reads=[ktokb, vtokb], writes=[pm], inc=last)
                            P.op("dve", lambda e, pm=pm, c0=c0, half=half: e.tensor_tensor(
                                out=kvs[:, :, c0 + half:c0 + 8:2].rearrange("p v c -> p c v"), in0=pm.ap[:, 0:256].rearrange("p (c v) -> p c v", v=64),
                                in1=e123[:, 1, c0 + half:c0 + 8:2].unsqueeze(2).to_broadcast([128, 4, 64]), op=ALU.mult),
                                reads=[pm, eb], writes=[kvsb])
                    P.op("dve", lambda e: e.memset(e3bc[:, :, 0:1], 0.0), writes=[e3b])
                    P.op("dve", lambda e: e.tensor_copy(out=e3bc[:, :, 1:32], in_=e123[:, 2, 1:32].unsqueeze(1).to_broadcast([128, 64, 31])),
                         reads=[eb], writes=[e3b])
                    P.op("dve", lambda e: e.tensor_tensor_scan(out=kvs[:].rearrange("p v c -> p (v c)"), data0=e3bc[:].rearrange("p v c -> p (v c)"),
                                                               data1=kvs[:].rearrange("p v c -> p (v c)"), initial=0.0, op0=ALU.mult, op1=ALU.add),
                         reads=[kvsb, e3b], writes=[kvsb])
                    P.op("dve", lambda e: e.memset(smid[:, 0, :], 0.0), writes=[smb])
                    P.op("dve", lambda e: e.tensor_tensor(out=smid[:, 1:32, :], in0=kvs[:, :, 0:31].rearrange("p v c -> p c v"),
                                                          in1=e123[:, 0, 1:32].unsqueeze(2).to_broadcast([128, 31, 64]), op=ALU.mult),
                         reads=[kvsb, eb], writes=[smb])
                    r2, wo = load_wout(l, slot * 128, 1)
                    for t in range(4):
                        pos_ = [psum(hold=True), psum(hold=True)]
                        for i in range(4):
                            tt = t * 4 + i
                            q = tt % 2
                            tsl = slice(tt * 128, (tt + 1) * 128)
                            pscs = [psum(), psum()]
                            for cc in range(2):
                                c = 2 * tt + cc
                                R = slice(cc * 64, (cc + 1) * 64)
                                for part in range(2):
                                    pr = slice(part * 64, (part + 1) * 64)
                                    psc = pscs[part]
                                    P.op("pe", lambda e, psc=psc, c=c, R=R, pr=pr: e.matmul(
                                        psc.ap[R, 0:32], lhsT=kB[pr, c * 64:(c + 1) * 64], rhs=qB[pr, c * 64:c * 64 + 32],
                                        start=True, stop=True), reads=[kBb, qBb], writes=[psc], inc=False)
                                    P.op("pe", lambda e, psc=psc, c=c, R=R, pr=pr: e.matmul(
                                        psc.ap[R, 32:64], lhsT=kB[pr, c * 64:(c + 1) * 64], rhs=qB[pr, c * 64 + 32:c * 64 + 64],
                                        start=True, stop=True), reads=[kBb, qBb], writes=[psc], inc=False)
                                    P.op("pe", lambda e, psc=psc, c=c, R=R, pr=pr: e.matmul(
                                        psc.ap[R, 64:96], lhsT=kt[pr, c * 64:(c + 1) * 64], rhs=qt[pr, c * 64 + 32:c * 64 + 64],
                                        start=True, stop=True), reads=[ktb, qtb], writes=[psc], inc=True)
                            for cc in range(2):
                                R = slice(cc * 64, (cc + 1) * 64)
                                c0_ = cc * 64
                                for part in range(2):
                                    psc = pscs[part]
                                    P.op("dve", lambda e, q=q, R=R, psc=psc, c0_=c0_, part=part: e.tensor_tensor(
                                        out=scm[q][R, part, c0_:c0_ + 32], in0=psc.ap[R, 0:32], in1=bdmask_t[R, c0_:c0_ + 32], op=ALU.mult),
                                        reads=[psc, cst], writes=[scb[q]])
                                    P.op("dve", lambda e, q=q, R=R, psc=psc, c0_=c0_, part=part: e.tensor_tensor(
                                        out=scm[q][R, part, c0_ + 32:c0_ + 64], in0=psc.ap[R, 32:64], in1=maskD2_t[R, c0_ + 32:c0_ + 64], op=ALU.mult),
                                        reads=[psc, cst], writes=[scb[q]])
                                    P.op("dve", lambda e, R=R, psc=psc, c0_=c0_, part=part: e.tensor_tensor(
                                        out=sctmp[R, part, :], in0=psc.ap[R, 64:96], in1=maskX_t[R, c0_ + 32:c0_ + 64], op=ALU.mult),
                                        reads=[psc, cst], writes=[sctb])
                                    P.op("dve", lambda e, q=q, R=R, c0_=c0_, part=part: e.tensor_tensor(
                                        out=scm[q][R, part, c0_ + 32:c0_ + 64], in0=scm[q][R, part, c0_ + 32:c0_ + 64], in1=sctmp[R, part, :], op=ALU.add),
                                        reads=[scb[q], sctb], writes=[scb[q]])
                            for part in range(2):
                                po = pos_[part]
                                P.op("pe", lambda e, po=po, part=part, i=i, tt=tt, q=q: e.matmul(
                                    po.ap[part * 64:(part + 1) * 64, i * 128:(i + 1) * 128], lhsT=vtok[:, tt, part * 64:(part + 1) * 64],
                                    rhs=scm[q][:, part, :], start=True, stop=False),
                                    reads=[vtokb, scb[q]], writes=[po], inc=False)
                                for cc in range(2):
                                    c = 2 * tt + cc
                                    P.op("pe", lambda e, po=po, part=part, i=i, cc=cc, c=c: e.matmul(
                                        po.ap[part * 64:(part + 1) * 64, i * 128 + cc * 64:i * 128 + (cc + 1) * 64],
                                        lhsT=smid[part * 64:(part + 1) * 64, c, :], rhs=qt[part * 64:(part + 1) * 64, c * 64:(c + 1) * 64],
                                        start=False, stop=(cc == 1)),
                                        reads=[smb, qtb], writes=[po], inc=(cc == 1))
                        for part in range(2):
                            pr = slice(part * 64, (part + 1) * 64)
                            P.op("act", lambda e, part=part, pr=pr: e.activation(out=osb[pr, :], in_=pos_[part].ap[pr, :], func=AF.Copy),
                                 reads=[pos_[part]], writes=[osbb])
                            psum_release(pos_[part])
                        ov = V(osb[:], osbb)
                        rms_run(st, [ov], [ov], lambda c: hgng_t[:, l, slot:slot + 1], None)
                        pg = proj_fm(wv[f"HG{slot}"], r, t)
                        P.op("act", lambda e: e.activation(out=sgl[:], in_=pg.ap, func=AF.Silu), reads=[pg], writes=[sglb])
                        oq = t % 2
                        P.op("dve", lambda e, oq=oq: e.tensor_tensor(out=oA[oq][:], in0=osb[:], in1=sgl[:], op=ALU.mult),
                             reads=[osbb, sglb], writes=[oAb[oq]])
                        add_wout(l, s, r2, wo, 1, lambda jj, oq=oq: oA[oq][:], [oAb[oq]], t * 512, 512)

        def mixer_dsa(l, s):
            with ExitStack() as ph:
                kT2 = sb("ds_kT2", [128, S], BF16, ph)
                ikT2 = sb("ds_ikT2", [128, S], BF16, ph)
                vaug = sb("ds_vaug", [128, NT, 65], BF16, ph)
                iwt = sb("ds_iwt", [128, NT, 8], F32, ph)
                qT = sb("ds_qT", [128, 2, S], BF16, ph)
                iqT = sb("ds_iqT", [128, 4, S], BF16, ph)
                kTb, ikTb, vab, iwb, qTb, iqTb = (P.buf("kT2"), P.buf("ikT2"), P.buf("vaug"), P.buf("iwt"), P.buf("qT"), P.buf("iqT"))
                r1, w1 = load_wset(l, ["AQ0", "AQW0", "AQ1", "AQW1", "AK2", "AKW2"])
                r2, w2 = load_wset(l, ["IQ0", "IQW0", "IQ1", "IQW1", "IQ2", "IQW2"])
                r3, w3 = load_wset(l, ["IQ3", "IQW3", "IK2", "IKW2", "AVIW"])
                with ExitStack() as pa:
                    cosT = sb("ds_cos", [128, S], BF16, pa)
                    sinT = sb("ds_sin", [128, S], BF16, pa)
                    tbl = P.buf("ropetab")
                    P.dma("pool", cosT[:], cd["cosT"], misc_sem, writes=[tbl])
                    P.dma("pool", sinT[:], cd["sinT"], misc_sem, writes=[tbl])
                    t1 = sb("ds_t1", [128, 512], F32, pa)
                    t2 = sb("ds_t2", [128, 512], F32, pa)
                    t1b, t2b = P.buf("t1"), P.buf("t2")
                    x1 = sb("ds_x1", [128, 512], F32, pa)
                    x2 = sb("ds_x2", [128, 512], F32, pa)
                    xq = sb("ds_xq", [128, 512], BF16, pa)
                    rsn = sb("ds_rsn", [128, 512], F32, pa)
                    x1b, x2b, xqb, rsnb = P.buf("x1"), P.buf("x2"), P.buf("xq"), P.buf("rsn")

                    def rope_comb(dst_ap, dstbuf, a_v, b_v, t):
                        sl = slice(t * 512, (t + 1) * 512)
                        P.op("dve", lambda e: e.tensor_tensor(out=t1[:], in0=a_v.ap, in1=cosT[:, sl], op=ALU.mult), reads=[a_v, tbl], writes=[t1b])
                        P.op("dve", lambda e: e.tensor_tensor(out=t2[:], in0=b_v.ap, in1=sinT[:, sl], op=ALU.mult), reads=[b_v, tbl], writes=[t2b])
                        P.op("dve", lambda e: e.tensor_tensor(out=dst_ap, in0=t1[:], in1=t2[:], op=ALU.add), reads=[t1b, t2b], writes=[dstbuf])

                    for t in range(4):
                        sl = slice(t * 512, (t + 1) * 512)
                        for sq_ in range(2):
                            pa_ = proj_fm(w1[f"AQ{sq_}"], r1, t)
                            pb_ = proj_fm(w1[f"AQW{sq_}"], r1, t)
                            rope_comb(qT[:, sq_, sl], qTb, pa_, pb_, t)
                        pa_ = proj_fm(w1["AK2"], r1, t)
                        pb_ = proj_fm(w1["AKW2"], r1, t)
                        rope_comb(kT2[:, sl], kTb, pa_, pb_, t)
                        for sq_ in range(4):
                            rr, ww = (r2, w2) if sq_ < 3 else (r3, w3)
                            pa_ = proj_fm(ww[f"IQ{sq_}"], rr, t)
                            pb_ = proj_fm(ww[f"IQW{sq_}"], rr, t)
                            rope_comb(iqT[:, sq_, sl], iqTb, pa_, pb_, t)
                        pa_ = proj_fm(w3["IK2"], r3, t)
                        pb_ = proj_fm(w3["IKW2"], r3, t)
                        P.op("act", lambda e: e.activation(out=x1[:], in_=pa_.ap, func=AF.Copy), reads=[pa_], writes=[x1b])
                        P.op("act", lambda e: e.activation(out=x2[:], in_=pb_.ap, func=AF.Copy), reads=[pb_], writes=[x2b])
                        P.op("act", lambda e: e.activation(out=xq[:], in_=x1[:], func=AF.Copy), reads=[x1b], writes=[xqb])
                        pmn = psum()
                        P.op("pe", lambda e: e.matmul(pmn.ap, lhsT=ones64b_t[:], rhs=xq[:], start=True, stop=True), reads=[xqb, cst], writes=[pmn])
                        P.op("dve", lambda e: e.tensor_tensor(out=x1[:], in0=x1[:], in1=pmn.ap, op=ALU.subtract), reads=[x1b, pmn], writes=[x1b])
                        P.op("dve", lambda e: e.tensor_tensor(out=x2[:], in0=x2[:], in1=pmn.ap, op=ALU.subtract), reads=[x2b, pmn], writes=[x2b])
                        P.op("act", lambda e: e.activation(out=xq[:], in_=x1[:], func=AF.Square), reads=[x1b], writes=[xqb])
                        pvr = psum()
                        P.op("pe", lambda e: e.matmul(pvr.ap, lhsT=ones64b_t[:], rhs=xq[:], start=True, stop=True), reads=[xqb, cst], writes=[pvr])
                        P.op("act", lambda e: e.activation(out=rsn[:], in_=pvr.ap, func=AF.Sqrt, bias=eps_t[:], scale=1.0), reads=[pvr, cst], writes=[rsnb])
                        P.op("dve", lambda e: e.reciprocal(out=rsn[:], in_=rsn[:]), reads=[rsnb], writes=[rsnb])
                        P.op("dve", lambda e: e.scalar_tensor_tensor(out=x1[:], in0=x1[:], scalar=kn_t[:, l, 0:1], in1=rsn[:], op0=ALU.mult, op1=ALU.mult),
                             reads=[x1b, rsnb, cst], writes=[x1b])
                        P.op("act", lambda e: e.activation(out=x1[:], in_=x1[:], func=AF.Identity, bias=kn_t[:, l, 1:2], scale=1.0), reads=[x1b, cst], writes=[x1b])
                        P.op("dve", lambda e: e.scalar_tensor_tensor(out=x2[:], in0=x2[:], scalar=kn_t[:, l, 2:3], in1=rsn[:], op0=ALU.mult, op1=ALU.mult),
                             reads=[x2b, rsnb, cst], writes=[x2b])
                        P.op("act", lambda e: e.activation(out=x2[:], in_=x2[:], func=AF.Identity, bias=kn_t[:, l, 3:4], scale=1.0), reads=[x2b, cst], writes=[x2b])
                        rope_comb(ikT2[:, sl], ikTb, V(x1[:], x1b), V(x2[:], x2b), t)
                    P.op("dve", lambda e: e.memset(vaug[:, :, 64:65], 1.0), writes=[vab])
                    for tt in range(NT):
                        pv = proj_tm(w3["AVIW"], ring_b[r3], tt, 72)
                        P.op("act", lambda e, tt=tt: e.activation(out=vaug[:, tt, 0:64], in_=pv.ap[:, 0:64], func=AF.Copy), reads=[pv], writes=[vab])
                        P.op("act", lambda e, tt=tt: e.activation(out=iwt[:, tt, :], in_=pv.ap[:, 64:72], func=AF.Copy, scale=IWSCALE),
                             reads=[pv], writes=[iwb])
                with ExitStack() as pb:
                    acc = sb("ds_acc", [128, S], F32, pb)
                    mask = sb("ds_mask", [128, S], BF16, pb)
                    rl = [sb(f"ds_rl{i}", [128, 512], F32, pb) for i in range(2)]
                    PT = [sb(f"ds_PT{i}", [128, 4, 128], BF16, pb) for i in range(2)]
                    otok = sb("ds_otok", [128, 4, 64], BF16, pb)
                    oBt = sb("ds_oBt", [128, 2, 128], BF16, pb)
                    sm_ = sb("ds_small", [128, 8], F32, pb)
                    HWt = sb("ds_HW", [128, NIT + 1], F32, pb)
                    HW2 = sb("ds_HW2", [128, NIT + 1], F32, pb)
                    rden = sb("ds_rden", [128, 4], F32, pb)
                    accb, maskb, rlb, PTb, otb, oBb, smb_, rdb = (P.buf("acc"), P.buf("mask"), [P.buf("rl0"), P.buf("rl1")],
                                                                 [P.buf("PT0"), P.buf("PT1")], P.buf("otok"), P.buf("oBt"), P.buf("small"), P.buf("rden"))
                    r4, wo = load_wout(l, 256, 2)
                    rlq = [0]
                    ptq = [0]
                    for j in range(NT):
                        W = 128 * (j + 1)
                        qsl = slice(j * 128, (j + 1) * 128)
                        nkc = (W + 511) // 512
                        for kc in range(nkc):
                            Wc = min(512, W - kc * 512)
                            ksl = slice(kc * 512, kc * 512 + Wc)
                            for h in range(8):
                                part, slot = h % 2, h // 2
                                pl = psum()
                                P.op("pe", lambda e, part=part, slot=slot: e.matmul(
                                    pl.ap[:, 0:Wc], lhsT=iqT[part * 64:(part + 1) * 64, slot, qsl], rhs=ikT2[part * 64:(part + 1) * 64, ksl],
                                    start=True, stop=True), reads=[iqTb, ikTb], writes=[pl])
                                q = rlq[0] % 2
                                rlq[0] += 1
                                P.op("act", lambda e, q=q: e.activation(out=rl[q][:, 0:Wc], in_=pl.ap[:, 0:Wc], func=AF.Relu), reads=[pl], writes=[rlb[q]])
                                if h == 0:
                                    P.op("dve", lambda e, q=q: e.tensor_scalar(out=acc[:, ksl], in0=rl[q][:, 0:Wc], scalar1=iwt[:, j, 0:1], scalar2=None, op0=ALU.mult),
                                         reads=[rlb[q], iwb], writes=[accb])
                                else:
                                    P.op("dve", lambda e, q=q, h=h: e.scalar_tensor_tensor(
                                        out=acc[:, ksl], in0=rl[q][:, 0:Wc], scalar=iwt[:, j, h:h + 1], in1=acc[:, ksl], op0=ALU.mult, op1=ALU.add),
                                        reads=[rlb[q], iwb, accb], writes=[accb])
                        P.op("dve", lambda e: e.tensor_tensor(out=acc[:, qsl], in0=acc[:, qsl], in1=causb_t[:], op=ALU.add), reads=[accb, cst], writes=[accb])
                        if j >= 2:
                            P.op("dve", lambda e: e.tensor_reduce(out=sm_[:, 0:1], in_=acc[:, 0:W - 128], axis=AX.X, op=ALU.min), reads=[accb], writes=[smb_])
                            P.op("dve", lambda e: e.tensor_reduce(out=sm_[:, 1:2], in_=acc[:, 0:W], axis=AX.X, op=ALU.max), reads=[accb, smb_], writes=[smb_])
                            P.op("dve", lambda e: e.tensor_tensor(out=sm_[:, 2:3], in0=sm_[:, 1:2], in1=sm_[:, 0:1], op=ALU.subtract), reads=[smb_], writes=[smb_])
                            P.op("dve", lambda e: e.tensor_scalar(out=HWt[:], in0=pow2_t[:], scalar1=sm_[:, 2:3], scalar2=None, op0=ALU.mult), reads=[smb_, cst], writes=[smb_])
                            P.op("dve", lambda e: e.tensor_scalar(out=HW2[:], in0=HWt[:], scalar1=2.0, scalar2=None, op0=ALU.mult), reads=[smb_], writes=[smb_])
                            P.op("dve", lambda e: e.tensor_tensor(out=sm_[:, 3:4], in0=sm_[:, 0:1], in1=HWt[:, 0:1], op=ALU.add), reads=[smb_], writes=[smb_])
                            for n in range(NIT):
                                P.op("dve", lambda e: e.tensor_scalar(out=mask[:, 0:W], in0=acc[:, 0:W], scalar1=sm_[:, 3:4], scalar2=None,
                                                                      op0=ALU.is_ge, op1=ALU.add, accum_out=sm_[:, 4:5]),
                                     reads=[accb, smb_], writes=[maskb, smb_])
                                P.op("dve", lambda e, n=n: e.tensor_scalar(out=sm_[:, 5:6], in0=sm_[:, 4:5], scalar1=255.5, scalar2=HW2[:, n + 1:n + 2],
                                                                           op0=ALU.is_ge, op1=ALU.mult), reads=[smb_], writes=[smb_])
                                P.op("dve", lambda e, n=n: e.scalar_tensor_tensor(out=sm_[:, 3:4], in0=sm_[:, 5:6], scalar=HWt[:, n + 1:n + 2], in1=sm_[:, 3:4],
                                                                                  op0=ALU.subtract, op1=ALU.add), reads=[smb_], writes=[smb_])
                            P.op("dve", lambda e: e.tensor_tensor(out=sm_[:, 6:7], in0=sm_[:, 3:4], in1=HWt[:, NIT:NIT + 1], op=ALU.subtract), reads=[smb_], writes=[smb_])
                            thr = sm_[:, 6:7]
                        else:
                            thr = negthr_t[:]
                        P.op("dve", lambda e: e.tensor_scalar(out=mask[:, 0:W], in0=acc[:, 0:W], scalar1=thr, scalar2=None, op0=ALU.is_ge),
                             reads=[accb, smb_, cst], writes=[maskb])
                        po = psum(hold=True)
                        po3 = po.ap[:, 0:260].rearrange("p (h e) -> p h e", e=65)
                        first = True
                        for kb in range(j + 1):
                            kbs = slice(kb * 128, (kb + 1) * 128)
                            pmT = psum()
                            pmTb = bf(pmT)
                            P.op("pe", lambda e: e.transpose(pmTb[:, 0:128], mask[:, kbs], identb_t[:]), reads=[maskb, cst], writes=[pmT])
                            psts = [psum(), psum()]
                            q = ptq[0] % 2
                            ptq[0] += 1
                            for part in range(2):
                                for slot in range(2):
                                    P.op("pe", lambda e, part=part, slot=slot: e.matmul(
                                        psts[part].ap[:, slot * 128:(slot + 1) * 128], lhsT=kT2[part * 64:(part + 1) * 64, kbs],
                                        rhs=qT[part * 64:(part + 1) * 64, slot, qsl], start=True, stop=True),
                                        reads=[kTb, qTb], writes=[psts[part]], inc=(slot == 1))
                                P.op("act", lambda e, q=q, part=part: e.activation(
                                    out=PT[q][:, part:4:2, :], in_=psts[part].ap[:, 0:256].rearrange("p (h t) -> p h t", t=128), func=AF.Exp, scale=0.125),
                                    reads=[psts[part]], writes=[PTb[q]])
                            P.op("dve", lambda e, q=q: e.tensor_tensor(out=PT[q][:], in0=PT[q][:], in1=pmTb[:, 0:128].unsqueeze(1).to_broadcast([128, 4, 128]), op=ALU.mult),
                                 reads=[PTb[q], pmT], writes=[PTb[q]])
                            for h in range(4):
                                lastmm = (kb == j and h == 3)
                                P.op("pe", lambda e, h=h, q=q, first=first, lastmm=lastmm: e.matmul(
                                    po3[:, h, :], lhsT=PT[q][:, h, :], rhs=vaug[:, kb, :], start=first, stop=lastmm, skip_group_check=True),
                                    reads=[PTb[q], vab], writes=[po], inc=(h == 3))
                                first = False
                        P.op("dve", lambda e: e.reciprocal(out=rden[:], in_=po3[:, :, 64]), reads=[po], writes=[rdb])
                        P.op("dve", lambda e: e.tensor_tensor(out=otok[:], in0=po3[:, :, 0:64], in1=rden[:].unsqueeze(2).to_broadcast([128, 4, 64]), op=ALU.mult),
                             reads=[po, rdb], writes=[otb])
                        psum_release(po)
                        pmo = psum()
                        pmob = bf(pmo)
                        of = otok[:].rearrange("p h d -> p (h d)")
                        for sl_ in range(2):
                            P.op("pe", lambda e, sl_=sl_: e.transpose(pmob[:, sl_ * 128:(sl_ + 1) * 128], of[:, sl_ * 128:(sl_ + 1) * 128], identb_t[:]),
                                 reads=[otb, cst], writes=[pmo], inc=(sl_ == 1))
                        P.op("dve", lambda e: e.tensor_copy(out=oBt[:], in_=pmob[:, 0:256].rearrange("p (a b) -> p a b", b=128)),
                             reads=[pmo], writes=[oBb])
                        add_wout(l, s, r4, wo, 2, lambda jj: oBt[:, jj, :], [oBb], j * 128, 128)

        def mixer_ssd(l, s):
            with ExitStack() as ph:
                wsdt = sb("ss_wsdt", [128, 8, 8], BF16, ph)
                dtb = sb("ss_dt", [128, NT, 8], F32, ph)
                dA = sb("ss_dA", [128, NT, 8], F32, ph)
                wsb, dtbb = P.buf("wsdt"), P.buf("dt")
                P.dma("pool", wsdt[:], win2_d[l][:, W2OFF["SDT"]:W2OFF["SDT"] + 8].rearrange("(k p) c -> p k c", p=128), misc_sem, writes=[wsb])
                for tt in range(NT):
                    pm = proj_tm(wsdt[:], wsb, tt, 8)
                    P.op("dve", lambda e, tt=tt: e.tensor_tensor(out=dtb[:, tt, :], in0=pm.ap[:, 0:8], in1=dtb_t[:, l, :], op=ALU.add),
                         reads=[pm, cst], writes=[dtbb])
                P.op("act", lambda e: e.activation(out=dtb[:], in_=dtb[:], func=AF.Exp), reads=[dtbb], writes=[dtbb])
                P.op("act", lambda e: e.activation(out=dtb[:], in_=dtb[:], func=AF.Ln, bias=one_t[:], scale=1.0), reads=[dtbb, cst], writes=[dtbb])
                P.op("dve", lambda e: e.tensor_tensor(out=dA[:], in0=dtb[:], in1=nega_t[:, l, :].unsqueeze(1).to_broadcast([128, NT, 8]), op=ALU.mult),
                     reads=[dtbb, cst], writes=[dtbb])
                for g in range(2):
                    with ExitStack() as pg_:
                        zs = sb("ss_zs", [128, 2, S], BF16, pg_)
                        xsT = sb("ss_xsT", [128, 2, S], BF16, pg_)
                        BT = sb("ss_BT", [128, S], BF16, pg_)
                        CT = sb("ss_CT", [128, S], BF16, pg_)
                        yz = sb("ss_yz", [128, 2, S], BF16, pg_)
                        zsb, xsb, BTb, CTb = P.buf("zs"), P.buf("xsT"), P.buf("BT"), P.buf("CT")
                        yzb = [[P.buf(f"yz{i}{t}") for t in range(4)] for i in range(2)]
                        r, wv = load_wset(l, [f"Z{g}0", f"Z{g}1", f"XS{g}0", f"XS{g}1", f"B{g}", f"C{g}"])
                        with ExitStack() as pa:
                            pre = sb("ss_pre", [128, 3 + S], F32, pa)
                            cac = sb("ss_cac", [128, S], F32, pa)
                            preb, cacb = P.buf("pre"), P.buf("cac")
                            for i in range(2):
                                for t in range(4):
                                    pm = proj_fm(wv[f"Z{g}{i}"], r, t)
                                    P.op("act", lambda e, i=i, t=t: e.activation(out=zs[:, i, t * 512:(t + 1) * 512], in_=pm.ap, func=AF.Silu),
                                         reads=[pm], writes=[zsb])
                            blocks = [(f"XS{g}0", 2 * g, xsT[:, 0, :], xsb), (f"XS{g}1", 2 * g + 1, xsT[:, 1, :], xsb),
                                      (f"B{g}", 4 + g, BT[:], BTb), (f"C{g}", 6 + g, CT[:], CTb)]
                            for nm, ch, dst, dstb in blocks:
                                P.op("dve", lambda e: e.memset(pre[:, 0:3], 0.0), writes=[preb])
                                for t in range(4):
                                    pm = proj_fm(wv[nm], r, t)
                                    P.op("act", lambda e, t=t: e.activation(out=pre[:, 3 + t * 512:3 + (t + 1) * 512], in_=pm.ap, func=AF.Copy),
                                         reads=[pm], writes=[preb])
                                P.op("dve", lambda e, ch=ch: e.tensor_scalar(out=cac[:], in0=pre[:, 3:3 + S], scalar1=cw_t[:, l, ch, 3:4], scalar2=cb_t[:, l, ch:ch + 1],
                                                                             op0=ALU.mult, op1=ALU.add), reads=[preb, cst], writes=[cacb])
                                for jt in range(3):
                                    P.op("dve", lambda e, ch=ch, jt=jt: e.scalar_tensor_tensor(out=cac[:], in0=pre[:, jt:jt + S], scalar=cw_t[:, l, ch, jt:jt + 1], in1=cac[:],
                                                                                               op0=ALU.mult, op1=ALU.add), reads=[preb, cacb, cst], writes=[cacb])
                                P.op("act", lambda e, dst=dst: e.activation(out=dst, in_=cac[:], func=AF.Silu), reads=[cacb], writes=[dstb])
                        with ExitStack() as pb:
                            rhsU = sb("ss_rhsU", [128, 4, 128], F32, pb)
                            Eseg = sb("ss_Eseg", [128, 4, 128], BF16, pb)
                            Ebc = sb("ss_Ebc", [128, 4, 128], BF16, pb)
                            smx = sb("ss_smx", [128, 8], F32, pb)
                            CBm = sb("ss_CBm", [128, 128], BF16, pb)
                            MT = sb("ss_MT", [128, 4, 128], BF16, pb)
                            CsT = sb("ss_CsT", [128, 4, 128], BF16, pb)
                            xdt = sb("ss_xdt", [128, 4, 64], BF16, pb)
                            xdtd = sb("ss_xdtd", [128, 4, 64], BF16, pb)
                            Btok = sb("ss_Btok", [128, 128], BF16, pb)
                            ytmp = sb("ss_ytmp", [128, 2, 128], F32, pb)
                            prev = sb("ss_prev", [128, 4, 64], F32, pb)
                            prevb16 = sb("ss_prevb", [128, 4, 64], BF16, pb)
                            (rhsUb, Esegb, Ebcb, smxb, CBmb, MTb, CsTb, xdtb, xdtdb, Btokb, ytmpb, prevb, prevbb) = [P.buf(n) for n in
                                ("rhsU", "Eseg", "Ebc", "smx", "CBm", "MT", "CsT", "xdt", "xdtd", "Btok", "ytmp", "prev", "prevb16")]
                            st = rms_state(pb, "ss", 2, ones256_t)
                            P.op("dve", lambda e: e.memset(prev[:], 0.0), writes=[prevb])
                            for c in range(NT):
                                csl = slice(c * 128, (c + 1) * 128)
                                dAc = dA[:, c, 4 * g:4 * g + 4]
                                P.op("dve", lambda e: e.tensor_tensor(out=rhsU[:], in0=Uincl_t[:].unsqueeze(1).to_broadcast([128, 4, 128]),
                                                                      in1=dAc.unsqueeze(2).to_broadcast([128, 4, 128]), op=ALU.mult),
                                     reads=[cst, dtbb], writes=[rhsUb])
                                rf = rhsU[:].rearrange("p a b -> p (a b)")
                                pseg = psum()
                                P.op("pe", lambda e: e.matmul(pseg.ap, lhsT=Lgt_t[:], rhs=rf, start=True, stop=True), reads=[rhsUb, cst], writes=[pseg])
                                pacs = psum()
                                P.op("pe", lambda e: e.matmul(pacs.ap, lhsT=onesf_t[:], rhs=rf, start=True, stop=True), reads=[rhsUb, cst], writes=[pacs])
                                psm = psum()
                                P.op("pe", lambda e: e.matmul(psm.ap[:, 0:4], lhsT=Lgt_t[:], rhs=dAc, start=True, stop=True), reads=[dtbb, cst], writes=[psm], inc=False)
                                P.op("pe", lambda e: e.matmul(psm.ap[:, 4:8], lhsT=onesf_t[:], rhs=dAc, start=True, stop=True), reads=[dtbb, cst], writes=[psm])
                                P.op("act", lambda e: e.activation(out=Eseg[:], in_=pseg.ap.rearrange("p (a b) -> p a b", b=128), func=AF.Exp), reads=[pseg], writes=[Esegb])
                                P.op("act", lambda e: e.activation(out=Ebc[:], in_=pacs.ap.rearrange("p (a b) -> p a b", b=128), func=AF.Exp), reads=[pacs], writes=[Ebcb])
                                P.op("act", lambda e: e.activation(out=smx[:], in_=psm.ap[:, 0:8], func=AF.Exp), reads=[psm], writes=[smxb])
                                pcb = psum()
                                P.op("pe", lambda e: e.matmul(pcb.ap[:, 0:128], lhsT=BT[:, csl], rhs=CT[:, csl], start=True, stop=True), reads=[BTb, CTb], writes=[pcb])
                                P.op("dve", lambda e: e.tensor_tensor(out=CBm[:], in0=pcb.ap[:, 0:128], in1=causT_t[:], op=ALU.mult), reads=[pcb, cst], writes=[CBmb])
                                P.op("dve", lambda e: e.tensor_tensor(out=MT[:], in0=Eseg[:], in1=CBm[:].unsqueeze(1).to_broadcast([128, 4, 128]), op=ALU.mult),
                                     reads=[Esegb, CBmb], writes=[MTb])
                                P.op("dve", lambda e: e.tensor_tensor(out=CsT[:], in0=Ebc[:], in1=CT[:, csl].unsqueeze(1).to_broadcast([128, 4, 128]), op=ALU.mult),
                                     reads=[Ebcb, CTb], writes=[CsTb])
                                pxt = psum()
                                pxtb = bf(pxt)
                                for i in range(2):
                                    P.op("pe", lambda e, i=i: e.transpose(pxtb[:, i * 128:(i + 1) * 128], xsT[:, i, csl], identb_t[:]), reads=[xsb, cst], writes=[pxt], inc=False)
                                P.op("pe", lambda e: e.transpose(pxtb[:, 256:384], BT[:, csl], identb_t[:]), reads=[BTb, cst], writes=[pxt])
                                P.op("dve", lambda e: e.tensor_tensor(out=xdt[:], in0=pxtb[:, 0:256].rearrange("p (a b) -> p a b", b=64),
                                                                      in1=dtb[:, c, 4 * g:4 * g + 4].unsqueeze(2).to_broadcast([128, 4, 64]), op=ALU.mult),
                                     reads=[pxt, dtbb], writes=[xdtb])
                                P.op("dve", lambda e: e.tensor_tensor(out=xdtd[:], in0=xdt[:], in1=smx[:, 0:4].unsqueeze(2).to_broadcast([128, 4, 64]), op=ALU.mult),
                                     reads=[xdtb, smxb], writes=[xdtdb])
                                P.op("dve", lambda e: e.tensor_copy(out=Btok[:], in_=pxtb[:, 256:384]), reads=[pxt], writes=[Btokb])
                                py = psum()
                                for rr in range(4):
                                    i, part = rr // 2, rr % 2
                                    P.op("pe", lambda e, rr=rr, i=i, part=part: e.matmul(
                                        py.ap[part * 64:(part + 1) * 64, i * 128:(i + 1) * 128], lhsT=xdt[:, rr, :], rhs=MT[:, rr, :], start=True, stop=(c == 0)),
                                        reads=[xdtb, MTb], writes=[py], inc=(c == 0))
                                    if c > 0:
                                        P.op("pe", lambda e, rr=rr, i=i, part=part: e.matmul(
                                            py.ap[part * 64:(part + 1) * 64, i * 128:(i + 1) * 128], lhsT=prevb16[:, rr, :], rhs=CsT[:, rr, :], start=False, stop=True),
                                            reads=[prevbb, CsTb], writes=[py])
                                for i in range(2):
                                    P.op("dve", lambda e, i=i: e.scalar_tensor_tensor(out=ytmp[:, i, :], in0=xsT[:, i, csl], scalar=dsk_t[:, l, 2 * g + i:2 * g + i + 1],
                                                                                      in1=py.ap[:, i * 128:(i + 1) * 128], op0=ALU.mult, op1=ALU.add),
                                         reads=[xsb, py, cst], writes=[ytmpb])
                                P.op("dve", lambda e: e.tensor_tensor(out=yz[:, :, csl], in0=ytmp[:], in1=zs[:, :, csl], op=ALU.mult),
                                     reads=[ytmpb, zsb], writes=[yzb[0][c // 4], yzb[1][c // 4]])
                                pstt = psum()
                                P.op("pe", lambda e: e.matmul(pstt.ap[:, 0:256], lhsT=Btok[:], rhs=xdtd[:].rearrange("p a b -> p (a b)"), start=True, stop=True),
                                     reads=[Btokb, xdtdb], writes=[pstt])
                                P.op("dve", lambda e: e.tensor_tensor(out=prev[:], in0=prev[:], in1=smx[:, 4:8].unsqueeze(2).to_broadcast([128, 4, 64]), op=ALU.mult),
                                     reads=[prevb, smxb], writes=[prevb])
                                P.op("dve", lambda e: e.tensor_tensor(out=prev[:], in0=prev[:], in1=pstt.ap[:, 0:256].rearrange("p (a b) -> p a b", b=64), op=ALU.add),
                                     reads=[prevb, pstt], writes=[prevb])
                                P.op("act", lambda e: e.activation(out=prevb16[:], in_=prev[:], func=AF.Copy), reads=[prevb], writes=[prevbb])
                            r2, wo = load_wout(l, 512 + g * 256, 2)
                            for t in range(4):
                                tsl = slice(t * 512, (t + 1) * 512)
                                vs = [V(yz[:, i, tsl], yzb[i][t]) for i in range(2)]
                                rms_run(st, vs, vs, lambda i: ssmg_t[:, l, 2 * g + i:2 * g + i + 1], None)
                                add_wout(l, s, r2, wo, 2, lambda jj: yz[:, jj, tsl], [yzb[0][t], yzb[1][t]], t * 512, 512)

        xin_sem = [P.dma_sem(f"xin{i}") for i in range(2)]
        out_sem = [P.dma_sem(f"xout{i}") for i in range(2)]
        for s in range(NSQ):
            with ExitStack() as ph:
                st_t = [sb(f"xin{i}", [128, D], F32, ph) for i in range(2)]
                st_b = [P.buf(f"xin{i}") for i in range(2)]
                for tt in range(NT):
                    i = tt % 2
                    P.dma("sp", st_t[i][:], x_d[s, tt * 128:(tt + 1) * 128, :], xin_sem[i], writes=[st_b[i]])
                    for half in range(2):
                        pm = psum()
                        for cc in range(4):
                            c = half * 4 + cc
                            P.op("pe", lambda e, c=c, cc=cc, i=i: e.transpose(pm.ap[:, cc * 128:(cc + 1) * 128], st_t[i][:, c * 128:(c + 1) * 128], ident_t[:]),
                                 reads=[st_b[i], cst], writes=[pm], inc=(cc == 3))
                        bl = [xb[half * 4 + cc][tt // 4] for cc in range(4)]
                        if half == 0:
                            P.op("act", lambda e, half=half, tt=tt: e.activation(
                                out=xT_t[:, half * 4:(half + 1) * 4, tt * 128:(tt + 1) * 128],
                                in_=pm.ap.rearrange("p (c t) -> p c t", t=128), func=AF.Copy), reads=[pm], writes=bl)
                        else:
                            P.op("dve", lambda e, half=half, tt=tt: e.tensor_copy(
                                out=xT_t[:, half * 4:(half + 1) * 4, tt * 128:(tt + 1) * 128],
                                in_=pm.ap.rearrange("p (c t) -> p c t", t=128)), reads=[pm], writes=bl)
            for l in LAYERS:
                if cfg["ffn1"]:
                    ffn(l, s, 0)
                if MIX:
                    norm_mod(l, s, 1)
                    if "hg" in MIX:
                        for slot in range(2):
                            mixer_hg(l, s, slot)
                    if "dsa" in MIX:
                        mixer_dsa(l, s)
                    if "ssd" in MIX:
                        mixer_ssd(l, s)
                if cfg["ffn2"]:
                    ffn(l, s, 1)
            with ExitStack() as ph:
                st = rms_state(ph, "fin", 8, onesd_t)
                y_t = [sb(f"yfin{i}", [128, 8, 512], F32, ph) for i in range(2)]
                y_b = [P.buf(f"yfin{i}") for i in range(2)]
                o_t = [sb(f"otok{i}", [128, D], F32, ph) for i in range(2)]
                o_b = [P.buf(f"otok{i}") for i in range(2)]
                oq = 0
                for t in range(4):
                    yi = t % 2
                    rms_run(st, [xv(c, t) for c in range(8)], [V(y_t[yi][:, c, :], y_b[yi]) for c in range(8)],
                            lambda c: fng_t[:, c:c + 1], None)
                    for sub in range(4):
                        tt = t * 4 + sub
                        oi = oq % 2
                        oq += 1
                        for half in range(2):
                            pm = psum()
                            for cc in range(4):
                                c = half * 4 + cc
                                P.op("pe", lambda e, c=c, cc=cc, yi=yi, sub=sub: e.transpose(
                                    pm.ap[:, cc * 128:(cc + 1) * 128], y_t[yi][:, c, sub * 128:(sub + 1) * 128], ident_t[:]),
                                    reads=[y_b[yi], cst], writes=[pm], inc=(cc == 3))
                            if half == 0:
                                P.op("act", lambda e, oi=oi: e.activation(out=o_t[oi][:, 0:512], in_=pm.ap, func=AF.Copy),
                                     reads=[pm], writes=[o_b[oi]])
                            else:
                                P.op("dve", lambda e, oi=oi: e.tensor_copy(out=o_t[oi][:, 512:1024], in_=pm.ap),
                                     reads=[pm], writes=[o_b[oi]])
                        P.dma("sp", out_d[s, tt * 128:(tt + 1) * 128, :], o_t[oi][:], out_sem[oi], reads=[o_b[oi]])
        P.wait_all("sp")
        print("instructions emitted:", P.ninstr)
    return nc


def kernel(**inputs):
    inputs = {k: np.asarray(v, dtype=np.float32) for k, v in inputs.items()}
    nc = build_program(FULL_CFG)
    shared = _shared_inputs(inputs, FULL_CFG)
    in_maps = []
    for core in range(8):
        m = dict(shared)
        m.update(_host_inputs(inputs, core, FULL_CFG))
        in_maps.append(m)
    res = run_bass_kernel_spmd(nc, in_maps, core_ids=list(range(8)))
    out = np.concatenate([np.asarray(r["out"]).reshape(NSEQ, S, D) for r in res.results], axis=0)
    return out.astype(np.float32)
```

```python
import numpy as np
from contextlib import ExitStack
import concourse.bass as bass
import concourse.mybir as mybir
from concourse.bass_utils import run_bass_kernel_spmd

F32 = mybir.dt.float32
BF16 = mybir.dt.bfloat16
AF = mybir.ActivationFunctionType
ALU = mybir.AluOpType
AX = mybir.AxisListType

D = 1024
S = 2048
DEPTH = 2
DFF = 2816
NSEQ = 2
NT = S // 128
EPS = 1e-6
NEG = -1e30


class Buf:
    __slots__ = ("w", "r", "name")

    def __init__(self, name="", w=None):
        self.w = dict(w) if w else {}
        self.r = {}
        self.name = name


class V:
    __slots__ = ("ap", "buf")

    def __init__(self, ap, buf):
        self.ap = ap
        self.buf = buf

    def __getitem__(self, k):
        return V(self.ap[k], self.buf)


class Prog:
    def __init__(self, nc, es):
        self.nc = nc
        self.es = es
        self.E = dict(pe=nc.tensor, act=nc.scalar, dve=nc.vector, pool=nc.gpsimd, sp=nc.sync)
        self.sem = {}
        self.cnt = {}
        for e in ("pe", "act", "dve", "pool"):
            self.sem[e] = es.enter_context(nc.semaphore("s_" + e))
            self.cnt[e] = 0
        self.waited = {e: {} for e in self.E}
        self.ninstr = 0

    def dma_sem(self, name):
        key = "d_" + name
        self.sem[key] = self.es.enter_context(self.nc.semaphore("s_" + key))
        self.cnt[key] = 0
        return key

    def snapshot(self):
        return {k: v for k, v in self.cnt.items() if v > 0}

    def buf(self, name="", fresh=True):
        return Buf(name, self.snapshot() if fresh else None)

    def _emit_waits(self, eng, reads, writes, skipkey=None):
        need = {}
        for b in reads:
            for k, v in b.w.items():
                if v > need.get(k, 0):
                    need[k] = v
        for b in writes:
            for k, v in b.w.items():
                if k == skipkey:
                    continue
                if v > need.get(k, 0):
                    need[k] = v
            for k, v in b.r.items():
                if v > need.get(k, 0):
                    need[k] = v
        e = self.E[eng]
        wd = self.waited[eng]
        for k, v in need.items():
            if k == "pe" and eng == "pe":
                continue
            if wd.get(k, 0) >= v:
                continue
            wd[k] = v
            e.wait_ge(self.sem[k], v)
            self.ninstr += 1

    def op(self, eng, fn, reads=(), writes=(), inc=True):
        reads = [x.buf if isinstance(x, V) else x for x in reads]
        writes = [x.buf if isinstance(x, V) else x for x in writes]
        self._emit_waits(eng, reads, writes)
        ins = fn(self.E[eng])
        self.ninstr += 1
        if inc:
            self.cnt[eng] += 1
            ins.then_inc(self.sem[eng], 1)
            t = self.cnt[eng]
        else:
            t = self.cnt[eng] + 1
        for b in reads:
            if t > b.r.get(eng, 0):
                b.r[eng] = t
        for b in writes:
            b.w = {eng: t}
            b.r = {}
        return ins

    def dma(self, q, out, in_, semkey, reads=(), writes=(), **kw):
        reads = [x.buf if isinstance(x, V) else x for x in reads]
        writes = [x.buf if isinstance(x, V) else x for x in writes]
        self._emit_waits(q, reads, writes, skipkey=semkey)
        ins = self.E[q].dma_start(out=out, in_=in_, **kw)
        self.ninstr += 1
        self.cnt[semkey] += 16
        ins.then_inc(self.sem[semkey], 16)
        t = self.cnt[semkey]
        for b in reads:
            if t > b.r.get(semkey, 0):
                b.r[semkey] = t
        for b in writes:
            if semkey in b.w and len(b.w) == 1:
                b.w[semkey] = t
            else:
                b.w = {semkey: t}
            b.r = {}
        return ins

    def wait_all(self, eng):
        e = self.E[eng]
        for k, v in self.snapshot().items():
            if self.waited[eng].get(k, 0) >= v:
                continue
            self.waited[eng][k] = v
            e.wait_ge(self.sem[k], v)


O_HQ, O_HF, O_HI, O_HG, O_AQ, O_AK, O_AV, O_IQ, O_IK, O_IW, O_SZ, O_SX, O_SDT = (
    0, 256, 512, 768, 1024, 1280, 1344, 1408, 1920, 1984, 1992, 2504, 3528)
IWSCALE = float(8 ** -0.5 * 64 ** -0.5)
NIT = 11


def _win2_layout():
    off = {}
    ncol = {}
    cols = []

    def add(name, idx):
        idx = list(idx)
        off[name] = len(cols)
        ncol[name] = len(idx)
        cols.extend(idx)

    def sw(base, n):
        out = []
        for h in range(n // 64):
            b = base + h * 64
            out += list(range(b + 32, b + 64)) + list(range(b, b + 32))
        return out

    for s in range(2):
        add(f"HQ{s}", range(O_HQ + s * 128, O_HQ + (s + 1) * 128))
        add(f"HF{s}", range(O_HF + s * 128, O_HF + (s + 1) * 128))
        add(f"HG{s}", range(O_HG + s * 128, O_HG + (s + 1) * 128))
        add(f"HI{s}", range(O_HI + s * 128, O_HI + (s + 1) * 128))
    for s in range(2):
        add(f"AQ{s}", range(O_AQ + s * 128, O_AQ + (s + 1) * 128))
        add(f"AQW{s}", sw(O_AQ + s * 128, 128))
    add("AK2", list(range(O_AK, O_AK + 64)) * 2)
    add("AKW2", sw(O_AK, 64) * 2)
    for s in range(4):
        add(f"IQ{s}", range(O_IQ + s * 128, O_IQ + (s + 1) * 128))
        add(f"IQW{s}", sw(O_IQ + s * 128, 128))
    add("IK2", list(range(O_IK, O_IK + 64)) * 2)
    add("IKW2", sw(O_IK, 64) * 2)
    add("AVIW", list(range(O_AV, O_AV + 64)) + list(range(O_IW, O_IW + 8)))
    for g in range(2):
        add(f"Z{g}0", range(O_SZ + (2 * g) * 128, O_SZ + (2 * g + 1) * 128))
        add(f"Z{g}1", range(O_SZ + (2 * g + 1) * 128, O_SZ + (2 * g + 2) * 128))
        add(f"XS{g}0", range(O_SX + (2 * g) * 128, O_SX + (2 * g + 1) * 128))
        add(f"XS{g}1", range(O_SX + (2 * g + 1) * 128, O_SX + (2 * g + 2) * 128))
        add(f"B{g}", range(O_SX + 512 + g * 128, O_SX + 512 + (g + 1) * 128))
        add(f"C{g}", range(O_SX + 768 + g * 128, O_SX + 768 + (g + 1) * 128))
    add("SDT", range(O_SDT, O_SDT + 8))
    return np.array(cols, dtype=np.int64), off, ncol


W2COLS, W2OFF, W2N = _win2_layout()
NC2 = len(W2COLS)


def _consts():
    c = {}
    p = np.arange(128)
    c["ident"] = np.eye(128, dtype=np.float32)
    c["ones_d"] = np.full((128, 128), 1.0 / 1024.0, np.float32)
    c["ones_256"] = np.full((128, 128), 1.0 / 256.0, np.float32)
    c["ones_f"] = np.ones((128, 128), np.float32)
    o64 = np.zeros((128, 128), np.float32)
    o64[:64, :64] = 1.0 / 64.0
    o64[64:, 64:] = 1.0 / 64.0
    c["ones_64b"] = o64
    s_, t_ = p[:, None], p[None, :]
    c["bdmask"] = ((s_ // 64 == t_ // 64) & (t_ >= s_)).astype(np.float32)
    c["causT"] = (t_ >= s_).astype(np.float32)
    same = (s_ // 64 == t_ // 64)
    c["maskD2"] = (same & (t_ >= s_) & (s_ % 64 >= 32) & (t_ % 64 >= 32)).astype(np.float32)
    c["maskX"] = (same & (s_ % 64 < 32) & (t_ % 64 >= 32)).astype(np.float32)
    c["causbias"] = np.where(t_ <= s_, 0.0, NEG).astype(np.float32)
    c["Lgt"] = (s_ > t_).astype(np.float32)
    c["Uincl"] = (s_ <= t_).astype(np.float32)
    tt = np.arange(S)
    c["rmask"] = np.broadcast_to((tt % 64 != 0).astype(np.float32)[None, :], (128, S)).copy()
    inv_freq = 1.0 / (10000.0 ** (np.arange(0, 64, 2, dtype=np.float32) / 64.0))
    ang = tt.astype(np.float32)[:, None] * inv_freq[None, :]
    cos = np.cos(ang).astype(np.float32).T
    sin = np.sin(ang).astype(np.float32).T
    c["cosT"] = np.concatenate([cos, cos, cos, cos], axis=0)
    c["sinT"] = np.concatenate([-sin, sin, -sin, sin], axis=0)
    c["pow2"] = np.broadcast_to((2.0 ** -(np.arange(NIT + 1, dtype=np.float32) + 1.0))[None, :], (128, NIT + 1)).copy()
    return c


CONST_SHAPES = dict(ident=[128, 128], ones_d=[128, 128], ones_256=[128, 128], ones_f=[128, 128], ones_64b=[128, 128],
                    bdmask=[128, 128], maskD2=[128, 128], maskX=[128, 128], causT=[128, 128], causbias=[128, 128], Lgt=[128, 128], Uincl=[128, 128],
                    rmask=[128, S], cosT=[128, S], sinT=[128, S], pow2=[128, NIT + 1])

FULL_CFG = dict(nseq=2, layers=(0, 1), ffn1=True, mix=("hg", "dsa", "ssd"), ffn2=True, hostmod=False)


def _fm(v, nchunk):
    v = np.asarray(v, np.float32)
    lead = v.shape[:-1]
    r = v.reshape(lead + (nchunk, 128))
    r = np.moveaxis(r, -1, 0)
    return np.ascontiguousarray(r)


def _shared_inputs(inputs, cfg=FULL_CFG):
    m = {}
    if not cfg["hostmod"]:
        m["w_ada"] = np.ascontiguousarray(inputs["w_ada"])
        m["b_adaT"] = _fm(inputs["b_ada"], 72)
    m["norm_gT"] = _fm(inputs["norm_g"].reshape(DEPTH, 3 * D), 24)
    m["fnorm_gT"] = _fm(inputs["final_norm_g"], 8)
    if cfg["ffn1"] or cfg["ffn2"]:
        m["w_ffn_gu"] = np.ascontiguousarray(inputs["w_ffn_gu"])
        m["w_ffn_down"] = np.ascontiguousarray(inputs["w_ffn_down"])
    if cfg["mix"]:
        m["win2"] = np.ascontiguousarray(inputs["w_in"][:, :, W2COLS])
        m["w_out"] = np.ascontiguousarray(inputs["w_out"])
        m["lblT"] = _fm(inputs["lb_logits"], 2)
        m["hgngT"] = _fm(inputs["hg_norm_g"], 2)
        g64 = inputs["idx_k_norm_g"]
        b64 = inputs["idx_k_norm_b"]
        swp = np.concatenate([np.arange(32, 64), np.arange(0, 32)])
        kn = np.stack([np.tile(g64, (1, 2)), np.tile(b64, (1, 2)), np.tile(g64[:, swp], (1, 2)), np.tile(b64[:, swp], (1, 2))], axis=1)
        m["knT"] = np.ascontiguousarray(kn.transpose(2, 0, 1))
        m["convwT"] = np.ascontiguousarray(inputs["conv_w"].reshape(DEPTH, 4, 8, 128).transpose(3, 0, 2, 1))
        m["convbT"] = _fm(inputs["conv_b"], 8)
        m["ssmgT"] = _fm(inputs["ssm_norm_g"], 4)
        dsk = np.repeat(inputs["d_skip"], 64, axis=1)
        m["dskT"] = _fm(dsk, 4)
        m["dtbB"] = np.ascontiguousarray(np.broadcast_to(inputs["dt_bias"][None], (128, DEPTH, 8)))
        m["alogB"] = np.ascontiguousarray(np.broadcast_to(inputs["a_log"][None], (128, DEPTH, 8)))
    cc = _consts()
    for k in CONST_SHAPES:
        m[k] = cc[k]
    return m


def _host_inputs(inputs, core, cfg=FULL_CFG):
    ns = cfg["nseq"]
    b0 = core * NSEQ
    m = {"x": np.ascontiguousarray(inputs["x"][b0:b0 + ns])}
    if not cfg["hostmod"]:
        c = inputs["c"][b0:b0 + ns]
        m["cT"] = np.ascontiguousarray(c.reshape(ns, 8, 128).transpose(2, 1, 0))
    return m


def build_program(cfg=None):
    cfg = dict(FULL_CFG if cfg is None else cfg)
    NSQ = cfg["nseq"]
    LAYERS = list(cfg["layers"])
    MIX = tuple(cfg["mix"])
    nc = bass.Bass("TRN2", target_bir_lowering=False)

    def din(name, shape, dtype=F32):
        return nc.dram_tensor(name, list(shape), dtype, kind="ExternalInput").ap()

    x_d = din("x", [NSQ, S, D])
    if cfg["hostmod"]:
        modin_d = din("modT_in", [128, DEPTH, 72, NSQ])
    else:
        cT_d = din("cT", [128, 8, NSQ])
        wada_d = din("w_ada", [DEPTH, D, 9 * D])
        badaT_d = din("b_adaT", [128, DEPTH, 72])
    normgT_d = din("norm_gT", [128, DEPTH, 24])
    fngT_d = din("fnorm_gT", [128, 8])
    if cfg["ffn1"] or cfg["ffn2"]:
        wgu_d = din("w_ffn_gu", [DEPTH, 2, D, 2 * DFF])
        wdn_d = din("w_ffn_down", [DEPTH, 2, DFF, D])
    if MIX:
        win2_d = din("win2", [DEPTH, D, NC2])
        wout_d = din("w_out", [DEPTH, D, D])
        lblT_d = din("lblT", [128, DEPTH, 2])
        hgngT_d = din("hgngT", [128, DEPTH, 2])
        knT_d = din("knT", [128, DEPTH, 4])
        convwT_d = din("convwT", [128, DEPTH, 8, 4])
        convbT_d = din("convbT", [128, DEPTH, 8])
        ssmgT_d = din("ssmgT", [128, DEPTH, 4])
        dskT_d = din("dskT", [128, DEPTH, 4])
        dtbB_d = din("dtbB", [128, DEPTH, 8])
        alogB_d = din("alogB", [128, DEPTH, 8])
    cd = {k: din(k, shp) for k, shp in CONST_SHAPES.items()}
    out_d = nc.dram_tensor("out", [NSQ, S, D], F32, kind="ExternalOutput").ap()

    es = ExitStack()
    with es:
        P = Prog(nc, es)
        uid = [0]

        def sb(name, shape, dtype, st=es):
            uid[0] += 1
            return st.enter_context(nc.sbuf_tensor(f"sb{uid[0]}_{name}", list(shape), dtype))

        xT_t = sb("xT", [128, 8, S], F32)
        hT_t = sb("hT", [128, 8, S], BF16)
        RING = 3
        ring_t = [sb(f"ring{i}", [128, 6144], BF16) for i in range(RING)]
        ring_b = [Buf(f"ring{i}") for i in range(RING)]
        ring_sem = [P.dma_sem(f"ring{i}") for i in range(RING)]
        modT_t = sb("modT", [128, DEPTH, 72, NSQ], F32)
        AG_t = sb("AG", [128, DEPTH, NSQ, 6, 8], F32)
        normg_t = sb("normg", [128, DEPTH, 24], F32)
        fng_t = sb("fng", [128, 8], F32)
        eps_t = sb("eps", [128, 1], F32)
        one_t = sb("one", [128, 1], F32)
        ident_t = sb("ident", [128, 128], F32)
        identb_t = sb("identb", [128, 128], BF16)
        onesd_t = sb("onesd", [128, 128], BF16)

        cst = Buf("consts")
        modb = Buf("mod")
        csem = P.dma_sem("const")
        csemp = P.dma_sem("constp")
        misc_sem = P.dma_sem("miscp")
        P.dma("sp", ident_t[:], cd["ident"], csem, writes=[cst])
        P.dma("pool", identb_t[:], cd["ident"], csemp, writes=[cst])
        P.dma("pool", onesd_t[:], cd["ones_d"], csemp, writes=[cst])
        P.dma("sp", normg_t[:], normgT_d, csem, writes=[cst])
        P.dma("sp", fng_t[:], fngT_d, csem, writes=[cst])
        if MIX:
            ones256_t = sb("ones256", [128, 128], BF16)
            ones64b_t = sb("ones64b", [128, 128], BF16)
            onesf_t = sb("onesf", [128, 128], F32)
            bdmask_t = sb("bdmask", [128, 128], BF16)
            maskD2_t = sb("maskD2", [128, 128], BF16)
            maskX_t = sb("maskX", [128, 128], BF16)
            causT_t = sb("causT", [128, 128], BF16)
            causb_t = sb("causb", [128, 128], F32)
            Lgt_t = sb("Lgt", [128, 128], F32)
            Uincl_t = sb("Uincl", [128, 128], F32)
            pow2_t = sb("pow2", [128, NIT + 1], F32)
            lbl_t = sb("lbl", [128, DEPTH, 2], F32)
            lbv_t = sb("lbv", [128, DEPTH, 2], F32)
            oml_t = sb("oml", [128, DEPTH, 2], F32)
            hgng_t = sb("hgng", [128, DEPTH, 2], F32)
            kn_t = sb("kn", [128, DEPTH, 4], F32)
            cw_t = sb("cw", [128, DEPTH, 8, 4], F32)
            cb_t = sb("cb", [128, DEPTH, 8], F32)
            ssmg_t = sb("ssmg", [128, DEPTH, 4], F32)
            dsk_t = sb("dsk", [128, DEPTH, 4], F32)
            dtb_t = sb("dtbias", [128, DEPTH, 8], F32)
            nega_t = sb("nega", [128, DEPTH, 8], F32)
            negthr_t = sb("negthr", [128, 1], F32)
            for tname, tl, q in (("ones_256", ones256_t, "pool"), ("ones_64b", ones64b_t, "pool"), ("ones_f", onesf_t, "sp"),
                                 ("bdmask", bdmask_t, "pool"), ("maskD2", maskD2_t, "pool"), ("maskX", maskX_t, "pool"), ("causT", causT_t, "pool"), ("causbias", causb_t, "sp"),
                                 ("Lgt", Lgt_t, "sp"), ("Uincl", Uincl_t, "sp"), ("pow2", pow2_t, "sp")):
                P.dma(q, tl[:], cd[tname], csemp if q == "pool" else csem, writes=[cst])
            for dsrc, tl in ((lblT_d, lbl_t), (hgngT_d, hgng_t), (knT_d, kn_t), (convwT_d, cw_t), (convbT_d, cb_t),
                             (ssmgT_d, ssmg_t), (dskT_d, dsk_t), (dtbB_d, dtb_t), (alogB_d, nega_t)):
                P.dma("sp", tl[:], dsrc, csem, writes=[cst])
        P.op("dve", lambda e: e.memset(eps_t[:], EPS), writes=[cst])
        P.op("dve", lambda e: e.memset(one_t[:], 1.0), writes=[cst])
        cst.w = P.snapshot()
        if MIX:
            P.op("dve", lambda e: e.memset(negthr_t[:], -1e29), reads=[cst], writes=[cst])
            P.op("dve", lambda e: e.memset(lbv_t[:], 0.0), reads=[cst], writes=[cst])
            P.op("dve", lambda e: e.tensor_tensor(out=lbv_t[:, 1, :], in0=lbl_t[:, 1, :], in1=lbl_t[:, 0, :], op=ALU.subtract),
                 reads=[cst], writes=[cst])
            P.op("act", lambda e: e.activation(out=lbv_t[:, 1, :], in_=lbv_t[:, 1, :], func=AF.Sigmoid), reads=[cst], writes=[cst])
            P.op("dve", lambda e: e.tensor_scalar(out=oml_t[:], in0=lbv_t[:], scalar1=-1.0, scalar2=1.0, op0=ALU.mult, op1=ALU.add),
                 reads=[cst], writes=[cst])
            P.op("act", lambda e: e.activation(out=nega_t[:], in_=nega_t[:], func=AF.Exp), reads=[cst], writes=[cst])
            P.op("dve", lambda e: e.tensor_scalar(out=nega_t[:], in0=nega_t[:], scalar1=-1.0, scalar2=None, op0=ALU.mult),
                 reads=[cst], writes=[cst])

        ps_t = [es.enter_context(nc.psum_tensor(f"ps{i}", [128, 512], F32)) for i in range(8)]
        ps_b = [Buf(f"ps{i}") for i in range(8)]
        ps_rr = [0]

        ps_held = set()

        def psum(hold=False):
            while True:
                i = ps_rr[0] % 8
                ps_rr[0] += 1
                if i not in ps_held:
                    break
            if hold:
                ps_held.add(i)
            return V(ps_t[i][:], ps_b[i])

        def psum_release(v):
            ps_held.discard(ps_b.index(v.buf))

        def bf(pm):
            return pm.ap.bitcast(BF16)

        xb = [[Buf(f"x{c}_{t}") for t in range(4)] for c in range(8)]
        hb = [[Buf(f"h{c}_{t}") for t in range(4)] for c in range(8)]

        def xv(c, t):
            return V(xT_t[:, c, t * 512:(t + 1) * 512], xb[c][t])

        def hv(c, t):
            return V(hT_t[:, c, t * 512:(t + 1) * 512], hb[c][t])

        ring_rr = [0]

        def ring_next():
            i = ring_rr[0] % RING
            ring_rr[0] += 1
            return i

        def load_wset(l, names):
            i = ring_next()
            views = {}
            pos = 0
            for nm in names:
                n = W2N[nm]
                dst = ring_t[i][:, pos * 8:(pos + n) * 8].rearrange("p (k c) -> p k c", c=n)
                src = win2_d[l][:, W2OFF[nm]:W2OFF[nm] + n].rearrange("(k p) c -> p k c", p=128)
                P.dma("pool", dst, src, ring_sem[i], writes=[ring_b[i]])
                views[nm] = dst
                pos += n
            return i, views

        def load_wout(l, row0, nj):
            i = ring_next()
            dst = ring_t[i][:, 0:nj * 1024].rearrange("p (j m) -> p j m", m=1024)
            P.dma("pool", dst, wout_d[l][row0:row0 + nj * 128, :].rearrange("(j p) m -> p j m", p=128), ring_sem[i],
                  writes=[ring_b[i]])
            return i, dst

        def proj_fm(wview, r, t):
            pm = psum()
            for k in range(8):
                P.op("pe", lambda e, k=k: e.matmul(pm.ap, lhsT=wview[:, k, :], rhs=hT_t[:, k, t * 512:(t + 1) * 512],
                                                   start=(k == 0), stop=(k == 7)),
                     reads=[ring_b[r], hb[k][t]], writes=[pm], inc=(k == 7))
            return pm

        def proj_tm(wview, rbuf, tt, n):
            pm = psum()
            for k in range(8):
                P.op("pe", lambda e, k=k: e.matmul(pm.ap[:, 0:n], lhsT=hT_t[:, k, tt * 128:(tt + 1) * 128], rhs=wview[:, k, :],
                                                   start=(k == 0), stop=(k == 7)),
                     reads=[rbuf, hb[k][tt // 4]], writes=[pm], inc=(k == 7))
            return pm

        if cfg["hostmod"]:
            P.dma("sp", modT_t[:], modin_d, P.dma_sem("modin"), writes=[modb])
        else:
            with ExitStack() as ph:
                wa_t = [sb(f"wada{i}", [128, 8, 768], BF16, ph) for i in range(2)]
                cTb_t = sb("cTb", [128, 8, NSQ], BF16, ph)
                wa_b = [P.buf(f"wada{i}") for i in range(2)]
                wa_sem = [P.dma_sem(f"wada{i}") for i in range(2)]
                cT_t = sb("cT", [128, 8, NSQ], F32, ph)
                bada_t = sb("badaT", [128, DEPTH, 72], F32, ph)
                condb = P.buf("cond")
                cond_sem = P.dma_sem("cond")
                P.dma("sp", cT_t[:], cT_d, cond_sem, writes=[condb])
                P.dma("sp", bada_t[:], badaT_d, cond_sem, writes=[condb])
                P.op("act", lambda e: e.activation(out=cTb_t[:], in_=cT_t[:], func=AF.Silu), reads=[condb], writes=[condb])
                npiece = 12
                for l in LAYERS:
                    pm = psum()
                    for pc in range(npiece):
                        i = pc % 2
                        P.dma("pool", wa_t[i][:], wada_d[l][:, pc * 768:(pc + 1) * 768].rearrange("(k p) c -> p k c", p=128),
                              wa_sem[i], writes=[wa_b[i]])
                        for m in range(6):
                            mg = pc * 6 + m
                            for k in range(8):
                                P.op("pe", lambda e, i=i, m=m, k=k, mg=mg: e.matmul(
                                    pm.ap[:, mg * NSQ:(mg + 1) * NSQ], lhsT=wa_t[i][:, k, m * 128:(m + 1) * 128], rhs=cTb_t[:, k, :],
                                    start=(k == 0), stop=(k == 7)),
                                    reads=[wa_b[i], condb], writes=[pm], inc=(k == 7))
                    P.op("dve", lambda e, l=l: e.tensor_tensor(
                        out=modT_t[:, l, :, :], in0=pm.ap[:, 0:72 * NSQ].rearrange("p (m s) -> p m s", s=NSQ),
                        in1=bada_t[:, l, :].unsqueeze(2).to_broadcast([128, 72, NSQ]), op=ALU.add),
                        reads=[pm, condb], writes=[modb])
        for l in LAYERS:
            for s in range(NSQ):
                for j in range(3):
                    P.op("dve", lambda e, l=l, s=s, j=j: e.scalar_tensor_tensor(
                        out=AG_t[:, l, s, j, :], in0=modT_t[:, l, (3 * j + 1) * 8:(3 * j + 2) * 8, s], scalar=1.0,
                        in1=normg_t[:, l, j * 8:(j + 1) * 8], op0=ALU.add, op1=ALU.mult),
                        reads=[modb, cst], writes=[modb])
                    P.op("dve", lambda e, l=l, s=s, j=j: e.tensor_scalar(
                        out=AG_t[:, l, s, 3 + j, :], in0=modT_t[:, l, (3 * j + 2) * 8:(3 * j + 3) * 8, s],
                        scalar1=(1.0 if j == 1 else 0.5), scalar2=None, op0=ALU.mult),
                        reads=[modb], writes=[modb])

        def Avec(l, s, j, c):
            return AG_t[:, l, s, j, c:c + 1]

        def Gvec(l, s, j, c):
            return AG_t[:, l, s, 3 + j, c:c + 1]

        def Bvec(l, s, j, c):
            return modT_t[:, l, 3 * j * 8 + c, s:s + 1]

        def rms_state(ph, tag, C, ones_t):
            return dict(C=C, T=512, ones=ones_t,
                        sq=sb(f"sq_{tag}", [128, C, 512], BF16, ph), rs=sb(f"rs_{tag}", [128, 512], F32, ph),
                        tm=sb(f"tm_{tag}", [128, 2, 512], F32, ph),
                        sqb=P.buf("sq"), rsb=P.buf("rs"), tmb=[P.buf("tm0"), P.buf("tm1")])

        def rms_run(st, srcs, outs, scale_fn, bias_fn):
            C = st["C"]
            T = st["T"]
            sqv = V(st["sq"][:], st["sqb"])
            for c in range(C):
                P.op("act", lambda e, c=c: e.activation(out=st["sq"][:, c, :], in_=srcs[c].ap, func=AF.Square),
                     reads=[srcs[c]], writes=[sqv])
            pm = psum()
            for c in range(C):
                P.op("pe", lambda e, c=c: e.matmul(pm.ap[:, 0:T], lhsT=st["ones"][:], rhs=st["sq"][:, c, :],
                                                   start=(c == 0), stop=(c == C - 1)),
                     reads=[sqv, cst], writes=[pm], inc=(c == C - 1))
            rsv = V(st["rs"][:], st["rsb"])
            P.op("act", lambda e: e.activation(out=st["rs"][:], in_=pm.ap[:, 0:T], func=AF.Sqrt, bias=eps_t[:], scale=1.0),
                 reads=[pm, cst], writes=[rsv])
            P.op("dve", lambda e: e.reciprocal(out=st["rs"][:], in_=st["rs"][:]), reads=[rsv], writes=[rsv])
            for c in range(C):
                bias = bias_fn(c) if bias_fn is not None else None
                if bias is None:
                    P.op("dve", lambda e, c=c: e.scalar_tensor_tensor(
                        out=outs[c].ap, in0=srcs[c].ap, scalar=scale_fn(c), in1=st["rs"][:], op0=ALU.mult, op1=ALU.mult),
                        reads=[srcs[c], rsv, modb, cst], writes=[outs[c]])
                else:
                    tb = st["tmb"][c % 2]
                    P.op("dve", lambda e, c=c: e.scalar_tensor_tensor(
                        out=st["tm"][:, c % 2, :], in0=srcs[c].ap, scalar=scale_fn(c), in1=st["rs"][:], op0=ALU.mult, op1=ALU.mult),
                        reads=[srcs[c], rsv, modb, cst], writes=[tb])
                    P.op("act", lambda e, c=c, bias=bias: e.activation(
                        out=outs[c].ap, in_=st["tm"][:, c % 2, :], func=AF.Identity, bias=bias, scale=1.0),
                        reads=[tb, modb], writes=[outs[c]])

        def norm_mod(l, s, j):
            with ExitStack() as ph:
                st = rms_state(ph, "nm", 8, onesd_t)
                for t in range(4):
                    rms_run(st, [xv(c, t) for c in range(8)], [hv(c, t) for c in range(8)],
                            lambda c: Avec(l, s, j, c), lambda c: Bvec(l, s, j, c))

        def add_wout(l, s, r2, wo, nj, rhs_fn, rhs_bufs, t0, ntok):
            for m in range(8):
                pw = psum()
                for jj in range(nj):
                    P.op("pe", lambda e, jj=jj, m=m: e.matmul(pw.ap[:, 0:ntok], lhsT=wo[:, jj, m * 128:(m + 1) * 128], rhs=rhs_fn(jj),
                                                             start=(jj == 0), stop=(jj == nj - 1)),
                         reads=[ring_b[r2]] + list(rhs_bufs), writes=[pw], inc=(jj == nj - 1))
                xbuf = xb[m][t0 // 512]
                P.op("dve", lambda e, m=m: e.scalar_tensor_tensor(
                    out=xT_t[:, m, t0:t0 + ntok], in0=pw.ap[:, 0:ntok], scalar=Gvec(l, s, 1, m), in1=xT_t[:, m, t0:t0 + ntok],
                    op0=ALU.mult, op1=ALU.add), reads=[pw, xbuf, modb], writes=[xbuf])

        def ffn(l, s, i):
            j = 0 if i == 0 else 2
            norm_mod(l, s, j)
            with ExitStack() as ph:
                a_t = [sb(f"a{q}", [128, 2, 512], BF16, ph) for q in range(3)]
                a_b = [P.buf(f"a{q}") for q in range(3)]
                sg_t = [sb(f"sg{q}", [128, 512], F32, ph) for q in range(2)]
                sg_b = [P.buf(f"sg{q}") for q in range(2)]
                NG = 11
                wgu = wgu_d[l, i].rearrange("(k p) c -> p k c", p=128)
                wdn = wdn_d[l, i]

                def load(g):
                    r = ring_next()
                    rt = ring_t[r]
                    P.dma("pool", rt[:, 0:2048].rearrange("p (k c) -> p k c", c=256), wgu[:, :, g * 256:(g + 1) * 256],
                          ring_sem[r], writes=[ring_b[r]])
                    P.dma("pool", rt[:, 2048:4096].rearrange("p (k c) -> p k c", c=256),
                          wgu[:, :, DFF + g * 256:DFF + (g + 1) * 256], ring_sem[r], writes=[ring_b[r]])
                    P.dma("pool", rt[:, 4096:6144].rearrange("p (j m) -> p j m", m=1024),
                          wdn[g * 256:(g + 1) * 256, :].rearrange("(j p) m -> p j m", p=128), ring_sem[r], writes=[ring_b[r]])
                    return r
                slots = {0: load(0)}
                slots[1] = load(1)
                items = [(g, t) for g in range(NG) for t in range(4)]
                sgq = [0]

                def emit_gu(n):
                    g, t = items[n]
                    r = slots[g]
                    wg = ring_t[r][:, 0:2048].rearrange("p (k c) -> p k c", c=256)
                    wu = ring_t[r][:, 2048:4096].rearrange("p (k c) -> p k c", c=256)
                    av = a_b[n % 3]
                    for jj in range(2):
                        pg = psum()
                        pu = psum()
                        for k in range(8):
                            P.op("pe", lambda e, k=k, jj=jj: e.matmul(pg.ap, lhsT=wg[:, k, jj * 128:(jj + 1) * 128], rhs=hv(k, t).ap,
                                                                     start=(k == 0), stop=(k == 7)),
                                 reads=[ring_b[r], hv(k, t)], writes=[pg], inc=(k == 7))
                        for k in range(8):
                            P.op("pe", lambda e, k=k, jj=jj: e.matmul(pu.ap, lhsT=wu[:, k, jj * 128:(jj + 1) * 128], rhs=hv(k, t).ap,
                                                                     start=(k == 0), stop=(k == 7)),
                                 reads=[ring_b[r], hv(k, t)], writes=[pu], inc=(k == 7))
                        q = sgq[0] % 2
                        sgq[0] += 1
                        P.op("act", lambda e, q=q: e.activation(out=sg_t[q][:], in_=pg.ap, func=AF.Silu),
                             reads=[pg], writes=[sg_b[q]])
                        P.op("dve", lambda e, q=q, jj=jj: e.tensor_tensor(out=a_t[n % 3][:, jj, :], in0=pu.ap, in1=sg_t[q][:], op=ALU.mult),
                             reads=[pu, sg_b[q]], writes=[av])

                def emit_down(n):
                    g, t = items[n]
                    r = slots[g]
                    wd = ring_t[r][:, 4096:6144].rearrange("p (j m) -> p j m", m=1024)
                    for m in range(8):
                        po = psum()
                        for jj in range(2):
                            P.op("pe", lambda e, jj=jj, m=m: e.matmul(po.ap, lhsT=wd[:, jj, m * 128:(m + 1) * 128], rhs=a_t[n % 3][:, jj, :],
                                                                     start=(jj == 0), stop=(jj == 1)),
                                 reads=[ring_b[r], a_b[n % 3]], writes=[po], inc=(jj == 1))
                        P.op("dve", lambda e, m=m: e.scalar_tensor_tensor(
                            out=xv(m, t).ap, in0=po.ap, scalar=Gvec(l, s, j, m), in1=xv(m, t).ap, op0=ALU.mult, op1=ALU.add),
                            reads=[po, xv(m, t), modb], writes=[xv(m, t)])

                for n in range(len(items)):
                    g, t = items[n]
                    emit_gu(n)
                    if n > 0:
                        emit_down(n - 1)
                    if t == 0 and g + 2 < NG:
                        slots[g + 2] = load(g + 2)
                emit_down(len(items) - 1)

        def mixer_hg(l, s, slot):
            with ExitStack() as ph:
                qt = sb("hg_qt", [128, S], BF16, ph)
                kt = sb("hg_kt", [128, S], BF16, ph)
                ktok = sb("hg_ktok", [128, NT, 128], BF16, ph)
                vtok = sb("hg_vtok", [128, NT, 128], BF16, ph)
                e123 = sb("hg_e", [128, 3, 32], F32, ph)
                bmid = sb("hg_bmid", [128, 32], F32, ph)
                qB = sb("hg_qB", [128, S], BF16, ph)
                kB = sb("hg_kB", [128, S], BF16, ph)
                bh = sb("hg_bh", [128, 64], F32, ph)
                qtb, ktb, ktokb, vtokb, eb = P.buf("qt"), P.buf("kt"), P.buf("ktok"), P.buf("vtok"), P.buf("e")
                qBb, kBb = P.buf("qB"), P.buf("kB")
                r, wv = load_wset(l, [f"HQ{slot}", f"HF{slot}", f"HG{slot}", f"HI{slot}"])
                rb = ring_b[r]
                lbp = lbv_t[:, l, slot:slot + 1]
                omlp = oml_t[:, l, slot:slot + 1]
                with ExitStack() as pa:
                    bb = sb("hg_bb", [128, S], F32, pa)
                    bb2 = sb("hg_bb2", [128, S], F32, pa)
                    bb2b = P.buf("bb2")
                    tA = [sb(f"hg_tA{i}", [128, 512], F32, pa) for i in range(2)]
                    rmask = sb("hg_rmask", [128, S], BF16, pa)
                    bbb, tAb, rmb = P.buf("bb"), [P.buf("tA0"), P.buf("tA1")], P.buf("rmask")
                    P.dma("pool", rmask[:], cd["rmask"], misc_sem, writes=[rmb])
                    for t in range(4):
                        sl = slice(t * 512, (t + 1) * 512)
                        pf = proj_fm(wv[f"HF{slot}"], r, t)
                        P.op("act", lambda e: e.activation(out=tA[0][:], in_=pf.ap, func=AF.Sigmoid), reads=[pf], writes=[tAb[0]])
                        P.op("dve", lambda e: e.tensor_scalar(out=tA[0][:], in0=tA[0][:], scalar1=omlp, scalar2=lbp, op0=ALU.mult, op1=ALU.add),
                             reads=[tAb[0], cst], writes=[tAb[0]])
                        P.op("act", lambda e: e.activation(out=bb[:, sl], in_=tA[0][:], func=AF.Ln), reads=[tAb[0]], writes=[bbb])
                        P.op("dve", lambda e: e.tensor_scalar(out=kt[:, sl], in0=tA[0][:], scalar1=-1.0, scalar2=1.0, op0=ALU.mult, op1=ALU.add),
                             reads=[tAb[0]], writes=[ktb])
                        pq = proj_fm(wv[f"HQ{slot}"], r, t)
                        P.op("act", lambda e: e.activation(out=qt[:, sl], in_=pq.ap, func=AF.Copy), reads=[pq], writes=[qtb])
                        for tt in range(t * 4, t * 4 + 4):
                            pv = proj_tm(wv[f"HI{slot}"], rb, tt, 128)
                            P.op("dve", lambda e, tt=tt: e.tensor_copy(out=vtok[:, tt, :], in_=pv.ap[:, 0:128]), reads=[pv], writes=[vtokb])
                    P.op("dve", lambda e: e.tensor_tensor_scan(out=bb[:], data0=rmask[:], data1=bb[:], initial=0.0, op0=ALU.mult, op1=ALU.add),
                         reads=[bbb, rmb], writes=[bbb])
                    bb3 = bb[:].rearrange("p (c j) -> p c j", j=64)
                    bb4 = bb[:].rearrange("p (c j) -> p c j", j=32)
                    P.op("dve", lambda e: e.tensor_copy(out=bh[:], in_=bb4[:, :, 15]), reads=[bbb], writes=[eb])
                    P.op("dve", lambda e: e.tensor_tensor(out=bb2[:].rearrange("p (c j) -> p c j", j=32), in0=bb4,
                                                          in1=bh[:].unsqueeze(2).to_broadcast([128, 64, 32]), op=ALU.subtract),
                         reads=[bbb, eb], writes=[bb2b])
                    for t in range(4):
                        sl = slice(t * 512, (t + 1) * 512)
                        P.op("act", lambda e: e.activation(out=tA[0][:], in_=bb2[:, sl], func=AF.Exp), reads=[bb2b], writes=[tAb[0]])
                        P.op("dve", lambda e: e.tensor_tensor(out=qB[:, sl], in0=qt[:, sl], in1=tA[0][:], op=ALU.mult), reads=[qtb, tAb[0]], writes=[qBb])
                        P.op("act", lambda e: e.activation(out=tA[1][:], in_=bb2[:, sl], func=AF.Exp, scale=-1.0), reads=[bb2b], writes=[tAb[1]])
                        P.op("dve", lambda e: e.tensor_tensor(out=kB[:, sl], in0=kt[:, sl], in1=tA[1][:], op=ALU.mult), reads=[ktb, tAb[1]], writes=[kBb])
                    P.op("act", lambda e: e.activation(out=e123[:, 0, :], in_=bb3[:, :, 31], func=AF.Exp), reads=[bbb], writes=[eb])
                    P.op("act", lambda e: e.activation(out=e123[:, 2, :], in_=bb3[:, :, 63], func=AF.Exp), reads=[bbb], writes=[eb])
                    P.op("dve", lambda e: e.tensor_copy(out=bmid[:], in_=bb3[:, :, 31]), reads=[bbb], writes=[eb])
                    P.op("dve", lambda e: e.tensor_tensor(out=bb3, in0=bb3, in1=bmid[:].unsqueeze(2).to_broadcast([128, 32, 64]), op=ALU.subtract),
                         reads=[bbb, eb], writes=[bbb])
                    P.op("act", lambda e: e.activation(out=e123[:, 1, :], in_=bb3[:, :, 63], func=AF.Exp), reads=[bbb], writes=[eb])
                    for t in range(4):
                        sl = slice(t * 512, (t + 1) * 512)
                        P.op("act", lambda e: e.activation(out=tA[0][:], in_=bb[:, sl], func=AF.Exp), reads=[bbb], writes=[tAb[0]])
                        P.op("dve", lambda e: e.tensor_tensor(out=qt[:, sl], in0=qt[:, sl], in1=tA[0][:], op=ALU.mult), reads=[qtb, tAb[0]], writes=[qtb])
                        P.op("act", lambda e: e.activation(out=tA[1][:], in_=bb[:, sl], func=AF.Exp, scale=-1.0), reads=[bbb], writes=[tAb[1]])
                        P.op("dve", lambda e: e.tensor_tensor(out=kt[:, sl], in0=kt[:, sl], in1=tA[1][:], op=ALU.mult), reads=[ktb, tAb[1]], writes=[ktb])
                for t4 in range(4):
                    pm = psum()
                    pmb = bf(pm)
                    for i in range(4):
                        tt = t4 * 4 + i
                        P.op("pe", lambda e, i=i, tt=tt: e.transpose(pmb[:, i * 128:(i + 1) * 128], kt[:, tt * 128:(tt + 1) * 128], identb_t[:]),
                             reads=[ktb, cst], writes=[pm], inc=(i == 3))
                    P.op("dve", lambda e, t4=t4: e.tensor_copy(out=ktok[:, t4 * 4:(t4 + 1) * 4, :],
                                                               in_=pmb[:, 0:512].rearrange("p (a b) -> p a b", b=128)),
                         reads=[pm], writes=[ktokb])
                with ExitStack() as pb:
                    kvs = sb("hg_kvs", [128, 64, 32], F32, pb)
                    e3bc = sb("hg_e3bc", [128, 64, 32], F32, pb)
                    smid = sb("hg_smid", [128, 32, 64], BF16, pb)
                    scm = [sb(f"hg_scm{i}", [128, 2, 128], BF16, pb) for i in range(2)]
                    sctmp = sb("hg_sctmp", [128, 2, 32], BF16, pb)
                    sctb = P.buf("sctmp")
                    osb = sb("hg_osb", [128, 512], F32, pb)
                    sgl = sb("hg_sgl", [128, 512], F32, pb)
                    oA = [sb(f"hg_oA{i}", [128, 512], BF16, pb) for i in range(2)]
                    kvsb, e3b, smb, scb, osbb, sglb = P.buf("kvs"), P.buf("e3bc"), P.buf("smid"), [P.buf("scm0"), P.buf("scm1")], P.buf("osb"), P.buf("sgl")
                    oAb = [P.buf("oA0"), P.buf("oA1")]
                    st = rms_state(pb, "hg", 1, ones64b_t)
                    for q_ in range(2):
                        P.op("dve", lambda e, q_=q_: e.memset(scm[q_][:], 0.0), writes=[scb[q_]])
                    for c0 in range(0, 32, 8):
                        pmh = [psum(), psum()]
                        for half in range(2):
                            pm = pmh[half]
                            for idx in range(4):
                                c = c0 + 2 * idx + half
                                tt = c // 2
                                for part in range(2):
                                    last = (idx == 3 and part == 1)
                                    P.op("pe", lambda e, pm=pm, idx=idx, tt=tt, half=half, part=part: e.matmul(
                                        pm.ap[part * 64:(part + 1) * 64, idx * 64:(idx + 1) * 64],
                                        lhsT=ktok[half * 64:(half + 1) * 64, tt, part * 64:(part + 1) * 64],
                                        rhs=vtok[half * 64:(half + 1) * 64, tt, part * 64:(part + 1) * 64], start=True, stop=True),
                                        reads=[ktokb, vtokb], writes=[pm], inc=last)
                            P.op("dve", lambda e, pm=pm, c0=c0, half=half: e.tensor_tensor(
                                out=kvs[:, :, c0 + half:c0 + 8:2].rearrange("p v c -> p c v"), in0=pm.ap[:, 0:256].rearrange("p (c v) -> p c v", v=64),
                                in1=e123[:, 1, c0 + half:c0 + 8:2].unsqueeze(2).to_broadcast([128, 4, 64]), op=ALU.mult),
                                reads=[pm, eb], writes=[kvsb])
                    P.op("dve", lambda e: e.memset(e3bc[:, :, 0:1], 0.0), writes=[e3b])
                    P.op("dve", lambda e: e.tensor_copy(out=e3bc[:, :, 1:32], in_=e123[:, 2, 1:32].unsqueeze(1).to_broadcast([128, 64, 31])),
                         reads=[eb], writes=[e3b])
                    P.op("dve", lambda e: e.tensor_tensor_scan(out=kvs[:].rearrange("p v c -> p (v c)"), data0=e3bc[:].rearrange("p v c -> p (v c)"),
                                                               data1=kvs[:].rearrange("p v c -> p (v c)"), initial=0.0, op0=ALU.mult, op1=ALU.add),
                         reads=[kvsb, e3b], writes=[kvsb])
                    P.op("dve", lambda e: e.memset(smid[:, 0, :], 0.0), writes=[smb])
                    P.op("dve", lambda e: e.tensor_tensor(out=smid[:, 1:32, :], in0=kvs[:, :, 0:31].rearrange("p v c -> p c v"),
                                                          in1=e123[:, 0, 1:32].unsqueeze(2).to_broadcast([128, 31, 64]), op=ALU.mult),
                         reads=[kvsb, eb], writes=[smb])
                    r2, wo = load_wout(l, slot * 128, 1)
                    for t in range(4):
                        pos_ = [psum(hold=True), psum(hold=True)]
                        for i in range(4):
                            tt = t * 4 + i
                            q = tt % 2
                            tsl = slice(tt * 128, (tt + 1) * 128)
                            pscs = [psum(), psum()]
                            for cc in range(2):
                                c = 2 * tt + cc
                                R = slice(cc * 64, (cc + 1) * 64)
                                for part in range(2):
                                    pr = slice(part * 64, (part + 1) * 64)
                                    psc = pscs[part]
                                    P.op("pe", lambda e, psc=psc, c=c, R=R, pr=pr: e.matmul(
                                        psc.ap[R, 0:32], lhsT=kB[pr, c * 64:(c + 1) * 64], rhs=qB[pr, c * 64:c * 64 + 32],
                                        start=True, stop=True), reads=[kBb, qBb], writes=[psc], inc=False)
                                    P.op("pe", lambda e, psc=psc, c=c, R=R, pr=pr: e.matmul(
                                        psc.ap[R, 32:64], lhsT=kB[pr, c * 64:(c + 1) * 64], rhs=qB[pr, c * 64 + 32:c * 64 + 64],
                                        start=True, stop=True), reads=[kBb, qBb], writes=[psc], inc=False)
                                    P.op("pe", lambda e, psc=psc, c=c, R=R, pr=pr: e.matmul(
                                        psc.ap[R, 64:96], lhsT=kt[pr, c * 64:(c + 1) * 64], rhs=qt[pr, c * 64 + 32:c * 64 + 64],
                                        start=True, stop=True), reads=[ktb, qtb], writes=[psc], inc=True)
                            for cc in range(2):
                                R = slice(cc * 64, (cc + 1) * 64)
                                c0_ = cc * 64
                                for part in range(2):
                                    psc = pscs[part]
                                    P.op("dve", lambda e, q=q, R=R, psc=psc, c0_=c0_, part=part: e.tensor_tensor(
                                        out=scm[q][R, part, c0_:c0_ + 32], in0=psc.ap[R, 0:32], in1=bdmask_t[R, c0_:c0_ + 32], op=ALU.mult),
                                        reads=[psc, cst], writes=[scb[q]])
                                    P.op("dve", lambda e, q=q, R=R, psc=psc, c0_=c0_, part=part: e.tensor_tensor(
                                        out=scm[q][R, part, c0_ + 32:c0_ + 64], in0=psc.ap[R, 32:64], in1=maskD2_t[R, c0_ + 32:c0_ + 64], op=ALU.mult),
                                        reads=[psc, cst], writes=[scb[q]])
                                    P.op("dve", lambda e, R=R, psc=psc, c0_=c0_, part=part: e.tensor_tensor(
                                        out=sctmp[R, part, :], in0=psc.ap[R, 64:96], in1=maskX_t[R, c0_ + 32:c0_ + 64], op=ALU.mult),
                                        reads=[psc, cst], writes=[sctb])
                                    P.op("dve", lambda e, q=q, R=R, c0_=c0_, part=part: e.tensor_tensor(
                                        out=scm[q][R, part, c0_ + 32:c0_ + 64], in0=scm[q][R, part, c0_ + 32:c0_ + 64], in1=sctmp[R, part, :], op=ALU.add),
                                        reads=[scb[q], sctb], writes=[scb[q]])
                            for part in range(2):
                                po = pos_[part]
                                P.op("pe", lambda e, po=po, part=part, i=i, tt=tt, q=q: e.matmul(
                                    po.ap[part * 64:(part + 1) * 64, i * 128:(i + 1) * 128], lhsT=vtok[:, tt, part * 64:(part + 1) * 64],
                                    rhs=scm[q][:, part, :], start=True, stop=False),
                                    reads=[vtokb, scb[q]], writes=[po], inc=False)
                                for cc in range(2):
                                    c = 2 * tt + cc
                                    P.op("pe", lambda e, po=po, part=part, i=i, cc=cc, c=c: e.matmul(
                                        po.ap[part * 64:(part + 1) * 64, i * 128 + cc * 64:i * 128 + (cc + 1) * 64],
                                        lhsT=smid[part * 64:(part + 1) * 64, c, :], rhs=qt[part * 64:(part + 1) * 64, c * 64:(c + 1) * 64],
                                        start=False, stop=(cc == 1)),
                                        reads=[smb, qtb], writes=[po], inc=(cc == 1))
                        for part in range(2):
                            pr = slice(part * 64, (part + 1) * 64)
                            P.op("act", lambda e, part=part, pr=pr: e.activation(out=osb[pr, :], in_=pos_[part].ap[pr, :], func=AF.Copy),
                                 reads=[pos_[part]], writes=[osbb])
                            psum_release(pos_[part])
                        ov = V(osb[:], osbb)
                        rms_run(st, [ov], [ov], lambda c: hgng_t[:, l, slot:slot + 1], None)
                        pg = proj_fm(wv[f"HG{slot}"], r, t)
                        P.op("act", lambda e: e.activation(out=sgl[:], in_=pg.ap, func=AF.Silu), reads=[pg], writes=[sglb])
                        oq = t % 2
                        P.op("dve", lambda e, oq=oq: e.tensor_tensor(out=oA[oq][:], in0=osb[:], in1=sgl[:], op=ALU.mult),
                             reads=[osbb, sglb], writes=[oAb[oq]])
                        add_wout(l, s, r2, wo, 1, lambda jj, oq=oq: oA[oq][:], [oAb[oq]], t * 512, 512)

        def mixer_dsa(l, s):
            with ExitStack() as ph:
                kT2 = sb("ds_kT2", [128, S], BF16, ph)
                ikT2 = sb("ds_ikT2", [128, S], BF16, ph)
                vaug = sb("ds_vaug", [128, NT, 65], BF16, ph)
                iwt = sb("ds_iwt", [128, NT, 8], F32, ph)
                qT = sb("ds_qT", [128, 2, S], BF16, ph)
                iqT = sb("ds_iqT", [128, 4, S], BF16, ph)
                kTb, ikTb, vab, iwb, qTb, iqTb = (P.buf("kT2"), P.buf("ikT2"), P.buf("vaug"), P.buf("iwt"), P.buf("qT"), P.buf("iqT"))
                r1, w1 = load_wset(l, ["AQ0", "AQW0", "AQ1", "AQW1", "AK2", "AKW2"])
                r2, w2 = load_wset(l, ["IQ0", "IQW0", "IQ1", "IQW1", "IQ2", "IQW2"])
                r3, w3 = load_wset(l, ["IQ3", "IQW3", "IK2", "IKW2", "AVIW"])
                with ExitStack() as pa:
                    cosT = sb("ds_cos", [128, S], BF16, pa)
                    sinT = sb("ds_sin", [128, S], BF16, pa)
                    tbl = P.buf("ropetab")
                    P.dma("pool", cosT[:], cd["cosT"], misc_sem, writes=[tbl])
                    P.dma("pool", sinT[:], cd["sinT"], misc_sem, writes=[tbl])
                    t1 = sb("ds_t1", [128, 512], F32, pa)
                    t2 = sb("ds_t2", [128, 512], F32, pa)
                    t1b, t2b = P.buf("t1"), P.buf("t2")
                    x1 = sb("ds_x1", [128, 512], F32, pa)
                    x2 = sb("ds_x2", [128, 512], F32, pa)
                    xq = sb("ds_xq", [128, 512], BF16, pa)
                    rsn = sb("ds_rsn", [128, 512], F32, pa)
                    x1b, x2b, xqb, rsnb = P.buf("x1"), P.buf("x2"), P.buf("xq"), P.buf("rsn")

                    def rope_comb(dst_ap, dstbuf, a_v, b_v, t):
                        sl = slice(t * 512, (t + 1) * 512)
                        P.op("dve", lambda e: e.tensor_tensor(out=t1[:], in0=a_v.ap, in1=cosT[:, sl], op=ALU.mult), reads=[a_v, tbl], writes=[t1b])
                        P.op("dve", lambda e: e.tensor_tensor(out=t2[:], in0=b_v.ap, in1=sinT[:, sl], op=ALU.mult), reads=[b_v, tbl], writes=[t2b])
                        P.op("dve", lambda e: e.tensor_tensor(out=dst_ap, in0=t1[:], in1=t2[:], op=ALU.add), reads=[t1b, t2b], writes=[dstbuf])

                    for t in range(4):
                        sl = slice(t * 512, (t + 1) * 512)
                        for sq_ in range(2):
                            pa_ = proj_fm(w1[f"AQ{sq_}"], r1, t)
                            pb_ = proj_fm(w1[f"AQW{sq_}"], r1, t)
                            rope_comb(qT[:, sq_, sl], qTb, pa_, pb_, t)
                        pa_ = proj_fm(w1["AK2"], r1, t)
                        pb_ = proj_fm(w1["AKW2"], r1, t)
                        rope_comb(kT2[:, sl], kTb, pa_, pb_, t)
                        for sq_ in range(4):
                            rr, ww = (r2, w2) if sq_ < 3 else (r3, w3)
                            pa_ = proj_fm(ww[f"IQ{sq_}"], rr, t)
                            pb_ = proj_fm(ww[f"IQW{sq_}"], rr, t)
                            rope_comb(iqT[:, sq_, sl], iqTb, pa_, pb_, t)
                        pa_ = proj_fm(w3["IK2"], r3, t)
                        pb_ = proj_fm(w3["IKW2"], r3, t)
                        P.op("act", lambda e: e.activation(out=x1[:], in_=pa_.ap, func=AF.Copy), reads=[pa_], writes=[x1b])
                        P.op("act", lambda e: e.activation(out=x2[:], in_=pb_.ap, func=AF.Copy), reads=[pb_], writes=[x2b])
                        P.op("act", lambda e: e.activation(out=xq[:], in_=x1[:], func=AF.Copy), reads=[x1b], writes=[xqb])
                        pmn = psum()
                        P.op("pe", lambda e: e.matmul(pmn.ap, lhsT=ones64b_t[:], rhs=xq[:], start=True, stop=True), reads=[xqb, cst], writes=[pmn])
                        P.op("dve", lambda e: e.tensor_tensor(out=x1[:], in0=x1[:], in1=pmn.ap, op=ALU.subtract), reads=[x1b, pmn], writes=[x1b])
                        P.op("dve", lambda e: e.tensor_tensor(out=x2[:], in0=x2[:], in1=pmn.ap, op=ALU.subtract), reads=[x2b, pmn], writes=[x2b])
                        P.op("act", lambda e: e.activation(out=xq[:], in_=x1[:], func=AF.Square), reads=[x1b], writes=[xqb])
                        pvr = psum()
                        P.op("pe", lambda e: e.matmul(pvr.ap, lhsT=ones64b_t[:], rhs=xq[:], start=True, stop=True), reads=[xqb, cst], writes=[pvr])
                        P.op("act", lambda e: e.activation(out=rsn[:], in_=pvr.ap, func=AF.Sqrt, bias=eps_t[:], scale=1.0), reads=[pvr, cst], writes=[rsnb])
                        P.op("dve", lambda e: e.reciprocal(out=rsn[:], in_=rsn[:]), reads=[rsnb], writes=[rsnb])
                        P.op("dve", lambda e: e.scalar_tensor_tensor(out=x1[:], in0=x1[:], scalar=kn_t[:, l, 0:1], in1=rsn[:], op0=ALU.mult, op1=ALU.mult),
                             reads=[x1b, rsnb, cst], writes=[x1b])
                        P.op("act", lambda e: e.activation(out=x1[:], in_=x1[:], func=AF.Identity, bias=kn_t[:, l, 1:2], scale=1.0), reads=[x1b, cst], writes=[x1b])
                        P.op("dve", lambda e: e.scalar_tensor_tensor(out=x2[:], in0=x2[:], scalar=kn_t[:, l, 2:3], in1=rsn[:], op0=ALU.mult, op1=ALU.mult),
                             reads=[x2b, rsnb, cst], writes=[x2b])
                        P.op("act", lambda e: e.activation(out=x2[:], in_=x2[:], func=AF.Identity, bias=kn_t[:, l, 3:4], scale=1.0), reads=[x2b, cst], writes=[x2b])
                        rope_comb(ikT2[:, sl], ikTb, V(x1[:], x1b), V(x2[:], x2b), t)
                    P.op("dve", lambda e: e.memset(vaug[:, :, 64:65], 1.0), writes=[vab])
                    for tt in range(NT):
                        pv = proj_tm(w3["AVIW"], ring_b[r3], tt, 72)
                        P.op("act", lambda e, tt=tt: e.activation(out=vaug[:, tt, 0:64], in_=pv.ap[:, 0:64], func=AF.Copy), reads=[pv], writes=[vab])
                        P.op("act", lambda e, tt=tt: e.activation(out=iwt[:, tt, :], in_=pv.ap[:, 64:72], func=AF.Copy, scale=IWSCALE),
                             reads=[pv], writes=[iwb])
                with ExitStack() as pb:
                    acc = sb("ds_acc", [128, S], F32, pb)
                    mask = sb("ds_mask", [128, S], BF16, pb)
                    rl = [sb(f"ds_rl{i}", [128, 512], F32, pb) for i in range(2)]
                    PT = [sb(f"ds_PT{i}", [128, 4, 128], BF16, pb) for i in range(2)]
                    otok = sb("ds_otok", [128, 4, 64], BF16, pb)
                    oBt = sb("ds_oBt", [128, 2, 128], BF16, pb)
                    sm_ = sb("ds_small", [128, 8], F32, pb)
                    HWt = sb("ds_HW", [128, NIT + 1], F32, pb)
                    HW2 = sb("ds_HW2", [128, NIT + 1], F32, pb)
                    rden = sb("ds_rden", [128, 4], F32, pb)
                    accb, maskb, rlb, PTb, otb, oBb, smb_, rdb = (P.buf("acc"), P.buf("mask"), [P.buf("rl0"), P.buf("rl1")],
                                                                 [P.buf("PT0"), P.buf("PT1")], P.buf("otok"), P.buf("oBt"), P.buf("small"), P.buf("rden"))
                    r4, wo = load_wout(l, 256, 2)
                    rlq = [0]
                    ptq = [0]
                    for j in range(NT):
                        W = 128 * (j + 1)
                        qsl = slice(j * 128, (j + 1) * 128)
                        nkc = (W + 511) // 512
                        for kc in range(nkc):
                            Wc = min(512, W - kc * 512)
                            ksl = slice(kc * 512, kc * 512 + Wc)
                            for h in range(8):
                                part, slot = h % 2, h // 2
                                pl = psum()
                                P.op("pe", lambda e, part=part, slot=slot: e.matmul(
                                    pl.ap[:, 0:Wc], lhsT=iqT[part * 64:(part + 1) * 64, slot, qsl], rhs=ikT2[part * 64:(part + 1) * 64, ksl],
                                    start=True, stop=True), reads=[iqTb, ikTb], writes=[pl])
                                q = rlq[0] % 2
                                rlq[0] += 1
                                P.op("act", lambda e, q=q: e.activation(out=rl[q][:, 0:Wc], in_=pl.ap[:, 0:Wc], func=AF.Relu), reads=[pl], writes=[rlb[q]])
                                if h == 0:
                                    P.op("dve", lambda e, q=q: e.tensor_scalar(out=acc[:, ksl], in0=rl[q][:, 0:Wc], scalar1=iwt[:, j, 0:1], scalar2=None, op0=ALU.mult),
                                         reads=[rlb[q], iwb], writes=[accb])
                                else:
                                    P.op("dve", lambda e, q=q, h=h: e.scalar_tensor_tensor(
                                        out=acc[:, ksl], in0=rl[q][:, 0:Wc], scalar=iwt[:, j, h:h + 1], in1=acc[:, ksl], op0=ALU.mult, op1=ALU.add),
                                        reads=[rlb[q], iwb, accb], writes=[accb])
                        P.op("dve", lambda e: e.tensor_tensor(out=acc[:, qsl], in0=acc[:, qsl], in1=causb_t[:], op=ALU.add), reads=[accb, cst], writes=[accb])
                        if j >= 2:
                            P.op("dve", lambda e: e.tensor_reduce(out=sm_[:, 0:1], in_=acc[:, 0:W - 128], axis=AX.X, op=ALU.min), reads=[accb], writes=[smb_])
                            P.op("dve", lambda e: e.tensor_reduce(out=sm_[:, 1:2], in_=acc[:, 0:W], axis=AX.X, op=ALU.max), reads=[accb, smb_], writes=[smb_])
                            P.op("dve", lambda e: e.tensor_tensor(out=sm_[:, 2:3], in0=sm_[:, 1:2], in1=sm_[:, 0:1], op=ALU.subtract), reads=[smb_], writes=[smb_])
                            P.op("dve", lambda e: e.tensor_scalar(out=HWt[:], in0=pow2_t[:], scalar1=sm_[:, 2:3], scalar2=None, op0=ALU.mult), reads=[smb_, cst], writes=[smb_])
                            P.op("dve", lambda e: e.tensor_scalar(out=HW2[:], in0=HWt[:], scalar1=2.0, scalar2=None, op0=ALU.mult), reads=[smb_], writes=[smb_])
                            P.op("dve", lambda e: e.tensor_tensor(out=sm_[:, 3:4], in0=sm_[:, 0:1], in1=HWt[:, 0:1], op=ALU.add), reads=[smb_], writes=[smb_])
                            for n in range(NIT):
                                P.op("dve", lambda e: e.tensor_scalar(out=mask[:, 0:W], in0=acc[:, 0:W], scalar1=sm_[:, 3:4], scalar2=None,
                                                                      op0=ALU.is_ge, op1=ALU.add, accum_out=sm_[:, 4:5]),
                                     reads=[accb, smb_], writes=[maskb, smb_])
                                P.op("dve", lambda e, n=n: e.tensor_scalar(out=sm_[:, 5:6], in0=sm_[:, 4:5], scalar1=255.5, scalar2=HW2[:, n + 1:n + 2],
                                                                           op0=ALU.is_ge, op1=ALU.mult), reads=[smb_], writes=[smb_])
                                P.op("dve", lambda e, n=n: e.scalar_tensor_tensor(out=sm_[:, 3:4], in0=sm_[:, 5:6], scalar=HWt[:, n + 1:n + 2], in1=sm_[:, 3:4],
                                                                                  op0=ALU.subtract, op1=ALU.add), reads=[smb_], writes=[smb_])
                            P.op("dve", lambda e: e.tensor_tensor(out=sm_[:, 6:7], in0=sm_[:, 3:4], in1=HWt[:, NIT:NIT + 1], op=ALU.subtract), reads=[smb_], writes=[smb_])
                            thr = sm_[:, 6:7]
                        else:
                            thr = negthr_t[:]
                        P.op("dve", lambda e: e.tensor_scalar(out=mask[:, 0:W], in0=acc[:, 0:W], scalar1=thr, scalar2=None, op0=ALU.is_ge),
                             reads=[accb, smb_, cst], writes=[maskb])
                        po = psum(hold=True)
                        po3 = po.ap[:, 0:260].rearrange("p (h e) -> p h e", e=65)
                        first = True
                        for kb in range(j + 1):
                            kbs = slice(kb * 128, (kb + 1) * 128)
                            pmT = psum()
                            pmTb = bf(pmT)
                            P.op("pe", lambda e: e.transpose(pmTb[:, 0:128], mask[:, kbs], identb_t[:]), reads=[maskb, cst], writes=[pmT])
                            psts = [psum(), psum()]
                            q = ptq[0] % 2
                            ptq[0] += 1
                            for part in range(2):
                                for slot in range(2):
                                    P.op("pe", lambda e, part=part, slot=slot: e.matmul(
                                        psts[part].ap[:, slot * 128:(slot + 1) * 128], lhsT=kT2[part * 64:(part + 1) * 64, kbs],
                                        rhs=qT[part * 64:(part + 1) * 64, slot, qsl], start=True, stop=True),
                                        reads=[kTb, qTb], writes=[psts[part]], inc=(slot == 1))
                                P.op("act", lambda e, q=q, part=part: e.activation(
                                    out=PT[q][:, part:4:2, :], in_=psts[part].ap[:, 0:256].rearrange("p (h t) -> p h t", t=128), func=AF.Exp, scale=0.125),
                                    reads=[psts[part]], writes=[PTb[q]])
                            P.op("dve", lambda e, q=q: e.tensor_tensor(out=PT[q][:], in0=PT[q][:], in1=pmTb[:, 0:128].unsqueeze(1).to_broadcast([128, 4, 128]), op=ALU.mult),
                                 reads=[PTb[q], pmT], writes=[PTb[q]])
                            for h in range(4):
                                lastmm = (kb == j and h == 3)
                                P.op("pe", lambda e, h=h, q=q, first=first, lastmm=lastmm: e.matmul(
                                    po3[:, h, :], lhsT=PT[q][:, h, :], rhs=vaug[:, kb, :], start=first, stop=lastmm, skip_group_check=True),
                                    reads=[PTb[q], vab], writes=[po], inc=(h == 3))
                                first = False
                        P.op("dve", lambda e: e.reciprocal(out=rden[:], in_=po3[:, :, 64]), reads=[po], writes=[rdb])
                        P.op("dve", lambda e: e.tensor_tensor(out=otok[:], in0=po3[:, :, 0:64], in1=rden[:].unsqueeze(2).to_broadcast([128, 4, 64]), op=ALU.mult),
                             reads=[po, rdb], writes=[otb])
                        psum_release(po)
                        pmo = psum()
                        pmob = bf(pmo)
                        of = otok[:].rearrange("p h d -> p (h d)")
                        for sl_ in range(2):
                            P.op("pe", lambda e, sl_=sl_: e.transpose(pmob[:, sl_ * 128:(sl_ + 1) * 128], of[:, sl_ * 128:(sl_ + 1) * 128], identb_t[:]),
                                 reads=[otb, cst], writes=[pmo], inc=(sl_ == 1))
                        P.op("dve", lambda e: e.tensor_copy(out=oBt[:], in_=pmob[:, 0:256].rearrange("p (a b) -> p a b", b=128)),
                             reads=[pmo], writes=[oBb])
                        add_wout(l, s, r4, wo, 2, lambda jj: oBt[:, jj, :], [oBb], j * 128, 128)

        def mixer_ssd(l, s):
            with ExitStack() as ph:
                wsdt = sb("ss_wsdt", [128, 8, 8], BF16, ph)
                dtb = sb("ss_dt", [128, NT, 8], F32, ph)
                dA = sb("ss_dA", [128, NT, 8], F32, ph)
                wsb, dtbb = P.buf("wsdt"), P.buf("dt")
                P.dma("pool", wsdt[:], win2_d[l][:, W2OFF["SDT"]:W2OFF["SDT"] + 8].rearrange("(k p) c -> p k c", p=128), misc_sem, writes=[wsb])
                for tt in range(NT):
                    pm = proj_tm(wsdt[:], wsb, tt, 8)
                    P.op("dve", lambda e, tt=tt: e.tensor_tensor(out=dtb[:, tt, :], in0=pm.ap[:, 0:8], in1=dtb_t[:, l, :], op=ALU.add),
                         reads=[pm, cst], writes=[dtbb])
                P.op("act", lambda e: e.activation(out=dtb[:], in_=dtb[:], func=AF.Exp), reads=[dtbb], writes=[dtbb])
                P.op("act", lambda e: e.activation(out=dtb[:], in_=dtb[:], func=AF.Ln, bias=one_t[:], scale=1.0), reads=[dtbb, cst], writes=[dtbb])
                P.op("dve", lambda e: e.tensor_tensor(out=dA[:], in0=dtb[:], in1=nega_t[:, l, :].unsqueeze(1).to_broadcast([128, NT, 8]), op=ALU.mult),
                     reads=[dtbb, cst], writes=[dtbb])
                for g in range(2):
                    with ExitStack() as pg_:
                        zs = sb("ss_zs", [128, 2, S], BF16, pg_)
                        xsT = sb("ss_xsT", [128, 2, S], BF16, pg_)
                        BT = sb("ss_BT", [128, S], BF16, pg_)
                        CT = sb("ss_CT", [128, S], BF16, pg_)
                        yz = sb("ss_yz", [128, 2, S], BF16, pg_)
                        zsb, xsb, BTb, CTb = P.buf("zs"), P.buf("xsT"), P.buf("BT"), P.buf("CT")
                        yzb = [[P.buf(f"yz{i}{t}") for t in range(4)] for i in range(2)]
                        r, wv = load_wset(l, [f"Z{g}0", f"Z{g}1", f"XS{g}0", f"XS{g}1", f"B{g}", f"C{g}"])
                        with ExitStack() as pa:
                            pre = sb("ss_pre", [128, 3 + S], F32, pa)
                            cac = sb("ss_cac", [128, S], F32, pa)
                            preb, cacb = P.buf("pre"), P.buf("cac")
                            for i in range(2):
                                for t in range(4):
                                    pm = proj_fm(wv[f"Z{g}{i}"], r, t)
                                    P.op("act", lambda e, i=i, t=t: e.activation(out=zs[:, i, t * 512:(t + 1) * 512], in_=pm.ap, func=AF.Silu),
                                         reads=[pm], writes=[zsb])
                            blocks = [(f"XS{g}0", 2 * g, xsT[:, 0, :], xsb), (f"XS{g}1", 2 * g + 1, xsT[:, 1, :], xsb),
                                      (f"B{g}", 4 + g, BT[:], BTb), (f"C{g}", 6 + g, CT[:], CTb)]
                            for nm, ch, dst, dstb in blocks:
                                P.op("dve", lambda e: e.memset(pre[:, 0:3], 0.0), writes=[preb])
                                for t in range(4):
                                    pm = proj_fm(wv[nm], r, t)
                                    P.op("act", lambda e, t=t: e.activation(out=pre[:, 3 + t * 512:3 + (t + 1) * 512], in_=pm.ap, func=AF.Copy),
                                         reads=[pm], writes=[preb])
                                P.op("dve", lambda e, ch=ch: e.tensor_scalar(out=cac[:], in0=pre[:, 3:3 + S], scalar1=cw_t[:, l, ch, 3:4], scalar2=cb_t[:, l, ch:ch + 1],
                                                                             op0=ALU.mult, op1=ALU.add), reads=[preb, cst], writes=[cacb])
                                for jt in range(3):
                                    P.op("dve", lambda e, ch=ch, jt=jt: e.scalar_tensor_tensor(out=cac[:], in0=pre[:, jt:jt + S], scalar=cw_t[:, l, ch, jt:jt + 1], in1=cac[:],
                                                                                               op0=ALU.mult, op1=ALU.add), reads=[preb, cacb, cst], writes=[cacb])
                                P.op("act", lambda e, dst=dst: e.activation(out=dst, in_=cac[:], func=AF.Silu), reads=[cacb], writes=[dstb])
                        with ExitStack() as pb:
                            def two(name, shape, dt_):
                                return [sb(f"{name}{q_}", shape, dt_, pb) for q_ in range(2)], [P.buf(f"{name}{q_}") for q_ in range(2)]
                            rhsU2, rhsUb2 = two("ss_rhsU", [128, 4, 128], F32)
                            Eseg2, Esegb2 = two("ss_Eseg", [128, 4, 128], BF16)
                            Ebc2, Ebcb2 = two("ss_Ebc", [128, 4, 128], BF16)
                            smx2, smxb2 = two("ss_smx", [128, 8], F32)
                            CBm2, CBmb2 = two("ss_CBm", [128, 128], BF16)
                            MT2, MTb2 = two("ss_MT", [128, 4, 128], BF16)
                            CsT2, CsTb2 = two("ss_CsT", [128, 4, 128], BF16)
                            xdt2, xdtb2 = two("ss_xdt", [128, 4, 64], BF16)
                            xdtd2, xdtdb2 = two("ss_xdtd", [128, 4, 64], BF16)
                            Btok2, Btokb2 = two("ss_Btok", [128, 128], BF16)
                            ytmp2, ytmpb2 = two("ss_ytmp", [128, 2, 128], F32)
                            prevb162, prevbb2 = two("ss_prevb", [128, 4, 64], BF16)
                            prev = sb("ss_prev", [128, 4, 64], F32, pb)
                            prevb = P.buf("prev")
                            st = rms_state(pb, "ss", 2, ones256_t)
                            P.op("dve", lambda e: e.memset(prev[:], 0.0), writes=[prevb])
                            for c in range(NT):
                                csl = slice(c * 128, (c + 1) * 128)
                                dAc = dA[:, c, 4 * g:4 * g + 4]
                                q_ = c % 2
                                rhsU, rhsUb, Eseg, Esegb, Ebc, Ebcb, smx, smxb = rhsU2[q_], rhsUb2[q_], Eseg2[q_], Esegb2[q_], Ebc2[q_], Ebcb2[q_], smx2[q_], smxb2[q_]
                                CBm, CBmb, MT, MTb, CsT, CsTb = CBm2[q_], CBmb2[q_], MT2[q_], MTb2[q_], CsT2[q_], CsTb2[q_]
                                xdt, xdtb, xdtd, xdtdb, Btok, Btokb, ytmp, ytmpb = xdt2[q_], xdtb2[q_], xdtd2[q_], xdtdb2[q_], Btok2[q_], Btokb2[q_], ytmp2[q_], ytmpb2[q_]
                                prevb16, prevbb = prevb162[q_], prevbb2[q_]
                                prevb16n, prevbbn = prevb162[1 - q_], prevbb2[1 - q_]
                                P.op("dve", lambda e: e.tensor_tensor(out=rhsU[:], in0=Uincl_t[:].unsqueeze(1).to_broadcast([128, 4, 128]),
                                                                      in1=dAc.unsqueeze(2).to_broadcast([128, 4, 128]), op=ALU.mult),
                                     reads=[cst, dtbb], writes=[rhsUb])
                                rf = rhsU[:].rearrange("p a b -> p (a b)")
                                pseg = psum()
                                P.op("pe", lambda e: e.matmul(pseg.ap, lhsT=Lgt_t[:], rhs=rf, start=True, stop=True), reads=[rhsUb, cst], writes=[pseg])
                                pacs = psum()
                                P.op("pe", lambda e: e.matmul(pacs.ap, lhsT=onesf_t[:], rhs=rf, start=True, stop=True), reads=[rhsUb, cst], writes=[pacs])
                                psm = psum()
                                P.op("pe", lambda e: e.matmul(psm.ap[:, 0:4], lhsT=Lgt_t[:], rhs=dAc, start=True, stop=True), reads=[dtbb, cst], writes=[psm], inc=False)
                                P.op("pe", lambda e: e.matmul(psm.ap[:, 4:8], lhsT=onesf_t[:], rhs=dAc, start=True, stop=True), reads=[dtbb, cst], writes=[psm])
                                P.op("act", lambda e: e.activation(out=Eseg[:], in_=pseg.ap.rearrange("p (a b) -> p a b", b=128), func=AF.Exp), reads=[pseg], writes=[Esegb])
                                P.op("act", lambda e: e.activation(out=Ebc[:], in_=pacs.ap.rearrange("p (a b) -> p a b", b=128), func=AF.Exp), reads=[pacs], writes=[Ebcb])
                                P.op("act", lambda e: e.activation(out=smx[:], in_=psm.ap[:, 0:8], func=AF.Exp), reads=[psm], writes=[smxb])
                                pcb = psum()
                                P.op("pe", lambda e: e.matmul(pcb.ap[:, 0:128], lhsT=BT[:, csl], rhs=CT[:, csl], start=True, stop=True), reads=[BTb, CTb], writes=[pcb])
                                P.op("dve", lambda e: e.tensor_tensor(out=CBm[:], in0=pcb.ap[:, 0:128], in1=causT_t[:], op=ALU.mult), reads=[pcb, cst], writes=[CBmb])
                                P.op("dve", lambda e: e.tensor_tensor(out=MT[:], in0=Eseg[:], in1=CBm[:].unsqueeze(1).to_broadcast([128, 4, 128]), op=ALU.mult),
                                     reads=[Esegb, CBmb], writes=[MTb])
                                P.op("dve", lambda e: e.tensor_tensor(out=CsT[:], in0=Ebc[:], in1=CT[:, csl].unsqueeze(1).to_broadcast([128, 4, 128]), op=ALU.mult),
                                     reads=[Ebcb, CTb], writes=[CsTb])
                                pxt = psum()
                                pxtb = bf(pxt)
                                for i in range(2):
                                    P.op("pe", lambda e, i=i: e.transpose(pxtb[:, i * 128:(i + 1) * 128], xsT[:, i, csl], identb_t[:]), reads=[xsb, cst], writes=[pxt], inc=False)
                                P.op("pe", lambda e: e.transpose(pxtb[:, 256:384], BT[:, csl], identb_t[:]), reads=[BTb, cst], writes=[pxt])
                                P.op("dve", lambda e: e.tensor_tensor(out=xdt[:], in0=pxtb[:, 0:256].rearrange("p (a b) -> p a b", b=64),
                                                                      in1=dtb[:, c, 4 * g:4 * g + 4].unsqueeze(2).to_broadcast([128, 4, 64]), op=ALU.mult),
                                     reads=[pxt, dtbb], writes=[xdtb])
                                P.op("dve", lambda e: e.tensor_tensor(out=xdtd[:], in0=xdt[:], in1=smx[:, 0:4].unsqueeze(2).to_broadcast([128, 4, 64]), op=ALU.mult),
                                     reads=[xdtb, smxb], writes=[xdtdb])
                                P.op("dve", lambda e: e.tensor_copy(out=Btok[:], in_=pxtb[:, 256:384]), reads=[pxt], writes=[Btokb])
                                py = psum()
                                for rr in range(4):
                                    i, part = rr // 2, rr % 2
                                    P.op("pe", lambda e, rr=rr, i=i, part=part: e.matmul(
                                        py.ap[part * 64:(part + 1) * 64, i * 128:(i + 1) * 128], lhsT=xdt[:, rr, :], rhs=MT[:, rr, :], start=True, stop=(c == 0)),
                                        reads=[xdtb, MTb], writes=[py], inc=(c == 0))
                                    if c > 0:
                                        P.op("pe", lambda e, rr=rr, i=i, part=part: e.matmul(
                                            py.ap[part * 64:(part + 1) * 64, i * 128:(i + 1) * 128], lhsT=prevb16[:, rr, :], rhs=CsT[:, rr, :], start=False, stop=True),
                                            reads=[prevbb, CsTb], writes=[py])
                                for i in range(2):
                                    P.op("dve", lambda e, i=i: e.scalar_tensor_tensor(out=ytmp[:, i, :], in0=xsT[:, i, csl], scalar=dsk_t[:, l, 2 * g + i:2 * g + i + 1],
                                                                                      in1=py.ap[:, i * 128:(i + 1) * 128], op0=ALU.mult, op1=ALU.add),
                                         reads=[xsb, py, cst], writes=[ytmpb])
                                P.op("dve", lambda e: e.tensor_tensor(out=yz[:, :, csl], in0=ytmp[:], in1=zs[:, :, csl], op=ALU.mult),
                                     reads=[ytmpb, zsb], writes=[yzb[0][c // 4], yzb[1][c // 4]])
                                pstt = psum()
                                P.op("pe", lambda e: e.matmul(pstt.ap[:, 0:256], lhsT=Btok[:], rhs=xdtd[:].rearrange("p a b -> p (a b)"), start=True, stop=True),
                                     reads=[Btokb, xdtdb], writes=[pstt])
                                P.op("dve", lambda e: e.tensor_tensor(out=prev[:], in0=prev[:], in1=smx[:, 4:8].unsqueeze(2).to_broadcast([128, 4, 64]), op=ALU.mult),
                                     reads=[prevb, smxb], writes=[prevb])
                                P.op("dve", lambda e: e.tensor_tensor(out=prev[:], in0=prev[:], in1=pstt.ap[:, 0:256].rearrange("p (a b) -> p a b", b=64), op=ALU.add),
                                     reads=[prevb, pstt], writes=[prevb])
                                P.op("act", lambda e: e.activation(out=prevb16n[:], in_=prev[:], func=AF.Copy), reads=[prevb], writes=[prevbbn])
                            r2, wo = load_wout(l, 512 + g * 256, 2)
                            for t in range(4):
                                tsl = slice(t * 512, (t + 1) * 512)
                                vs = [V(yz[:, i, tsl], yzb[i][t]) for i in range(2)]
                                rms_run(st, vs, vs, lambda i: ssmg_t[:, l, 2 * g + i:2 * g + i + 1], None)
                                add_wout(l, s, r2, wo, 2, lambda jj: yz[:, jj, tsl], [yzb[0][t], yzb[1][t]], t * 512, 512)

        xin_sem = [P.dma_sem(f"xin{i}") for i in range(2)]
        out_sem = [P.dma_sem(f"xout{i}") for i in range(2)]
        for s in range(NSQ):
            with ExitStack() as ph:
                st_t = [sb(f"xin{i}", [128, D], F32, ph) for i in range(2)]
                st_b = [P.buf(f"xin{i}") for i in range(2)]
                for tt in range(NT):
                    i = tt % 2
                    P.dma("sp", st_t[i][:], x_d[s, tt * 128:(tt + 1) * 128, :], xin_sem[i], writes=[st_b[i]])
                    for half in range(2):
                        pm = psum()
                        for cc in range(4):
                            c = half * 4 + cc
                            P.op("pe", lambda e, c=c, cc=cc, i=i: e.transpose(pm.ap[:, cc * 128:(cc + 1) * 128], st_t[i][:, c * 128:(c + 1) * 128], ident_t[:]),
                                 reads=[st_b[i], cst], writes=[pm], inc=(cc == 3))
                        bl = [xb[half * 4 + cc][tt // 4] for cc in range(4)]
                        if half == 0:
                            P.op("act", lambda e, half=half, tt=tt: e.activation(
                                out=xT_t[:, half * 4:(half + 1) * 4, tt * 128:(tt + 1) * 128],
                                in_=pm.ap.rearrange("p (c t) -> p c t", t=128), func=AF.Copy), reads=[pm], writes=bl)
                        else:
                            P.op("dve", lambda e, half=half, tt=tt: e.tensor_copy(
                                out=xT_t[:, half * 4:(half + 1) * 4, tt * 128:(tt + 1) * 128],
                                in_=pm.ap.rearrange("p (c t) -> p c t", t=128)), reads=[pm], writes=bl)
            for l in LAYERS:
                if cfg["ffn1"]:
                    ffn(l, s, 0)
                if MIX:
                    norm_mod(l, s, 1)
                    if "hg" in MIX:
                        for slot in range(2):
                            mixer_hg(l, s, slot)
                    if "dsa" in MIX:
                        mixer_dsa(l, s)
                    if "ssd" in MIX:
                        mixer_ssd(l, s)
                if cfg["ffn2"]:
                    ffn(l, s, 1)
            with ExitStack() as ph:
                st = rms_state(ph, "fin", 8, onesd_t)
                y_t = [sb(f"yfin{i}", [128, 8, 512], F32, ph) for i in range(2)]
                y_b = [P.buf(f"yfin{i}") for i in range(2)]
                o_t = [sb(f"otok{i}", [128, D], F32, ph) for i in range(2)]
                o_b = [P.buf(f"otok{i}") for i in range(2)]
                oq = 0
                for t in range(4):
                    yi = t % 2
                    rms_run(st, [xv(c, t) for c in range(8)], [V(y_t[yi][:, c, :], y_b[yi]) for c in range(8)],
                            lambda c: fng_t[:, c:c + 1], None)
                    for sub in range(4):
                        tt = t * 4 + sub
                        oi = oq % 2
                        oq += 1
                        for half in range(2):
                            pm = psum()
                            for cc in range(4):
                                c = half * 4 + cc
                                P.op("pe", lambda e, c=c, cc=cc, yi=yi, sub=sub: e.transpose(
                                    pm.ap[:, cc * 128:(cc + 1) * 128], y_t[yi][:, c, sub * 128:(sub + 1) * 128], ident_t[:]),
                                    reads=[y_b[yi], cst], writes=[pm], inc=(cc == 3))
                            if half == 0:
                                P.op("act", lambda e, oi=oi: e.activation(out=o_t[oi][:, 0:512], in_=pm.ap, func=AF.Copy),
                                     reads=[pm], writes=[o_b[oi]])
                            else:
                                P.op("dve", lambda e, oi=oi: e.tensor_copy(out=o_t[oi][:, 512:1024], in_=pm.ap),
                                     reads=[pm], writes=[o_b[oi]])
                        P.dma("sp", out_d[s, tt * 128:(tt + 1) * 128, :], o_t[oi][:], out_sem[oi], reads=[o_b[oi]])
        P.wait_all("sp")
        print("instructions emitted:", P.ninstr)
    return nc


def kernel(**inputs):
    inputs = {k: np.asarray(v, dtype=np.float32) for k, v in inputs.items()}
    nc = build_program(FULL_CFG)
    shared = _shared_inputs(inputs, FULL_CFG)
    in_maps = []
    for core in range(8):
        m = dict(shared)
        m.update(_host_inputs(inputs, core, FULL_CFG))
        in_maps.append(m)
    res = run_bass_kernel_spmd(nc, in_maps, core_ids=list(range(8)))
    out = np.concatenate([np.asarray(r["out"]).reshape(NSEQ, S, D) for r in res.results], axis=0)
    return out.astype(np.float32)
```

```python
import numpy as np
from contextlib import ExitStack
import concourse.bass as bass
import concourse.mybir as mybir
from concourse.bass_utils import run_bass_kernel_spmd

F32 = mybir.dt.float32
BF16 = mybir.dt.bfloat16
AF = mybir.ActivationFunctionType
ALU = mybir.AluOpType
AX = mybir.AxisListType

D = 1024
S = 2048
DEPTH = 2
DFF = 2816
NSEQ = 2
NT = S // 128
EPS = 1e-6
NEG = -1e30


class Buf:
    __slots__ = ("w", "r", "name")

    def __init__(self, name="", w=None):
        self.w = dict(w) if w else {}
        self.r = {}
        self.name = name


class V:
    __slots__ = ("ap", "buf")

    def __init__(self, ap, buf):
        self.ap = ap
        self.buf = buf

    def __getitem__(self, k):
        return V(self.ap[k], self.buf)


class Prog:
    def __init__(self, nc, es):
        self.nc = nc
        self.es = es
        self.E = dict(pe=nc.tensor, act=nc.scalar, dve=nc.vector, pool=nc.gpsimd, sp=nc.sync)
        self.sem = {}
        self.cnt = {}
        for e in ("pe", "act", "dve", "pool"):
            self.sem[e] = es.enter_context(nc.semaphore("s_" + e))
            self.cnt[e] = 0
        self.waited = {e: {} for e in self.E}
        self.ninstr = 0

    def dma_sem(self, name):
        key = "d_" + name
        self.sem[key] = self.es.enter_context(self.nc.semaphore("s_" + key))
        self.cnt[key] = 0
        return key

    def snapshot(self):
        return {k: v for k, v in self.cnt.items() if v > 0}

    def buf(self, name="", fresh=True):
        return Buf(name, self.snapshot() if fresh else None)

    def _emit_waits(self, eng, reads, writes, skipkey=None):
        need = {}
        for b in reads:
            for k, v in b.w.items():
                if v > need.get(k, 0):
                    need[k] = v
        for b in writes:
            for k, v in b.w.items():
                if k == skipkey:
                    continue
                if v > need.get(k, 0):
                    need[k] = v
            for k, v in b.r.items():
                if v > need.get(k, 0):
                    need[k] = v
        e = self.E[eng]
        wd = self.waited[eng]
        for k, v in need.items():
            if k == "pe" and eng == "pe":
                continue
            if wd.get(k, 0) >= v:
                continue
            wd[k] = v
            e.wait_ge(self.sem[k], v)
            self.ninstr += 1

    def op(self, eng, fn, reads=(), writes=(), inc=True):
        reads = [x.buf if isinstance(x, V) else x for x in reads]
        writes = [x.buf if isinstance(x, V) else x for x in writes]
        self._emit_waits(eng, reads, writes)
        ins = fn(self.E[eng])
        self.ninstr += 1
        if inc:
            self.cnt[eng] += 1
            ins.then_inc(self.sem[eng], 1)
            t = self.cnt[eng]
        else:
            t = self.cnt[eng] + 1
        for b in reads:
            if t > b.r.get(eng, 0):
                b.r[eng] = t
        for b in writes:
            b.w = {eng: t}
            b.r = {}
        return ins

    def dma(self, q, out, in_, semkey, reads=(), writes=(), **kw):
        reads = [x.buf if isinstance(x, V) else x for x in reads]
        writes = [x.buf if isinstance(x, V) else x for x in writes]
        self._emit_waits(q, reads, writes, skipkey=semkey)
        ins = self.E[q].dma_start(out=out, in_=in_, **kw)
        self.ninstr += 1
        self.cnt[semkey] += 16
        ins.then_inc(self.sem[semkey], 16)
        t = self.cnt[semkey]
        for b in reads:
            if t > b.r.get(semkey, 0):
                b.r[semkey] = t
        for b in writes:
            if semkey in b.w and len(b.w) == 1:
                b.w[semkey] = t
            else:
                b.w = {semkey: t}
            b.r = {}
        return ins

    def wait_all(self, eng):
        e = self.E[eng]
        for k, v in self.snapshot().items():
            if self.waited[eng].get(k, 0) >= v:
                continue
            self.waited[eng][k] = v
            e.wait_ge(self.sem[k], v)


O_HQ, O_HF, O_HI, O_HG, O_AQ, O_AK, O_AV, O_IQ, O_IK, O_IW, O_SZ, O_SX, O_SDT = (
    0, 256, 512, 768, 1024, 1280, 1344, 1408, 1920, 1984, 1992, 2504, 3528)
IWSCALE = float(8 ** -0.5 * 64 ** -0.5)
NIT = 11


def _win2_layout():
    off = {}
    ncol = {}
    cols = []

    def add(name, idx):
        idx = list(idx)
        off[name] = len(cols)
        ncol[name] = len(idx)
        cols.extend(idx)

    def sw(base, n):
        out = []
        for h in range(n // 64):
            b = base + h * 64
            out += list(range(b + 32, b + 64)) + list(range(b, b + 32))
        return out

    for s in range(2):
        add(f"HQ{s}", range(O_HQ + s * 128, O_HQ + (s + 1) * 128))
        add(f"HF{s}", range(O_HF + s * 128, O_HF + (s + 1) * 128))
        add(f"HG{s}", range(O_HG + s * 128, O_HG + (s + 1) * 128))
        add(f"HI{s}", range(O_HI + s * 128, O_HI + (s + 1) * 128))
    for s in range(2):
        add(f"AQ{s}", range(O_AQ + s * 128, O_AQ + (s + 1) * 128))
        add(f"AQW{s}", sw(O_AQ + s * 128, 128))
    add("AK2", list(range(O_AK, O_AK + 64)) * 2)
    add("AKW2", sw(O_AK, 64) * 2)
    for s in range(4):
        add(f"IQ{s}", range(O_IQ + s * 128, O_IQ + (s + 1) * 128))
        add(f"IQW{s}", sw(O_IQ + s * 128, 128))
    add("IK2", list(range(O_IK, O_IK + 64)) * 2)
    add("IKW2", sw(O_IK, 64) * 2)
    add("AVIW", list(range(O_AV, O_AV + 64)) + list(range(O_IW, O_IW + 8)))
    for g in range(2):
        add(f"Z{g}0", range(O_SZ + (2 * g) * 128, O_SZ + (2 * g + 1) * 128))
        add(f"Z{g}1", range(O_SZ + (2 * g + 1) * 128, O_SZ + (2 * g + 2) * 128))
        add(f"XS{g}0", range(O_SX + (2 * g) * 128, O_SX + (2 * g + 1) * 128))
        add(f"XS{g}1", range(O_SX + (2 * g + 1) * 128, O_SX + (2 * g + 2) * 128))
        add(f"B{g}", range(O_SX + 512 + g * 128, O_SX + 512 + (g + 1) * 128))
        add(f"C{g}", range(O_SX + 768 + g * 128, O_SX + 768 + (g + 1) * 128))
    add("SDT", range(O_SDT, O_SDT + 8))
    return np.array(cols, dtype=np.int64), off, ncol


W2COLS, W2OFF, W2N = _win2_layout()
NC2 = len(W2COLS)


def _consts():
    c = {}
    p = np.arange(128)
    c["ident"] = np.eye(128, dtype=np.float32)
    c["ones_d"] = np.full((128, 128), 1.0 / 1024.0, np.float32)
    c["ones_256"] = np.full((128, 128), 1.0 / 256.0, np.float32)
    c["ones_f"] = np.ones((128, 128), np.float32)
    o64 = np.zeros((128, 128), np.float32)
    o64[:64, :64] = 1.0 / 64.0
    o64[64:, 64:] = 1.0 / 64.0
    c["ones_64b"] = o64
    s_, t_ = p[:, None], p[None, :]
    c["bdmask"] = ((s_ // 64 == t_ // 64) & (t_ >= s_)).astype(np.float32)
    c["causT"] = (t_ >= s_).astype(np.float32)
    same = (s_ // 64 == t_ // 64)
    c["maskD2"] = (same & (t_ >= s_) & (s_ % 64 >= 32) & (t_ % 64 >= 32)).astype(np.float32)
    c["maskX"] = (same & (s_ % 64 < 32) & (t_ % 64 >= 32)).astype(np.float32)
    c["causbias"] = np.where(t_ <= s_, 0.0, NEG).astype(np.float32)
    c["Lgt"] = (s_ > t_).astype(np.float32)
    c["Uincl"] = (s_ <= t_).astype(np.float32)
    tt = np.arange(S)
    c["rmask"] = np.broadcast_to((tt % 64 != 0).astype(np.float32)[None, :], (128, S)).copy()
    inv_freq = 1.0 / (10000.0 ** (np.arange(0, 64, 2, dtype=np.float32) / 64.0))
    ang = tt.astype(np.float32)[:, None] * inv_freq[None, :]
    cos = np.cos(ang).astype(np.float32).T
    sin = np.sin(ang).astype(np.float32).T
    c["cosT"] = np.concatenate([cos, cos, cos, cos], axis=0)
    c["sinT"] = np.concatenate([-sin, sin, -sin, sin], axis=0)
    c["pow2"] = np.broadcast_to((2.0 ** -(np.arange(NIT + 1, dtype=np.float32) + 1.0))[None, :], (128, NIT + 1)).copy()
    return c


CONST_SHAPES = dict(ident=[128, 128], ones_d=[128, 128], ones_256=[128, 128], ones_f=[128, 128], ones_64b=[128, 128],
                    bdmask=[128, 128], maskD2=[128, 128], maskX=[128, 128], causT=[128, 128], causbias=[128, 128], Lgt=[128, 128], Uincl=[128, 128],
                    rmask=[128, S], cosT=[128, S], sinT=[128, S], pow2=[128, NIT + 1])

FULL_CFG = dict(nseq=2, layers=(0, 1), ffn1=True, mix=("hg", "dsa", "ssd"), ffn2=True, hostmod=False)


def _fm(v, nchunk):
    v = np.asarray(v, np.float32)
    lead = v.shape[:-1]
    r = v.reshape(lead + (nchunk, 128))
    r = np.moveaxis(r, -1, 0)
    return np.ascontiguousarray(r)


def _shared_inputs(inputs, cfg=FULL_CFG):
    m = {}
    if not cfg["hostmod"]:
        m["w_ada"] = np.ascontiguousarray(inputs["w_ada"])
        m["b_adaT"] = _fm(inputs["b_ada"], 72)
    m["norm_gT"] = _fm(inputs["norm_g"].reshape(DEPTH, 3 * D), 24)
    m["fnorm_gT"] = _fm(inputs["final_norm_g"], 8)
    if cfg["ffn1"] or cfg["ffn2"]:
        m["w_ffn_gu"] = np.ascontiguousarray(inputs["w_ffn_gu"])
        m["w_ffn_down"] = np.ascontiguousarray(inputs["w_ffn_down"])
    if cfg["mix"]:
        m["win2"] = np.ascontiguousarray(inputs["w_in"][:, :, W2COLS])
        m["w_out"] = np.ascontiguousarray(inputs["w_out"])
        m["lblT"] = _fm(inputs["lb_logits"], 2)
        m["hgngT"] = _fm(inputs["hg_norm_g"], 2)
        g64 = inputs["idx_k_norm_g"]
        b64 = inputs["idx_k_norm_b"]
        swp = np.concatenate([np.arange(32, 64), np.arange(0, 32)])
        kn = np.stack([np.tile(g64, (1, 2)), np.tile(b64, (1, 2)), np.tile(g64[:, swp], (1, 2)), np.tile(b64[:, swp], (1, 2))], axis=1)
        m["knT"] = np.ascontiguousarray(kn.transpose(2, 0, 1))
        m["convwT"] = np.ascontiguousarray(inputs["conv_w"].reshape(DEPTH, 4, 8, 128).transpose(3, 0, 2, 1))
        m["convbT"] = _fm(inputs["conv_b"], 8)
        m["ssmgT"] = _fm(inputs["ssm_norm_g"], 4)
        dsk = np.repeat(inputs["d_skip"], 64, axis=1)
        m["dskT"] = _fm(dsk, 4)
        m["dtbB"] = np.ascontiguousarray(np.broadcast_to(inputs["dt_bias"][None], (128, DEPTH, 8)))
        m["alogB"] = np.ascontiguousarray(np.broadcast_to(inputs["a_log"][None], (128, DEPTH, 8)))
    cc = _consts()
    for k in CONST_SHAPES:
        m[k] = cc[k]
    return m


def _host_inputs(inputs, core, cfg=FULL_CFG):
    ns = cfg["nseq"]
    b0 = core * NSEQ
    m = {"x": np.ascontiguousarray(inputs["x"][b0:b0 + ns])}
    if not cfg["hostmod"]:
        c = inputs["c"][b0:b0 + ns]
        m["cT"] = np.ascontiguousarray(c.reshape(ns, 8, 128).transpose(2, 1, 0))
    return m


def build_program(cfg=None):
    cfg = dict(FULL_CFG if cfg is None else cfg)
    NSQ = cfg["nseq"]
    LAYERS = list(cfg["layers"])
    MIX = tuple(cfg["mix"])
    nc = bass.Bass("TRN2", target_bir_lowering=False)

    def din(name, shape, dtype=F32):
        return nc.dram_tensor(name, list(shape), dtype, kind="ExternalInput").ap()

    x_d = din("x", [NSQ, S, D])
    if cfg["hostmod"]:
        modin_d = din("modT_in", [128, DEPTH, 72, NSQ])
    else:
        cT_d = din("cT", [128, 8, NSQ])
        wada_d = din("w_ada", [DEPTH, D, 9 * D])
        badaT_d = din("b_adaT", [128, DEPTH, 72])
    normgT_d = din("norm_gT", [128, DEPTH, 24])
    fngT_d = din("fnorm_gT", [128, 8])
    if cfg["ffn1"] or cfg["ffn2"]:
        wgu_d = din("w_ffn_gu", [DEPTH, 2, D, 2 * DFF])
        wdn_d = din("w_ffn_down", [DEPTH, 2, DFF, D])
    if MIX:
        win2_d = din("win2", [DEPTH, D, NC2])
        wout_d = din("w_out", [DEPTH, D, D])
        lblT_d = din("lblT", [128, DEPTH, 2])
        hgngT_d = din("hgngT", [128, DEPTH, 2])
        knT_d = din("knT", [128, DEPTH, 4])
        convwT_d = din("convwT", [128, DEPTH, 8, 4])
        convbT_d = din("convbT", [128, DEPTH, 8])
        ssmgT_d = din("ssmgT", [128, DEPTH, 4])
        dskT_d = din("dskT", [128, DEPTH, 4])
        dtbB_d = din("dtbB", [128, DEPTH, 8])
        alogB_d = din("alogB", [128, DEPTH, 8])
    cd = {k: din(k, shp) for k, shp in CONST_SHAPES.items()}
    out_d = nc.dram_tensor("out", [NSQ, S, D], F32, kind="ExternalOutput").ap()

    es = ExitStack()
    with es:
        P = Prog(nc, es)
        uid = [0]

        def sb(name, shape, dtype, st=es):
            uid[0] += 1
            return st.enter_context(nc.sbuf_tensor(f"sb{uid[0]}_{name}", list(shape), dtype))

        xT_t = sb("xT", [128, 8, S], F32)
        hT_t = sb("hT", [128, 8, S], BF16)
        RING = 3
        ring_t = [sb(f"ring{i}", [128, 6144], BF16) for i in range(RING)]
        ring_b = [Buf(f"ring{i}") for i in range(RING)]
        ring_sem = [P.dma_sem(f"ring{i}") for i in range(RING)]
        modT_t = sb("modT", [128, DEPTH, 72, NSQ], F32)
        AG_t = sb("AG", [128, DEPTH, NSQ, 6, 8], F32)
        normg_t = sb("normg", [128, DEPTH, 24], F32)
        fng_t = sb("fng", [128, 8], F32)
        eps_t = sb("eps", [128, 1], F32)
        one_t = sb("one", [128, 1], F32)
        ident_t = sb("ident", [128, 128], F32)
        identb_t = sb("identb", [128, 128], BF16)
        onesd_t = sb("onesd", [128, 128], BF16)

        cst = Buf("consts")
        modb = Buf("mod")
        csem = P.dma_sem("const")
        csemp = P.dma_sem("constp")
        misc_sem = P.dma_sem("miscp")
        P.dma("sp", ident_t[:], cd["ident"], csem, writes=[cst])
        P.dma("pool", identb_t[:], cd["ident"], csemp, writes=[cst])
        P.dma("pool", onesd_t[:], cd["ones_d"], csemp, writes=[cst])
        P.dma("sp", normg_t[:], normgT_d, csem, writes=[cst])
        P.dma("sp", fng_t[:], fngT_d, csem, writes=[cst])
        if MIX:
            ones256_t = sb("ones256", [128, 128], BF16)
            ones64b_t = sb("ones64b", [128, 128], BF16)
            onesf_t = sb("onesf", [128, 128], F32)
            bdmask_t = sb("bdmask", [128, 128], BF16)
            maskD2_t = sb("maskD2", [128, 128], BF16)
            maskX_t = sb("maskX", [128, 128], BF16)
            causT_t = sb("causT", [128, 128], BF16)
            causb_t = sb("causb", [128, 128], F32)
            Lgt_t = sb("Lgt", [128, 128], F32)
            Uincl_t = sb("Uincl", [128, 128], F32)
            pow2_t = sb("pow2", [128, NIT + 1], F32)
            lbl_t = sb("lbl", [128, DEPTH, 2], F32)
            lbv_t = sb("lbv", [128, DEPTH, 2], F32)
            oml_t = sb("oml", [128, DEPTH, 2], F32)
            hgng_t = sb("hgng", [128, DEPTH, 2], F32)
            kn_t = sb("kn", [128, DEPTH, 4], F32)
            cw_t = sb("cw", [128, DEPTH, 8, 4], F32)
            cb_t = sb("cb", [128, DEPTH, 8], F32)
            ssmg_t = sb("ssmg", [128, DEPTH, 4], F32)
            dsk_t = sb("dsk", [128, DEPTH, 4], F32)
            dtb_t = sb("dtbias", [128, DEPTH, 8], F32)
            nega_t = sb("nega", [128, DEPTH, 8], F32)
            negthr_t = sb("negthr", [128, 1], F32)
            for tname, tl, q in (("ones_256", ones256_t, "pool"), ("ones_64b", ones64b_t, "pool"), ("ones_f", onesf_t, "sp"),
                                 ("bdmask", bdmask_t, "pool"), ("maskD2", maskD2_t, "pool"), ("maskX", maskX_t, "pool"), ("causT", causT_t, "pool"), ("causbias", causb_t, "sp"),
                                 ("Lgt", Lgt_t, "sp"), ("Uincl", Uincl_t, "sp"), ("pow2", pow2_t, "sp")):
                P.dma(q, tl[:], cd[tname], csemp if q == "pool" else csem, writes=[cst])
            for dsrc, tl in ((lblT_d, lbl_t), (hgngT_d, hgng_t), (knT_d, kn_t), (convwT_d, cw_t), (convbT_d, cb_t),
                             (ssmgT_d, ssmg_t), (dskT_d, dsk_t), (dtbB_d, dtb_t), (alogB_d, nega_t)):
                P.dma("sp", tl[:], dsrc, csem, writes=[cst])
        P.op("dve", lambda e: e.memset(eps_t[:], EPS), writes=[cst])
        P.op("dve", lambda e: e.memset(one_t[:], 1.0), writes=[cst])
        cst.w = P.snapshot()
        if MIX:
            P.op("dve", lambda e: e.memset(negthr_t[:], -1e29), reads=[cst], writes=[cst])
            P.op("dve", lambda e: e.memset(lbv_t[:], 0.0), reads=[cst], writes=[cst])
            P.op("dve", lambda e: e.tensor_tensor(out=lbv_t[:, 1, :], in0=lbl_t[:, 1, :], in1=lbl_t[:, 0, :], op=ALU.subtract),
                 reads=[cst], writes=[cst])
            P.op("act", lambda e: e.activation(out=lbv_t[:, 1, :], in_=lbv_t[:, 1, :], func=AF.Sigmoid), reads=[cst], writes=[cst])
            P.op("dve", lambda e: e.tensor_scalar(out=oml_t[:], in0=lbv_t[:], scalar1=-1.0, scalar2=1.0, op0=ALU.mult, op1=ALU.add),
                 reads=[cst], writes=[cst])
            P.op("act", lambda e: e.activation(out=nega_t[:], in_=nega_t[:], func=AF.Exp), reads=[cst], writes=[cst])
            P.op("dve", lambda e: e.tensor_scalar(out=nega_t[:], in0=nega_t[:], scalar1=-1.0, scalar2=None, op0=ALU.mult),
                 reads=[cst], writes=[cst])

        ps_t = [es.enter_context(nc.psum_tensor(f"ps{i}", [128, 512], F32)) for i in range(8)]
        ps_b = [Buf(f"ps{i}") for i in range(8)]
        ps_rr = [0]

        ps_held = set()

        def psum(hold=False):
            while True:
                i = ps_rr[0] % 8
                ps_rr[0] += 1
                if i not in ps_held:
                    break
            if hold:
                ps_held.add(i)
            return V(ps_t[i][:], ps_b[i])

        def psum_release(v):
            ps_held.discard(ps_b.index(v.buf))

        def bf(pm):
            return pm.ap.bitcast(BF16)

        xb = [[Buf(f"x{c}_{t}") for t in range(4)] for c in range(8)]
        hb = [[Buf(f"h{c}_{t}") for t in range(4)] for c in range(8)]

        def xv(c, t):
            return V(xT_t[:, c, t * 512:(t + 1) * 512], xb[c][t])

        def hv(c, t):
            return V(hT_t[:, c, t * 512:(t + 1) * 512], hb[c][t])

        ring_rr = [0]

        def ring_next():
            i = ring_rr[0] % RING
            ring_rr[0] += 1
            return i

        def load_wset(l, names):
            i = ring_next()
            views = {}
            pos = 0
            for nm in names:
                n = W2N[nm]
                dst = ring_t[i][:, pos * 8:(pos + n) * 8].rearrange("p (k c) -> p k c", c=n)
                src = win2_d[l][:, W2OFF[nm]:W2OFF[nm] + n].rearrange("(k p) c -> p k c", p=128)
                P.dma("pool", dst, src, ring_sem[i], writes=[ring_b[i]])
                views[nm] = dst
                pos += n
            return i, views

        def load_wout(l, row0, nj):
            i = ring_next()
            dst = ring_t[i][:, 0:nj * 1024].rearrange("p (j m) -> p j m", m=1024)
            P.dma("pool", dst, wout_d[l][row0:row0 + nj * 128, :].rearrange("(j p) m -> p j m", p=128), ring_sem[i],
                  writes=[ring_b[i]])
            return i, dst

        def proj_fm(wview, r, t):
            pm = psum()
            for k in range(8):
                P.op("pe", lambda e, k=k: e.matmul(pm.ap, lhsT=wview[:, k, :], rhs=hT_t[:, k, t * 512:(t + 1) * 512],
                                                   start=(k == 0), stop=(k == 7)),
                     reads=[ring_b[r], hb[k][t]], writes=[pm], inc=(k == 7))
            return pm

        def proj_tm(wview, rbuf, tt, n):
            pm = psum()
            for k in range(8):
                P.op("pe", lambda e, k=k: e.matmul(pm.ap[:, 0:n], lhsT=hT_t[:, k, tt * 128:(tt + 1) * 128], rhs=wview[:, k, :],
                                                   start=(k == 0), stop=(k == 7)),
                     reads=[rbuf, hb[k][tt // 4]], writes=[pm], inc=(k == 7))
            return pm

        if cfg["hostmod"]:
            P.dma("sp", modT_t[:], modin_d, P.dma_sem("modin"), writes=[modb])
        else:
            with ExitStack() as ph:
                wa_t = [sb(f"wada{i}", [128, 8, 768], BF16, ph) for i in range(2)]
                cTb_t = sb("cTb", [128, 8, NSQ], BF16, ph)
                wa_b = [P.buf(f"wada{i}") for i in range(2)]
                wa_sem = [P.dma_sem(f"wada{i}") for i in range(2)]
                cT_t = sb("cT", [128, 8, NSQ], F32, ph)
                bada_t = sb("badaT", [128, DEPTH, 72], F32, ph)
                condb = P.buf("cond")
                cond_sem = P.dma_sem("cond")
                P.dma("sp", cT_t[:], cT_d, cond_sem, writes=[condb])
                P.dma("sp", bada_t[:], badaT_d, cond_sem, writes=[condb])
                P.op("act", lambda e: e.activation(out=cTb_t[:], in_=cT_t[:], func=AF.Silu), reads=[condb], writes=[condb])
                npiece = 12
                for l in LAYERS:
                    pm = psum()
                    for pc in range(npiece):
                        i = pc % 2
                        P.dma("pool", wa_t[i][:], wada_d[l][:, pc * 768:(pc + 1) * 768].rearrange("(k p) c -> p k c", p=128),
                              wa_sem[i], writes=[wa_b[i]])
                        for m in range(6):
                            mg = pc * 6 + m
                            for k in range(8):
                                P.op("pe", lambda e, i=i, m=m, k=k, mg=mg: e.matmul(
                                    pm.ap[:, mg * NSQ:(mg + 1) * NSQ], lhsT=wa_t[i][:, k, m * 128:(m + 1) * 128], rhs=cTb_t[:, k, :],
                                    start=(k == 0), stop=(k == 7)),
                                    reads=[wa_b[i], condb], writes=[pm], inc=(k == 7))
                    P.op("dve", lambda e, l=l: e.tensor_tensor(
                        out=modT_t[:, l, :, :], in0=pm.ap[:, 0:72 * NSQ].rearrange("p (m s) -> p m s", s=NSQ),
                        in1=bada_t[:, l, :].unsqueeze(2).to_broadcast([128, 72, NSQ]), op=ALU.add),
                        reads=[pm, condb], writes=[modb])
        for l in LAYERS:
            for s in range(NSQ):
                for j in range(3):
                    P.op("dve", lambda e, l=l, s=s, j=j: e.scalar_tensor_tensor(
                        out=AG_t[:, l, s, j, :], in0=modT_t[:, l, (3 * j + 1) * 8:(3 * j + 2) * 8, s], scalar=1.0,
                        in1=normg_t[:, l, j * 8:(j + 1) * 8], op0=ALU.add, op1=ALU.mult),
                        reads=[modb, cst], writes=[modb])
                    P.op("dve", lambda e, l=l, s=s, j=j: e.tensor_scalar(
                        out=AG_t[:, l, s, 3 + j, :], in0=modT_t[:, l, (3 * j + 2) * 8:(3 * j + 3) * 8, s],
                        scalar1=(1.0 if j == 1 else 0.5), scalar2=None, op0=ALU.mult),
                        reads=[modb], writes=[modb])

        def Avec(l, s, j, c):
            return AG_t[:, l, s, j, c:c + 1]

        def Gvec(l, s, j, c):
            return AG_t[:, l, s, 3 + j, c:c + 1]

        def Bvec(l, s, j, c):
            return modT_t[:, l, 3 * j * 8 + c, s:s + 1]

        def rms_state(ph, tag, C, ones_t):
            return dict(C=C, T=512, ones=ones_t,
                        sq=sb(f"sq_{tag}", [128, C, 512], BF16, ph), rs=sb(f"rs_{tag}", [128, 512], F32, ph),
                        tm=sb(f"tm_{tag}", [128, 2, 512], F32, ph),
                        sqb=P.buf("sq"), rsb=P.buf("rs"), tmb=[P.buf("tm0"), P.buf("tm1")])

        def rms_run(st, srcs, outs, scale_fn, bias_fn):
            C = st["C"]
            T = st["T"]
            sqv = V(st["sq"][:], st["sqb"])
            for c in range(C):
                P.op("act", lambda e, c=c: e.activation(out=st["sq"][:, c, :], in_=srcs[c].ap, func=AF.Square),
                     reads=[srcs[c]], writes=[sqv])
            pm = psum()
            for c in range(C):
                P.op("pe", lambda e, c=c: e.matmul(pm.ap[:, 0:T], lhsT=st["ones"][:], rhs=st["sq"][:, c, :],
                                                   start=(c == 0), stop=(c == C - 1)),
                     reads=[sqv, cst], writes=[pm], inc=(c == C - 1))
            rsv = V(st["rs"][:], st["rsb"])
            P.op("act", lambda e: e.activation(out=st["rs"][:], in_=pm.ap[:, 0:T], func=AF.Sqrt, bias=eps_t[:], scale=1.0),
                 reads=[pm, cst], writes=[rsv])
            P.op("dve", lambda e: e.reciprocal(out=st["rs"][:], in_=st["rs"][:]), reads=[rsv], writes=[rsv])
            for c in range(C):
                bias = bias_fn(c) if bias_fn is not None else None
                if bias is None:
                    P.op("dve", lambda e, c=c: e.scalar_tensor_tensor(
                        out=outs[c].ap, in0=srcs[c].ap, scalar=scale_fn(c), in1=st["rs"][:], op0=ALU.mult, op1=ALU.mult),
                        reads=[srcs[c], rsv, modb, cst], writes=[outs[c]])
                else:
                    tb = st["tmb"][c % 2]
                    P.op("dve", lambda e, c=c: e.scalar_tensor_tensor(
                        out=st["tm"][:, c % 2, :], in0=srcs[c].ap, scalar=scale_fn(c), in1=st["rs"][:], op0=ALU.mult, op1=ALU.mult),
                        reads=[srcs[c], rsv, modb, cst], writes=[tb])
                    P.op("act", lambda e, c=c, bias=bias: e.activation(
                        out=outs[c].ap, in_=st["tm"][:, c % 2, :], func=AF.Identity, bias=bias, scale=1.0),
                        reads=[tb, modb], writes=[outs[c]])

        def norm_mod(l, s, j):
            with ExitStack() as ph:
                st = rms_state(ph, "nm", 8, onesd_t)
                for t in range(4):
                    rms_run(st, [xv(c, t) for c in range(8)], [hv(c, t) for c in range(8)],
                            lambda c: Avec(l, s, j, c), lambda c: Bvec(l, s, j, c))

        def add_wout(l, s, r2, wo, nj, rhs_fn, rhs_bufs, t0, ntok):
            for m in range(8):
                pw = psum()
                for jj in range(nj):
                    P.op("pe", lambda e, jj=jj, m=m: e.matmul(pw.ap[:, 0:ntok], lhsT=wo[:, jj, m * 128:(m + 1) * 128], rhs=rhs_fn(jj),
                                                             start=(jj == 0), stop=(jj == nj - 1)),
                         reads=[ring_b[r2]] + list(rhs_bufs), writes=[pw], inc=(jj == nj - 1))
                xbuf = xb[m][t0 // 512]
                P.op("dve", lambda e, m=m: e.scalar_tensor_tensor(
                    out=xT_t[:, m, t0:t0 + ntok], in0=pw.ap[:, 0:ntok], scalar=Gvec(l, s, 1, m), in1=xT_t[:, m, t0:t0 + ntok],
                    op0=ALU.mult, op1=ALU.add), reads=[pw, xbuf, modb], writes=[xbuf])

        def ffn(l, s, i):
            j = 0 if i == 0 else 2
            norm_mod(l, s, j)
            with ExitStack() as ph:
                a_t = [sb(f"a{q}", [128, 2, 512], BF16, ph) for q in range(3)]
                a_b = [P.buf(f"a{q}") for q in range(3)]
                sg_t = [sb(f"sg{q}", [128, 512], F32, ph) for q in range(2)]
                sg_b = [P.buf(f"sg{q}") for q in range(2)]
                NG = 11
                wgu = wgu_d[l, i].rearrange("(k p) c -> p k c", p=128)
                wdn = wdn_d[l, i]

                def load(g):
                    r = ring_next()
                    rt = ring_t[r]
                    P.dma("pool", rt[:, 0:2048].rearrange("p (k c) -> p k c", c=256), wgu[:, :, g * 256:(g + 1) * 256],
                          ring_sem[r], writes=[ring_b[r]])
                    P.dma("pool", rt[:, 2048:4096].rearrange("p (k c) -> p k c", c=256),
                          wgu[:, :, DFF + g * 256:DFF + (g + 1) * 256], ring_sem[r], writes=[ring_b[r]])
                    P.dma("pool", rt[:, 4096:6144].rearrange("p (j m) -> p j m", m=1024),
                          wdn[g * 256:(g + 1) * 256, :].rearrange("(j p) m -> p j m", p=128), ring_sem[r], writes=[ring_b[r]])
                    return r
                slots = {0: load(0)}
                slots[1] = load(1)
                items = [(g, t) for g in range(NG) for t in range(4)]
                sgq = [0]

                def emit_gu(n):
                    g, t = items[n]
                    r = slots[g]
                    wg = ring_t[r][:, 0:2048].rearrange("p (k c) -> p k c", c=256)
                    wu = ring_t[r][:, 2048:4096].rearrange("p (k c) -> p k c", c=256)
                    av = a_b[n % 3]
                    for jj in range(2):
                        pg = psum()
                        pu = psum()
                        for k in range(8):
                            P.op("pe", lambda e, k=k, jj=jj: e.matmul(pg.ap, lhsT=wg[:, k, jj * 128:(jj + 1) * 128], rhs=hv(k, t).ap,
                                                                     start=(k == 0), stop=(k == 7)),
                                 reads=[ring_b[r], hv(k, t)], writes=[pg], inc=(k == 7))
                        for k in range(8):
                            P.op("pe", lambda e, k=k, jj=jj: e.matmul(pu.ap, lhsT=wu[:, k, jj * 128:(jj + 1) * 128], rhs=hv(k, t).ap,
                                                                     start=(k == 0), stop=(k == 7)),
                                 reads=[ring_b[r], hv(k, t)], writes=[pu], inc=(k == 7))
                        q = sgq[0] % 2
                        sgq[0] += 1
                        P.op("act", lambda e, q=q: e.activation(out=sg_t[q][:], in_=pg.ap, func=AF.Silu),
                             reads=[pg], writes=[sg_b[q]])
                        P.op("dve", lambda e, q=q, jj=jj: e.tensor_tensor(out=a_t[n % 3][:, jj, :], in0=pu.ap, in1=sg_t[q][:], op=ALU.mult),
                             reads=[pu, sg_b[q]], writes=[av])

                def emit_down(n):
                    g, t = items[n]
                    r = slots[g]
                    wd = ring_t[r][:, 4096:6144].rearrange("p (j m) -> p j m", m=1024)
                    for m in range(8):
                        po = psum()
                        for jj in range(2):
                            P.op("pe", lambda e, jj=jj, m=m: e.matmul(po.ap, lhsT=wd[:, jj, m * 128:(m + 1) * 128], rhs=a_t[n % 3][:, jj, :],
                                                                     start=(jj == 0), stop=(jj == 1)),
                                 reads=[ring_b[r], a_b[n % 3]], writes=[po], inc=(jj == 1))
                        P.op("dve", lambda e, m=m: e.scalar_tensor_tensor(
                            out=xv(m, t).ap, in0=po.ap, scalar=Gvec(l, s, j, m), in1=xv(m, t).ap, op0=ALU.mult, op1=ALU.add),
                            reads=[po, xv(m, t), modb], writes=[xv(m, t)])

                for n in range(len(items)):
                    g, t = items[n]
                    emit_gu(n)
                    if n > 0:
                        emit_down(n - 1)
                    if t == 0 and g + 2 < NG:
                        slots[g + 2] = load(g + 2)
                emit_down(len(items) - 1)

        def mixer_hg(l, s, slot):
            with ExitStack() as ph:
                qt = sb("hg_qt", [128, S], BF16, ph)
                kt = sb("hg_kt", [128, S], BF16, ph)
                ktok = sb("hg_ktok", [128, NT, 128], BF16, ph)
                vtok = sb("hg_vtok", [128, NT, 128], BF16, ph)
                e123 = sb("hg_e", [128, 3, 32], F32, ph)
                bmid = sb("hg_bmid", [128, 32], F32, ph)
                qB = sb("hg_qB", [128, S], BF16, ph)
                kB = sb("hg_kB", [128, S], BF16, ph)
                bh = sb("hg_bh", [128, 64], F32, ph)
                qtb, ktb, ktokb, vtokb, eb = P.buf("qt"), P.buf("kt"), P.buf("ktok"), P.buf("vtok"), P.buf("e")
                qBb, kBb = P.buf("qB"), P.buf("kB")
                r, wv = load_wset(l, [f"HQ{slot}", f"HF{slot}", f"HG{slot}", f"HI{slot}"])
                rb = ring_b[r]
                lbp = lbv_t[:, l, slot:slot + 1]
                omlp = oml_t[:, l, slot:slot + 1]
                with ExitStack() as pa:
                    bb = sb("hg_bb", [128, S], F32, pa)
                    bb2 = sb("hg_bb2", [128, S], F32, pa)
                    bb2b = P.buf("bb2")
                    tA = [sb(f"hg_tA{i}", [128, 512], F32, pa) for i in range(2)]
                    rmask = sb("hg_rmask", [128, S], BF16, pa)
                    bbb, tAb, rmb = P.buf("bb"), [P.buf("tA0"), P.buf("tA1")], P.buf("rmask")
                    P.dma("pool", rmask[:], cd["rmask"], misc_sem, writes=[rmb])
                    for t in range(4):
                        sl = slice(t * 512, (t + 1) * 512)
                        pf = proj_fm(wv[f"HF{slot}"], r, t)
                        P.op("act", lambda e: e.activation(out=tA[0][:], in_=pf.ap, func=AF.Sigmoid), reads=[pf], writes=[tAb[0]])
                        P.op("dve", lambda e: e.tensor_scalar(out=tA[0][:], in0=tA[0][:], scalar1=omlp, scalar2=lbp, op0=ALU.mult, op1=ALU.add),
                             reads=[tAb[0], cst], writes=[tAb[0]])
                        P.op("act", lambda e: e.activation(out=bb[:, sl], in_=tA[0][:], func=AF.Ln), reads=[tAb[0]], writes=[bbb])
                        P.op("dve", lambda e: e.tensor_scalar(out=kt[:, sl], in0=tA[0][:], scalar1=-1.0, scalar2=1.0, op0=ALU.mult, op1=ALU.add),
                             reads=[tAb[0]], writes=[ktb])
                        pq = proj_fm(wv[f"HQ{slot}"], r, t)
                        P.op("act", lambda e: e.activation(out=qt[:, sl], in_=pq.ap, func=AF.Copy), reads=[pq], writes=[qtb])
                        for tt in range(t * 4, t * 4 + 4):
                            pv = proj_tm(wv[f"HI{slot}"], rb, tt, 128)
                            P.op("dve", lambda e, tt=tt: e.tensor_copy(out=vtok[:, tt, :], in_=pv.ap[:, 0:128]), reads=[pv], writes=[vtokb])
                    P.op("dve", lambda e: e.tensor_tensor_scan(out=bb[:], data0=rmask[:], data1=bb[:], initial=0.0, op0=ALU.mult, op1=ALU.add),
                         reads=[bbb, rmb], writes=[bbb])
                    bb3 = bb[:].rearrange("p (c j) -> p c j", j=64)
                    bb4 = bb[:].rearrange("p (c j) -> p c j", j=32)
                    P.op("dve", lambda e: e.tensor_copy(out=bh[:], in_=bb4[:, :, 15]), reads=[bbb], writes=[eb])
                    P.op("dve", lambda e: e.tensor_tensor(out=bb2[:].rearrange("p (c j) -> p c j", j=32), in0=bb4,
                                                          in1=bh[:].unsqueeze(2).to_broadcast([128, 64, 32]), op=ALU.subtract),
                         reads=[bbb, eb], writes=[bb2b])
                    for t in range(4):
                        sl = slice(t * 512, (t + 1) * 512)
                        P.op("act", lambda e: e.activation(out=tA[0][:], in_=bb2[:, sl], func=AF.Exp), reads=[bb2b], writes=[tAb[0]])
                        P.op("dve", lambda e: e.tensor_tensor(out=qB[:, sl], in0=qt[:, sl], in1=tA[0][:], op=ALU.mult), reads=[qtb, tAb[0]], writes=[qBb])
                        P.op("act", lambda e: e.activation(out=tA[1][:], in_=bb2[:, sl], func=AF.Exp, scale=-1.0), reads=[bb2b], writes=[tAb[1]])
                        P.op("dve", lambda e: e.tensor_tensor(out=kB[:, sl], in0=kt[:, sl], in1=tA[1][:], op=ALU.mult), reads=[ktb, tAb[1]], writes=[kBb])
                    P.op("act", lambda e: e.activation(out=e123[:, 0, :], in_=bb3[:, :, 31], func=AF.Exp), reads=[bbb], writes=[eb])
                    P.op("act", lambda e: e.activation(out=e123[:, 2, :], in_=bb3[:, :, 63], func=AF.Exp), reads=[bbb], writes=[eb])
                    P.op("dve", lambda e: e.tensor_copy(out=bmid[:], in_=bb3[:, :, 31]), reads=[bbb], writes=[eb])
                    P.op("dve", lambda e: e.tensor_tensor(out=bb3, in0=bb3, in1=bmid[:].unsqueeze(2).to_broadcast([128, 32, 64]), op=ALU.subtract),
                         reads=[bbb, eb], writes=[bbb])
                    P.op("act", lambda e: e.activation(out=e123[:, 1, :], in_=bb3[:, :, 63], func=AF.Exp), reads=[bbb], writes=[eb])
                    for t in range(4):
                        sl = slice(t * 512, (t + 1) * 512)
                        P.op("act", lambda e: e.activation(out=tA[0][:], in_=bb[:, sl], func=AF.Exp), reads=[bbb], writes=[tAb[0]])
                        P.op("dve", lambda e: e.tensor_tensor(out=qt[:, sl], in0=qt[:, sl], in1=tA[0][:], op=ALU.mult), reads=[qtb, tAb[0]], writes=[qtb])
                        P.op("act", lambda e: e.activation(out=tA[1][:], in_=bb[:, sl], func=AF.Exp, scale=-1.0), reads=[bbb], writes=[tAb[1]])
                        P.op("dve", lambda e: e.tensor_tensor(out=kt[:, sl], in0=kt[:, sl], in1=tA[1][:], op=ALU.mult), reads=[ktb, tAb[1]], writes=[ktb])
                for t4 in range(4):
                    pm = psum()
                    pmb = bf(pm)
                    for i in range(4):
                        tt = t4 * 4 + i
                        P.op("pe", lambda e, i=i, tt=tt: e.transpose(pmb[:, i * 128:(i + 1) * 128], kt[:, tt * 128:(tt + 1) * 128], identb_t[:]),
                             reads=[ktb, cst], writes=[pm], inc=(i == 3))
                    P.op("dve", lambda e, t4=t4: e.tensor_copy(out=ktok[:, t4 * 4:(t4 + 1) * 4, :],
                                                               in_=pmb[:, 0:512].rearrange("p (a b) -> p a b", b=128)),
                         reads=[pm], writes=[ktokb])
                with ExitStack() as pb:
                    kvs = sb("hg_kvs", [128, 64, 32], F32, pb)
                    e3bc = sb("hg_e3bc", [128, 64, 32], F32, pb)
                    smid = sb("hg_smid", [128, 32, 64], BF16, pb)
                    scm = [sb(f"hg_scm{i}", [128, 2, 128], BF16, pb) for i in range(2)]
                    sctmp = sb("hg_sctmp", [128, 2, 32], BF16, pb)
                    sctb = P.buf("sctmp")
                    osb = sb("hg_osb", [128, 512], F32, pb)
                    sgl = sb("hg_sgl", [128, 512], F32, pb)
                    oA = [sb(f"hg_oA{i}", [128, 512], BF16, pb) for i in range(2)]
                    kvsb, e3b, smb, scb, osbb, sglb = P.buf("kvs"), P.buf("e3bc"), P.buf("smid"), [P.buf("scm0"), P.buf("scm1")], P.buf("osb"), P.buf("sgl")
                    oAb = [P.buf("oA0"), P.buf("oA1")]
                    st = rms_state(pb, "hg", 1, ones64b_t)
                    for q_ in range(2):
                        P.op("dve", lambda e, q_=q_: e.memset(scm[q_][:], 0.0), writes=[scb[q_]])
                    for c0 in range(0, 32, 8):
                        pmh = [psum(), psum()]
                        for half in range(2):
                            pm = pmh[half]
                            for idx in range(4):
                                c = c0 + 2 * idx + half
                                tt = c // 2
                                for part in range(2):
                                    last = (idx == 3 and part == 1)
                                    P.op("pe", lambda e, pm=pm, idx=idx, tt=tt, half=half, part=part: e.matmul(
                                        pm.ap[part * 64:(part + 1) * 64, idx * 64:(idx + 1) * 64],
                                        lhsT=ktok[half * 64:(half + 1) * 64, tt, part * 64:(part + 1) * 64],
                                        rhs=vtok[half * 64:(half + 1) * 64, tt, part * 64:(part + 1) * 64], start=True, stop=True),
                                        reads=[ktokb, vtokb], writes=[pm], inc=last)
                            P.op("dve", lambda e, pm=pm, c0=c0, half=half: e.tensor_tensor(
                                out=kvs[:, :, c0 + half:c0 + 8:2].rearrange("p v c -> p c v"), in0=pm.ap[:, 0:256].rearrange("p (c v) -> p c v", v=64),
                                in1=e123[:, 1, c0 + half:c0 + 8:2].unsqueeze(2).to_broadcast([128, 4, 64]), op=ALU.mult),
                                reads=[pm, eb], writes=[kvsb])
                    P.op("dve", lambda e: e.memset(e3bc[:, :, 0:1], 0.0), writes=[e3b])
                    P.op("dve", lambda e: e.tensor_copy(out=e3bc[:, :, 1:32], in_=e123[:, 2, 1:32].unsqueeze(1).to_broadcast([128, 64, 31])),
                         reads=[eb], writes=[e3b])
                    P.op("dve", lambda e: e.tensor_tensor_scan(out=kvs[:].rearrange("p v c -> p (v c)"), data0=e3bc[:].rearrange("p v c -> p (v c)"),
                                                               data1=kvs[:].rearrange("p v c -> p (v c)"), initial=0.0, op0=ALU.mult, op1=ALU.add),
                         reads=[kvsb, e3b], writes=[kvsb])
                    P.op("dve", lambda e: e.memset(smid[:, 0, :], 0.0), writes=[smb])
                    P.op("dve", lambda e: e.tensor_tensor(out=smid[:, 1:32, :], in0=kvs[:, :, 0:31].rearrange("p v c -> p c v"),
                                                          in1=e123[:, 0, 1:32].unsqueeze(2).to_broadcast([128, 31, 64]), op=ALU.mult),
                         reads=[kvsb, eb], writes=[smb])
                    r2, wo = load_wout(l, slot * 128, 1)
                    for t in range(4):
                        pos_ = [psum(hold=True), psum(hold=True)]
                        for i in range(4):
                            tt = t * 4 + i
                            q = tt % 2
                            tsl = slice(tt * 128, (tt + 1) * 128)
                            pscs = [psum(), psum()]
                            for cc in range(2):
                                c = 2 * tt + cc
                                R = slice(cc * 64, (cc + 1) * 64)
                                for part in range(2):
                                    pr = slice(part * 64, (part + 1) * 64)
                                    psc = pscs[part]
                                    P.op("pe", lambda e, psc=psc, c=c, R=R, pr=pr: e.matmul(
                                        psc.ap[R, 0:32], lhsT=kB[pr, c * 64:(c + 1) * 64], rhs=qB[pr, c * 64:c * 64 + 32],
                                        start=True, stop=True), reads=[kBb, qBb], writes=[psc], inc=False)
                                    P.op("pe", lambda e, psc=psc, c=c, R=R, pr=pr: e.matmul(
                                        psc.ap[R, 32:64], lhsT=kB[pr, c * 64:(c + 1) * 64], rhs=qB[pr, c * 64 + 32:c * 64 + 64],
                                        start=True, stop=True), reads=[kBb, qBb], writes=[psc], inc=False)
                                    P.op("pe", lambda e, psc=psc, c=c, R=R, pr=pr: e.matmul(
                                        psc.ap[R, 64:96], lhsT=kt[pr, c * 64:(c + 1) * 64], rhs=qt[pr, c * 64 + 32:c * 64 + 64],
                                        start=True, stop=True), reads=[ktb, qtb], writes=[psc], inc=True)
                            for cc in range(2):
                                R = slice(cc * 64, (cc + 1) * 64)
                                c0_ = cc * 64
                                for part in range(2):
                                    psc = pscs[part]
                                    P.op("dve", lambda e, q=q, R=R, psc=psc, c0_=c0_, part=part: e.tensor_tensor(
                                        out=scm[q][R, part, c0_:c0_ + 32], in0=psc.ap[R, 0:32], in1=bdmask_t[R, c0_:c0_ + 32], op=ALU.mult),
                                        reads=[psc, cst], writes=[scb[q]])
                                    P.op("dve", lambda e, q=q, R=R, psc=psc, c0_=c0_, part=part: e.tensor_tensor(
                                        out=scm[q][R, part, c0_ + 32:c0_ + 64], in0=psc.ap[R, 32:64], in1=maskD2_t[R, c0_ + 32:c0_ + 64], op=ALU.mult),
                                        reads=[psc, cst], writes=[scb[q]])
                                    P.op("dve", lambda e, R=R, psc=psc, c0_=c0_, part=part: e.tensor_tensor(
                                        out=sctmp[R, part, :], in0=psc.ap[R, 64:96], in1=maskX_t[R, c0_ + 32:c0_ + 64], op=ALU.mult),
                                        reads=[psc, cst], writes=[sctb])
                                    P.op("dve", lambda e, q=q, R=R, c0_=c0_, part=part: e.tensor_tensor(
                                        out=scm[q][R, part, c0_ + 32:c0_ + 64], in0=scm[q][R, part, c0_ + 32:c0_ + 64], in1=sctmp[R, part, :], op=ALU.add),
                                        reads=[scb[q], sctb], writes=[scb[q]])
                            for part in range(2):
                                po = pos_[part]
                                P.op("pe", lambda e, po=po, part=part, i=i, tt=tt, q=q: e.matmul(
                                    po.ap[part * 64:(part + 1) * 64, i * 128:(i + 1) * 128], lhsT=vtok[:, tt, part * 64:(part + 1) * 64],
                                    rhs=scm[q][:, part, :], start=True, stop=False),
                                    reads=[vtokb, scb[q]], writes=[po], inc=False)
                                for cc in range(2):
                                    c = 2 * tt + cc
                                    P.op("pe", lambda e, po=po, part=part, i=i, cc=cc, c=c: e.matmul(
                                        po.ap[part * 64:(part + 1) * 64, i * 128 + cc * 64:i * 128 + (cc + 1) * 64],
                                        lhsT=smid[part * 64:(part + 1) * 64, c, :], rhs=qt[part * 64:(part + 1) * 64, c * 64:(c + 1) * 64],
                                        start=False, stop=(cc == 1)),
                                        reads=[smb, qtb], writes=[po], inc=(cc == 1))
                        for part in range(2):
                            pr = slice(part * 64, (part + 1) * 64)
                            P.op("act", lambda e, part=part, pr=pr: e.activation(out=osb[pr, :], in_=pos_[part].ap[pr, :], func=AF.Copy),
                                 reads=[pos_[part]], writes=[osbb])
                            psum_release(pos_[part])
                        ov = V(osb[:], osbb)
                        rms_run(st, [ov], [ov], lambda c: hgng_t[:, l, slot:slot + 1], None)
                        pg = proj_fm(wv[f"HG{slot}"], r, t)
                        P.op("act", lambda e: e.activation(out=sgl[:], in_=pg.ap, func=AF.Silu), reads=[pg], writes=[sglb])
                        oq = t % 2
                        P.op("dve", lambda e, oq=oq: e.tensor_tensor(out=oA[oq][:], in0=osb[:], in1=sgl[:], op=ALU.mult),
                             reads=[osbb, sglb], writes=[oAb[oq]])
                        add_wout(l, s, r2, wo, 1, lambda jj, oq=oq: oA[oq][:], [oAb[oq]], t * 512, 512)

        def mixer_dsa(l, s):
            with ExitStack() as ph:
                kT2 = sb("ds_kT2", [128, S], BF16, ph)
                ikT2 = sb("ds_ikT2", [128, S], BF16, ph)
                vaug = sb("ds_vaug", [128, NT, 65], BF16, ph)
                iwt = sb("ds_iwt", [128, NT, 8], F32, ph)
                qT = sb("ds_qT", [128, 2, S], BF16, ph)
                iqT = sb("ds_iqT", [128, 4, S], BF16, ph)
                kTb, ikTb, vab, iwb, qTb, iqTb = (P.buf("kT2"), P.buf("ikT2"), P.buf("vaug"), P.buf("iwt"), P.buf("qT"), P.buf("iqT"))
                r1, w1 = load_wset(l, ["AQ0", "AQW0", "AQ1", "AQW1", "AK2", "AKW2"])
                r2, w2 = load_wset(l, ["IQ0", "IQW0", "IQ1", "IQW1", "IQ2", "IQW2"])
                r3, w3 = load_wset(l, ["IQ3", "IQW3", "IK2", "IKW2", "AVIW"])
                with ExitStack() as pa:
                    cosT = sb("ds_cos", [128, S], BF16, pa)
                    sinT = sb("ds_sin", [128, S], BF16, pa)
                    tbl = P.buf("ropetab")
                    P.dma("pool", cosT[:], cd["cosT"], misc_sem, writes=[tbl])
                    P.dma("pool", sinT[:], cd["sinT"], misc_sem, writes=[tbl])
                    t1 = sb("ds_t1", [128, 512], F32, pa)
                    t2 = sb("ds_t2", [128, 512], F32, pa)
                    t1b, t2b = P.buf("t1"), P.buf("t2")
                    x1 = sb("ds_x1", [128, 512], F32, pa)
                    x2 = sb("ds_x2", [128, 512], F32, pa)
                    xq = sb("ds_xq", [128, 512], BF16, pa)
                    rsn = sb("ds_rsn", [128, 512], F32, pa)
                    x1b, x2b, xqb, rsnb = P.buf("x1"), P.buf("x2"), P.buf("xq"), P.buf("rsn")

                    def rope_comb(dst_ap, dstbuf, a_v, b_v, t):
                        sl = slice(t * 512, (t + 1) * 512)
                        P.op("dve", lambda e: e.tensor_tensor(out=t1[:], in0=a_v.ap, in1=cosT[:, sl], op=ALU.mult), reads=[a_v, tbl], writes=[t1b])
                        P.op("dve", lambda e: e.tensor_tensor(out=t2[:], in0=b_v.ap, in1=sinT[:, sl], op=ALU.mult), reads=[b_v, tbl], writes=[t2b])
                        P.op("dve", lambda e: e.tensor_tensor(out=dst_ap, in0=t1[:], in1=t2[:], op=ALU.add), reads=[t1b, t2b], writes=[dstbuf])

                    for t in range(4):
                        sl = slice(t * 512, (t + 1) * 512)
                        for sq_ in range(2):
                            pa_ = proj_fm(w1[f"AQ{sq_}"], r1, t)
                            pb_ = proj_fm(w1[f"AQW{sq_}"], r1, t)
                            rope_comb(qT[:, sq_, sl], qTb, pa_, pb_, t)
                        pa_ = proj_fm(w1["AK2"], r1, t)
                        pb_ = proj_fm(w1["AKW2"], r1, t)
                        rope_comb(kT2[:, sl], kTb, pa_, pb_, t)
                        for sq_ in range(4):
                            rr, ww = (r2, w2) if sq_ < 3 else (r3, w3)
                            pa_ = proj_fm(ww[f"IQ{sq_}"], rr, t)
                            pb_ = proj_fm(ww[f"IQW{sq_}"], rr, t)
                            rope_comb(iqT[:, sq_, sl], iqTb, pa_, pb_, t)
                        pa_ = proj_fm(w3["IK2"], r3, t)
                        pb_ = proj_fm(w3["IKW2"], r3, t)
                        P.op("act", lambda e: e.activation(out=x1[:], in_=pa_.ap, func=AF.Copy), reads=[pa_], writes=[x1b])
                        P.op("act", lambda e: e.activation(out=x2[:], in_=pb_.ap, func=AF.Copy), reads=[pb_], writes=[x2b])
                        P.op("act", lambda e: e.activation(out=xq[:], in_=x1[:], func=AF.Copy), reads=[x1b], writes=[xqb])
                        pmn = psum()
                        P.op("pe", lambda e: e.matmul(pmn.ap, lhsT=ones64b_t[:], rhs=xq[:], start=True, stop=True), reads=[xqb, cst], writes=[pmn])
                        P.op("dve", lambda e: e.tensor_tensor(out=x1[:], in0=x1[:], in1=pmn.ap, op=ALU.subtract), reads=[x1b, pmn], writes=[x1b])
                        P.op("dve", lambda e: e.tensor_tensor(out=x2[:], in0=x2[:], in1=pmn.ap, op=ALU.subtract), reads=[x2b, pmn], writes=[x2b])
                        P.op("act", lambda e: e.activation(out=xq[:], in_=x1[:], func=AF.Square), reads=[x1b], writes=[xqb])
                        pvr = psum()
                        P.op("pe", lambda e: e.matmul(pvr.ap, lhsT=ones64b_t[:], rhs=xq[:], start=True, stop=True), reads=[xqb, cst], writes=[pvr])
                        P.op("act", lambda e: e.activation(out=rsn[:], in_=pvr.ap, func=AF.Sqrt, bias=eps_t[:], scale=1.0), reads=[pvr, cst], writes=[rsnb])
                        P.op("dve", lambda e: e.reciprocal(out=rsn[:], in_=rsn[:]), reads=[rsnb], writes=[rsnb])
                        P.op("dve", lambda e: e.scalar_tensor_tensor(out=x1[:], in0=x1[:], scalar=kn_t[:, l, 0:1], in1=rsn[:], op0=ALU.mult, op1=ALU.mult),
                             reads=[x1b, rsnb, cst], writes=[x1b])
                        P.op("act", lambda e: e.activation(out=x1[:], in_=x1[:], func=AF.Identity, bias=kn_t[:, l, 1:2], scale=1.0), reads=[x1b, cst], writes=[x1b])
                        P.op("dve", lambda e: e.scalar_tensor_tensor(out=x2[:], in0=x2[:], scalar=kn_t[:, l, 2:3], in1=rsn[:], op0=ALU.mult, op1=ALU.mult),
                             reads=[x2b, rsnb, cst], writes=[x2b])
                        P.op("act", lambda e: e.activation(out=x2[:], in_=x2[:], func=AF.Identity, bias=kn_t[:, l, 3:4], scale=1.0), reads=[x2b, cst], writes=[x2b])
                        rope_comb(ikT2[:, sl], ikTb, V(x1[:], x1b), V(x2[:], x2b), t)
                    P.op("dve", lambda e: e.memset(vaug[:, :, 64:65], 1.0), writes=[vab])
                    for tt in range(NT):
                        pv = proj_tm(w3["AVIW"], ring_b[r3], tt, 72)
                        P.op("act", lambda e, tt=tt: e.activation(out=vaug[:, tt, 0:64], in_=pv.ap[:, 0:64], func=AF.Copy), reads=[pv], writes=[vab])
                        P.op("act", lambda e, tt=tt: e.activation(out=iwt[:, tt, :], in_=pv.ap[:, 64:72], func=AF.Copy, scale=IWSCALE),
                             reads=[pv], writes=[iwb])
                with ExitStack() as pb:
                    acc = sb("ds_acc", [128, S], F32, pb)
                    mask = sb("ds_mask", [128, S], BF16, pb)
                    junk = sb("ds_junk", [128, S], F32, pb)
                    junkb = P.buf("junk")
                    rl = [sb(f"ds_rl{i}", [128, 512], F32, pb) for i in range(2)]
                    PT = [sb(f"ds_PT{i}", [128, 4, 128], BF16, pb) for i in range(2)]
                    otok = sb("ds_otok", [128, 4, 64], BF16, pb)
                    oBt = sb("ds_oBt", [128, 2, 128], BF16, pb)
                    sm_ = sb("ds_small", [128, 8], F32, pb)
                    HWt = sb("ds_HW", [128, NIT + 1], F32, pb)
                    HW2 = sb("ds_HW2", [128, NIT + 1], F32, pb)
                    rden = sb("ds_rden", [128, 4], F32, pb)
                    accb, maskb, rlb, PTb, otb, oBb, smb_, rdb = (P.buf("acc"), P.buf("mask"), [P.buf("rl0"), P.buf("rl1")],
                                                                 [P.buf("PT0"), P.buf("PT1")], P.buf("otok"), P.buf("oBt"), P.buf("small"), P.buf("rden"))
                    r4, wo = load_wout(l, 256, 2)
                    rlq = [0]
                    ptq = [0]
                    for j in range(NT):
                        W = 128 * (j + 1)
                        qsl = slice(j * 128, (j + 1) * 128)
                        nkc = (W + 511) // 512
                        for kc in range(nkc):
                            Wc = min(512, W - kc * 512)
                            ksl = slice(kc * 512, kc * 512 + Wc)
                            for h in range(8):
                                part, slot = h % 2, h // 2
                                pl = psum()
                                P.op("pe", lambda e, part=part, slot=slot: e.matmul(
                                    pl.ap[:, 0:Wc], lhsT=iqT[part * 64:(part + 1) * 64, slot, qsl], rhs=ikT2[part * 64:(part + 1) * 64, ksl],
                                    start=True, stop=True), reads=[iqTb, ikTb], writes=[pl])
                                q = rlq[0] % 2
                                rlq[0] += 1
                                P.op("act", lambda e, q=q: e.activation(out=rl[q][:, 0:Wc], in_=pl.ap[:, 0:Wc], func=AF.Relu), reads=[pl], writes=[rlb[q]])
                                if h == 0:
                                    P.op("dve", lambda e, q=q: e.tensor_scalar(out=acc[:, ksl], in0=rl[q][:, 0:Wc], scalar1=iwt[:, j, 0:1], scalar2=None, op0=ALU.mult),
                                         reads=[rlb[q], iwb], writes=[accb])
                                else:
                                    P.op("dve", lambda e, q=q, h=h: e.scalar_tensor_tensor(
                                        out=acc[:, ksl], in0=rl[q][:, 0:Wc], scalar=iwt[:, j, h:h + 1], in1=acc[:, ksl], op0=ALU.mult, op1=ALU.add),
                                        reads=[rlb[q], iwb, accb], writes=[accb])
                        P.op("dve", lambda e: e.tensor_tensor(out=acc[:, qsl], in0=acc[:, qsl], in1=causb_t[:], op=ALU.add), reads=[accb, cst], writes=[accb])
                        if j >= 2:
                            P.op("dve", lambda e: e.tensor_reduce(out=sm_[:, 0:1], in_=acc[:, 0:W - 128], axis=AX.X, op=ALU.min), reads=[accb], writes=[smb_])
                            P.op("dve", lambda e: e.tensor_reduce(out=sm_[:, 1:2], in_=acc[:, 0:W], axis=AX.X, op=ALU.max), reads=[accb, smb_], writes=[smb_])
                            P.op("dve", lambda e: e.tensor_tensor(out=sm_[:, 2:3], in0=sm_[:, 1:2], in1=sm_[:, 0:1], op=ALU.subtract), reads=[smb_], writes=[smb_])
                            P.op("dve", lambda e: e.tensor_scalar(out=HWt[:], in0=pow2_t[:], scalar1=sm_[:, 2:3], scalar2=None, op0=ALU.mult), reads=[smb_, cst], writes=[smb_])
                            P.op("dve", lambda e: e.tensor_scalar(out=HW2[:], in0=HWt[:], scalar1=2.0, scalar2=None, op0=ALU.mult), reads=[smb_], writes=[smb_])
                            P.op("dve", lambda e: e.tensor_tensor(out=sm_[:, 3:4], in0=sm_[:, 0:1], in1=HWt[:, 0:1], op=ALU.add), reads=[smb_], writes=[smb_])
                            for n in range(NIT):
                                P.op("dve", lambda e: e.tensor_scalar(out=junk[:, 0:W], in0=acc[:, 0:W], scalar1=sm_[:, 3:4], scalar2=None,
                                                                      op0=ALU.is_ge, op1=ALU.add, accum_out=sm_[:, 4:5]),
                                     reads=[accb, smb_], writes=[junkb, smb_])
                                P.op("dve", lambda e, n=n: e.tensor_scalar(out=sm_[:, 5:6], in0=sm_[:, 4:5], scalar1=255.5, scalar2=HW2[:, n + 1:n + 2],
                                                                           op0=ALU.is_ge, op1=ALU.mult), reads=[smb_], writes=[smb_])
                                P.op("dve", lambda e, n=n: e.scalar_tensor_tensor(out=sm_[:, 3:4], in0=sm_[:, 5:6], scalar=HWt[:, n + 1:n + 2], in1=sm_[:, 3:4],
                                                                                  op0=ALU.subtract, op1=ALU.add), reads=[smb_], writes=[smb_])
                            P.op("dve", lambda e: e.tensor_tensor(out=sm_[:, 6:7], in0=sm_[:, 3:4], in1=HWt[:, NIT:NIT + 1], op=ALU.subtract), reads=[smb_], writes=[smb_])
                            thr = sm_[:, 6:7]
                        else:
                            thr = negthr_t[:]
                        P.op("dve", lambda e: e.tensor_scalar(out=mask[:, 0:W], in0=acc[:, 0:W], scalar1=thr, scalar2=None, op0=ALU.is_ge),
                             reads=[accb, smb_, cst], writes=[maskb])
                        po = psum(hold=True)
                        po3 = po.ap[:, 0:260].rearrange("p (h e) -> p h e", e=65)
                        first = True
                        for kb in range(j + 1):
                            kbs = slice(kb * 128, (kb + 1) * 128)
                            pmT = psum()
                            pmTb = bf(pmT)
                            P.op("pe", lambda e: e.transpose(pmTb[:, 0:128], mask[:, kbs], identb_t[:]), reads=[maskb, cst], writes=[pmT])
                            psts = [psum(), psum()]
                            q = ptq[0] % 2
                            ptq[0] += 1
                            for part in range(2):
                                for slot in range(2):
                                    P.op("pe", lambda e, part=part, slot=slot: e.matmul(
                                        psts[part].ap[:, slot * 128:(slot + 1) * 128], lhsT=kT2[part * 64:(part + 1) * 64, kbs],
                                        rhs=qT[part * 64:(part + 1) * 64, slot, qsl], start=True, stop=True),
                                        reads=[kTb, qTb], writes=[psts[part]], inc=(slot == 1))
                                P.op("act", lambda e, q=q, part=part: e.activation(
                                    out=PT[q][:, part:4:2, :], in_=psts[part].ap[:, 0:256].rearrange("p (h t) -> p h t", t=128), func=AF.Exp, scale=0.125),
                                    reads=[psts[part]], writes=[PTb[q]])
                            P.op("dve", lambda e, q=q: e.tensor_tensor(out=PT[q][:], in0=PT[q][:], in1=pmTb[:, 0:128].unsqueeze(1).to_broadcast([128, 4, 128]), op=ALU.mult),
                                 reads=[PTb[q], pmT], writes=[PTb[q]])
                            for h in range(4):
                                lastmm = (kb == j and h == 3)
                                P.op("pe", lambda e, h=h, q=q, first=first, lastmm=lastmm: e.matmul(
                                    po3[:, h, :], lhsT=PT[q][:, h, :], rhs=vaug[:, kb, :], start=first, stop=lastmm, skip_group_check=True),
                                    reads=[PTb[q], vab], writes=[po], inc=(h == 3))
                                first = False
                        P.op("dve", lambda e: e.reciprocal(out=rden[:], in_=po3[:, :, 64]), reads=[po], writes=[rdb])
                        P.op("dve", lambda e: e.tensor_tensor(out=otok[:], in0=po3[:, :, 0:64], in1=rden[:].unsqueeze(2).to_broadcast([128, 4, 64]), op=ALU.mult),
                             reads=[po, rdb], writes=[otb])
                        psum_release(po)
                        pmo = psum()
                        pmob = bf(pmo)
                        of = otok[:].rearrange("p h d -> p (h d)")
                        for sl_ in range(2):
                            P.op("pe", lambda e, sl_=sl_: e.transpose(pmob[:, sl_ * 128:(sl_ + 1) * 128], of[:, sl_ * 128:(sl_ + 1) * 128], identb_t[:]),
                                 reads=[otb, cst], writes=[pmo], inc=(sl_ == 1))
                        P.op("dve", lambda e: e.tensor_copy(out=oBt[:], in_=pmob[:, 0:256].rearrange("p (a b) -> p a b", b=128)),
                             reads=[pmo], writes=[oBb])
                        add_wout(l, s, r4, wo, 2, lambda jj: oBt[:, jj, :], [oBb], j * 128, 128)

        def mixer_ssd(l, s):
            with ExitStack() as ph:
                wsdt = sb("ss_wsdt", [128, 8, 8], BF16, ph)
                dtb = sb("ss_dt", [128, NT, 8], F32, ph)
                dA = sb("ss_dA", [128, NT, 8], F32, ph)
                wsb, dtbb = P.buf("wsdt"), P.buf("dt")
                P.dma("pool", wsdt[:], win2_d[l][:, W2OFF["SDT"]:W2OFF["SDT"] + 8].rearrange("(k p) c -> p k c", p=128), misc_sem, writes=[wsb])
                for tt in range(NT):
                    pm = proj_tm(wsdt[:], wsb, tt, 8)
                    P.op("dve", lambda e, tt=tt: e.tensor_tensor(out=dtb[:, tt, :], in0=pm.ap[:, 0:8], in1=dtb_t[:, l, :], op=ALU.add),
                         reads=[pm, cst], writes=[dtbb])
                P.op("act", lambda e: e.activation(out=dtb[:], in_=dtb[:], func=AF.Exp), reads=[dtbb], writes=[dtbb])
                P.op("act", lambda e: e.activation(out=dtb[:], in_=dtb[:], func=AF.Ln, bias=one_t[:], scale=1.0), reads=[dtbb, cst], writes=[dtbb])
                P.op("dve", lambda e: e.tensor_tensor(out=dA[:], in0=dtb[:], in1=nega_t[:, l, :].unsqueeze(1).to_broadcast([128, NT, 8]), op=ALU.mult),
                     reads=[dtbb, cst], writes=[dtbb])
                for g in range(2):
                    with ExitStack() as pg_:
                        zs = sb("ss_zs", [128, 2, S], BF16, pg_)
                        xsT = sb("ss_xsT", [128, 2, S], BF16, pg_)
                        BT = sb("ss_BT", [128, S], BF16, pg_)
                        CT = sb("ss_CT", [128, S], BF16, pg_)
                        yz = sb("ss_yz", [128, 2, S], BF16, pg_)
                        zsb, xsb, BTb, CTb = P.buf("zs"), P.buf("xsT"), P.buf("BT"), P.buf("CT")
                        yzb = [[P.buf(f"yz{i}{t}") for t in range(4)] for i in range(2)]
                        r, wv = load_wset(l, [f"Z{g}0", f"Z{g}1", f"XS{g}0", f"XS{g}1", f"B{g}", f"C{g}"])
                        with ExitStack() as pa:
                            pre = sb("ss_pre", [128, 3 + S], F32, pa)
                            cac = sb("ss_cac", [128, S], F32, pa)
                            preb, cacb = P.buf("pre"), P.buf("cac")
                            for i in range(2):
                                for t in range(4):
                                    pm = proj_fm(wv[f"Z{g}{i}"], r, t)
                                    P.op("act", lambda e, i=i, t=t: e.activation(out=zs[:, i, t * 512:(t + 1) * 512], in_=pm.ap, func=AF.Silu),
                                         reads=[pm], writes=[zsb])
                            blocks = [(f"XS{g}0", 2 * g, xsT[:, 0, :], xsb), (f"XS{g}1", 2 * g + 1, xsT[:, 1, :], xsb),
                                      (f"B{g}", 4 + g, BT[:], BTb), (f"C{g}", 6 + g, CT[:], CTb)]
                            for nm, ch, dst, dstb in blocks:
                                P.op("dve", lambda e: e.memset(pre[:, 0:3], 0.0), writes=[preb])
                                for t in range(4):
                                    pm = proj_fm(wv[nm], r, t)
                                    P.op("act", lambda e, t=t: e.activation(out=pre[:, 3 + t * 512:3 + (t + 1) * 512], in_=pm.ap, func=AF.Copy),
                                         reads=[pm], writes=[preb])
                                P.op("dve", lambda e, ch=ch: e.tensor_scalar(out=cac[:], in0=pre[:, 3:3 + S], scalar1=cw_t[:, l, ch, 3:4], scalar2=cb_t[:, l, ch:ch + 1],
                                                                             op0=ALU.mult, op1=ALU.add), reads=[preb, cst], writes=[cacb])
                                for jt in range(3):
                                    P.op("dve", lambda e, ch=ch, jt=jt: e.scalar_tensor_tensor(out=cac[:], in0=pre[:, jt:jt + S], scalar=cw_t[:, l, ch, jt:jt + 1], in1=cac[:],
                                                                                               op0=ALU.mult, op1=ALU.add), reads=[preb, cacb, cst], writes=[cacb])
                                P.op("act", lambda e, dst=dst: e.activation(out=dst, in_=cac[:], func=AF.Silu), reads=[cacb], writes=[dstb])
                        with ExitStack() as pb:
                            def two(name, shape, dt_):
                                return [sb(f"{name}{q_}", shape, dt_, pb) for q_ in range(2)], [P.buf(f"{name}{q_}") for q_ in range(2)]
                            rhsU2, rhsUb2 = two("ss_rhsU", [128, 4, 128], F32)
                            Eseg2, Esegb2 = two("ss_Eseg", [128, 4, 128], BF16)
                            Ebc2, Ebcb2 = two("ss_Ebc", [128, 4, 128], BF16)
                            smx2, smxb2 = two("ss_smx", [128, 8], F32)
                            CBm2, CBmb2 = two("ss_CBm", [128, 128], BF16)
                            MT2, MTb2 = two("ss_MT", [128, 4, 128], BF16)
                            CsT2, CsTb2 = two("ss_CsT", [128, 4, 128], BF16)
                            xdt2, xdtb2 = two("ss_xdt", [128, 4, 64], BF16)
                            xdtd2, xdtdb2 = two("ss_xdtd", [128, 4, 64], BF16)
                            Btok2, Btokb2 = two("ss_Btok", [128, 128], BF16)
                            ytmp2, ytmpb2 = two("ss_ytmp", [128, 2, 128], F32)
                            prevb162, prevbb2 = two("ss_prevb", [128, 4, 64], BF16)
                            prev = sb("ss_prev", [128, 4, 64], F32, pb)
                            prevb = P.buf("prev")
                            st = rms_state(pb, "ss", 2, ones256_t)
                            P.op("dve", lambda e: e.memset(prev[:], 0.0), writes=[prevb])
                            for c in range(NT):
                                csl = slice(c * 128, (c + 1) * 128)
                                dAc = dA[:, c, 4 * g:4 * g + 4]
                                q_ = c % 2
                                rhsU, rhsUb, Eseg, Esegb, Ebc, Ebcb, smx, smxb = rhsU2[q_], rhsUb2[q_], Eseg2[q_], Esegb2[q_], Ebc2[q_], Ebcb2[q_], smx2[q_], smxb2[q_]
                                CBm, CBmb, MT, MTb, CsT, CsTb = CBm2[q_], CBmb2[q_], MT2[q_], MTb2[q_], CsT2[q_], CsTb2[q_]
                                xdt, xdtb, xdtd, xdtdb, Btok, Btokb, ytmp, ytmpb = xdt2[q_], xdtb2[q_], xdtd2[q_], xdtdb2[q_], Btok2[q_], Btokb2[q_], ytmp2[q_], ytmpb2[q_]
                                prevb16, prevbb = prevb162[q_], prevbb2[q_]
                                prevb16n, prevbbn = prevb162[1 - q_], prevbb2[1 - q_]
                                P.op("dve", lambda e: e.tensor_tensor(out=rhsU[:], in0=Uincl_t[:].unsqueeze(1).to_broadcast([128, 4, 128]),
                                                                      in1=dAc.unsqueeze(2).to_broadcast([128, 4, 128]), op=ALU.mult),
                                     reads=[cst, dtbb], writes=[rhsUb])
                                rf = rhsU[:].rearrange("p a b -> p (a b)")
                                pseg = psum()
                                P.op("pe", lambda e: e.matmul(pseg.ap, lhsT=Lgt_t[:], rhs=rf, start=True, stop=True), reads=[rhsUb, cst], writes=[pseg])
                                pacs = psum()
                                P.op("pe", lambda e: e.matmul(pacs.ap, lhsT=onesf_t[:], rhs=rf, start=True, stop=True), reads=[rhsUb, cst], writes=[pacs])
                                psm = psum()
                                P.op("pe", lambda e: e.matmul(psm.ap[:, 0:4], lhsT=Lgt_t[:], rhs=dAc, start=True, stop=True), reads=[dtbb, cst], writes=[psm], inc=False)
                                P.op("pe", lambda e: e.matmul(psm.ap[:, 4:8], lhsT=onesf_t[:], rhs=dAc, start=True, stop=True), reads=[dtbb, cst], writes=[psm])
                                P.op("act", lambda e: e.activation(out=Eseg[:], in_=pseg.ap.rearrange("p (a b) -> p a b", b=128), func=AF.Exp), reads=[pseg], writes=[Esegb])
                                P.op("act", lambda e: e.activation(out=Ebc[:], in_=pacs.ap.rearrange("p (a b) -> p a b", b=128), func=AF.Exp), reads=[pacs], writes=[Ebcb])
                                P.op("act", lambda e: e.activation(out=smx[:], in_=psm.ap[:, 0:8], func=AF.Exp), reads=[psm], writes=[smxb])
                                pcb = psum()
                                P.op("pe", lambda e: e.matmul(pcb.ap[:, 0:128], lhsT=BT[:, csl], rhs=CT[:, csl], start=True, stop=True), reads=[BTb, CTb], writes=[pcb])
                                P.op("dve", lambda e: e.tensor_tensor(out=CBm[:], in0=pcb.ap[:, 0:128], in1=causT_t[:], op=ALU.mult), reads=[pcb, cst], writes=[CBmb])
                                P.op("dve", lambda e: e.tensor_tensor(out=MT[:], in0=Eseg[:], in1=CBm[:].unsqueeze(1).to_broadcast([128, 4, 128]), op=ALU.mult),
                                     reads=[Esegb, CBmb], writes=[MTb])
                                P.op("dve", lambda e: e.tensor_tensor(out=CsT[:], in0=Ebc[:], in1=CT[:, csl].unsqueeze(1).to_broadcast([128, 4, 128]), op=ALU.mult),
                                     reads=[Ebcb, CTb], writes=[CsTb])
                                pxt = psum()
                                pxtb = bf(pxt)
                                for i in range(2):
                                    P.op("pe", lambda e, i=i: e.transpose(pxtb[:, i * 128:(i + 1) * 128], xsT[:, i, csl], identb_t[:]), reads=[xsb, cst], writes=[pxt], inc=False)
                                P.op("pe", lambda e: e.transpose(pxtb[:, 256:384], BT[:, csl], identb_t[:]), reads=[BTb, cst], writes=[pxt])
                                P.op("dve", lambda e: e.tensor_tensor(out=xdt[:], in0=pxtb[:, 0:256].rearrange("p (a b) -> p a b", b=64),
                                                                      in1=dtb[:, c, 4 * g:4 * g + 4].unsqueeze(2).to_broadcast([128, 4, 64]), op=ALU.mult),
                                     reads=[pxt, dtbb], writes=[xdtb])
                                P.op("dve", lambda e: e.tensor_tensor(out=xdtd[:], in0=xdt[:], in1=smx[:, 0:4].unsqueeze(2).to_broadcast([128, 4, 64]), op=ALU.mult),
                                     reads=[xdtb, smxb], writes=[xdtdb])
                                P.op("dve", lambda e: e.tensor_copy(out=Btok[:], in_=pxtb[:, 256:384]), reads=[pxt], writes=[Btokb])
                                py = psum()
                                for rr in range(4):
                                    i, part = rr // 2, rr % 2
                                    P.op("pe", lambda e, rr=rr, i=i, part=part: e.matmul(
                                        py.ap[part * 64:(part + 1) * 64, i * 128:(i + 1) * 128], lhsT=xdt[:, rr, :], rhs=MT[:, rr, :], start=True, stop=(c == 0)),
                                        reads=[xdtb, MTb], writes=[py], inc=(c == 0))
                                    if c > 0:
                                        P.op("pe", lambda e, rr=rr, i=i, part=part: e.matmul(
                                            py.ap[part * 64:(part + 1) * 64, i * 128:(i + 1) * 128], lhsT=prevb16[:, rr, :], rhs=CsT[:, rr, :], start=False, stop=True),
                                            reads=[prevbb, CsTb], writes=[py])
                                for i in range(2):
                                    P.op("dve", lambda e, i=i: e.scalar_tensor_tensor(out=ytmp[:, i, :], in0=xsT[:, i, csl], scalar=dsk_t[:, l, 2 * g + i:2 * g + i + 1],
                                                                                      in1=py.ap[:, i * 128:(i + 1) * 128], op0=ALU.mult, op1=ALU.add),
                                         reads=[xsb, py, cst], writes=[ytmpb])
                                P.op("dve", lambda e: e.tensor_tensor(out=yz[:, :, csl], in0=ytmp[:], in1=zs[:, :, csl], op=ALU.mult),
                                     reads=[ytmpb, zsb], writes=[yzb[0][c // 4], yzb[1][c // 4]])
                                pstt = psum()
                                P.op("pe", lambda e: e.matmul(pstt.ap[:, 0:256], lhsT=Btok[:], rhs=xdtd[:].rearrange("p a b -> p (a b)"), start=True, stop=True),
                                     reads=[Btokb, xdtdb], writes=[pstt])
                                P.op("dve", lambda e: e.tensor_tensor(out=prev[:], in0=prev[:], in1=smx[:, 4:8].unsqueeze(2).to_broadcast([128, 4, 64]), op=ALU.mult),
                                     reads=[prevb, smxb], writes=[prevb])
                                P.op("dve", lambda e: e.tensor_tensor(out=prev[:], in0=prev[:], in1=pstt.ap[:, 0:256].rearrange("p (a b) -> p a b", b=64), op=ALU.add),
                                     reads=[prevb, pstt], writes=[prevb])
                                P.op("act", lambda e: e.activation(out=prevb16n[:], in_=prev[:], func=AF.Copy), reads=[prevb], writes=[prevbbn])
                            r2, wo = load_wout(l, 512 + g * 256, 2)
                            for t in range(4):
                                tsl = slice(t * 512, (t + 1) * 512)
                                vs = [V(yz[:, i, tsl], yzb[i][t]) for i in range(2)]
                                rms_run(st, vs, vs, lambda i: ssmg_t[:, l, 2 * g + i:2 * g + i + 1], None)
                                add_wout(l, s, r2, wo, 2, lambda jj: yz[:, jj, tsl], [yzb[0][t], yzb[1][t]], t * 512, 512)

        xin_sem = [P.dma_sem(f"xin{i}") for i in range(2)]
        out_sem = [P.dma_sem(f"xout{i}") for i in range(2)]
        for s in range(NSQ):
            with ExitStack() as ph:
                st_t = [sb(f"xin{i}", [128, D], F32, ph) for i in range(2)]
                st_b = [P.buf(f"xin{i}") for i in range(2)]
                for tt in range(NT):
                    i = tt % 2
                    P.dma("sp", st_t[i][:], x_d[s, tt * 128:(tt + 1) * 128, :], xin_sem[i], writes=[st_b[i]])
                    for half in range(2):
                        pm = psum()
                        for cc in range(4):
                            c = half * 4 + cc
                            P.op("pe", lambda e, c=c, cc=cc, i=i: e.transpose(pm.ap[:, cc * 128:(cc + 1) * 128], st_t[i][:, c * 128:(c + 1) * 128], ident_t[:]),
                                 reads=[st_b[i], cst], writes=[pm], inc=(cc == 3))
                        bl = [xb[half * 4 + cc][tt // 4] for cc in range(4)]
                        if half == 0:
                            P.op("act", lambda e, half=half, tt=tt: e.activation(
                                out=xT_t[:, half * 4:(half + 1) * 4, tt * 128:(tt + 1) * 128],
                                in_=pm.ap.rearrange("p (c t) -> p c t", t=128), func=AF.Copy), reads=[pm], writes=bl)
                        else:
                            P.op("dve", lambda e, half=half, tt=tt: e.tensor_copy(
                                out=xT_t[:, half * 4:(half + 1) * 4, tt * 128:(tt + 1) * 128],
                                in_=pm.ap.rearrange("p (c t) -> p c t", t=128)), reads=[pm], writes=bl)
            for l in LAYERS:
                if cfg["ffn1"]:
                    ffn(l, s, 0)
                if MIX:
                    norm_mod(l, s, 1)
                    if "hg" in MIX:
                        for slot in range(2):
                            mixer_hg(l, s, slot)
                    if "dsa" in MIX:
                        mixer_dsa(l, s)
                    if "ssd" in MIX:
                        mixer_ssd(l, s)
                if cfg["ffn2"]:
                    ffn(l, s, 1)
            with ExitStack() as ph:
                st = rms_state(ph, "fin", 8, onesd_t)
                y_t = [sb(f"yfin{i}", [128, 8, 512], F32, ph) for i in range(2)]
                y_b = [P.buf(f"yfin{i}") for i in range(2)]
                o_t = [sb(f"otok{i}", [128, D], F32, ph) for i in range(2)]
                o_b = [P.buf(f"otok{i}") for i in range(2)]
                oq = 0
                for t in range(4):
                    yi = t % 2
                    rms_run(st, [xv(c, t) for c in range(8)], [V(y_t[yi][:, c, :], y_b[yi]) for c in range(8)],
                            lambda c: fng_t[:, c:c + 1], None)
                    for sub in range(4):
                        tt = t * 4 + sub
                        oi = oq % 2
                        oq += 1
                        for half in range(2):
                            pm = psum()
                            for cc in range(4):
                                c = half * 4 + cc
                                P.op("pe", lambda e, c=c, cc=cc, yi=yi, sub=sub: e.transpose(
                                    pm.ap[:, cc * 128:(cc + 1) * 128], y_t[yi][:, c, sub * 128:(sub + 1) * 128], ident_t[:]),
                                    reads=[y_b[yi], cst], writes=[pm], inc=(cc == 3))
                            if half == 0:
                                P.op("act", lambda e, oi=oi: e.activation(out=o_t[oi][:, 0:512], in_=pm.ap, func=AF.Copy),
                                     reads=[pm], writes=[o_b[oi]])
                            else:
                                P.op("dve", lambda e, oi=oi: e.tensor_copy(out=o_t[oi][:, 512:1024], in_=pm.ap),
                                     reads=[pm], writes=[o_b[oi]])
                        P.dma("sp", out_d[s, tt * 128:(tt + 1) * 128, :], o_t[oi][:], out_sem[oi], reads=[o_b[oi]])
        P.wait_all("sp")
        print("instructions emitted:", P.ninstr)
    return nc


def kernel(**inputs):
    inputs = {k: np.asarray(v, dtype=np.float32) for k, v in inputs.items()}
    nc = build_program(FULL_CFG)
    shared = _shared_inputs(inputs, FULL_CFG)
    in_maps = []
    for core in range(8):
        m = dict(shared)
        m.update(_host_inputs(inputs, core, FULL_CFG))
        in_maps.append(m)
    res = run_bass_kernel_spmd(nc, in_maps, core_ids=list(range(8)))
    out = np.concatenate([np.asarray(r["out"]).reshape(NSEQ, S, D) for r in res.results], axis=0)
    return out.astype(np.float32)
```

```python
import numpy as np
from contextlib import ExitStack
import concourse.bass as bass
import concourse.mybir as mybir
from concourse.bass_utils import run_bass_kernel_spmd

F32 = mybir.dt.float32
BF16 = mybir.dt.bfloat16
AF = mybir.ActivationFunctionType
ALU = mybir.AluOpType
AX = mybir.AxisListType

D = 1024
S = 2048
DEPTH = 2
DFF = 2816
NSEQ = 2
NT = S // 128
EPS = 1e-6
NEG = -1e30


class Buf:
    __slots__ = ("w", "r", "name")

    def __init__(self, name="", w=None):
        self.w = dict(w) if w else {}
        self.r = {}
        self.name = name


class V:
    __slots__ = ("ap", "buf")

    def __init__(self, ap, buf):
        self.ap = ap
        self.buf = buf

    def __getitem__(self, k):
        return V(self.ap[k], self.buf)


class Prog:
    def __init__(self, nc, es):
        self.nc = nc
        self.es = es
        self.E = dict(pe=nc.tensor, act=nc.scalar, dve=nc.vector, pool=nc.gpsimd, sp=nc.sync)
        self.sem = {}
        self.cnt = {}
        for e in ("pe", "act", "dve", "pool"):
            self.sem[e] = es.enter_context(nc.semaphore("s_" + e))
            self.cnt[e] = 0
        self.waited = {e: {} for e in self.E}
        self.ninstr = 0

    def dma_sem(self, name):
        key = "d_" + name
        self.sem[key] = self.es.enter_context(self.nc.semaphore("s_" + key))
        self.cnt[key] = 0
        return key

    def snapshot(self):
        return {k: v for k, v in self.cnt.items() if v > 0}

    def buf(self, name="", fresh=True):
        return Buf(name, self.snapshot() if fresh else None)

    def _emit_waits(self, eng, reads, writes, skipkey=None):
        need = {}
        for b in reads:
            for k, v in b.w.items():
                if v > need.get(k, 0):
                    need[k] = v
        for b in writes:
            for k, v in b.w.items():
                if k == skipkey:
                    continue
                if v > need.get(k, 0):
                    need[k] = v
            for k, v in b.r.items():
                if v > need.get(k, 0):
                    need[k] = v
        e = self.E[eng]
        wd = self.waited[eng]
        for k, v in need.items():
            if k == "pe" and eng == "pe":
                continue
            if wd.get(k, 0) >= v:
                continue
            wd[k] = v
            e.wait_ge(self.sem[k], v)
            self.ninstr += 1

    def op(self, eng, fn, reads=(), writes=(), inc=True):
        reads = [x.buf if isinstance(x, V) else x for x in reads]
        writes = [x.buf if isinstance(x, V) else x for x in writes]
        self._emit_waits(eng, reads, writes)
        ins = fn(self.E[eng])
        self.ninstr += 1
        if inc:
            self.cnt[eng] += 1
            ins.then_inc(self.sem[eng], 1)
            t = self.cnt[eng]
        else:
            t = self.cnt[eng] + 1
        for b in reads:
            if t > b.r.get(eng, 0):
                b.r[eng] = t
        for b in writes:
            b.w = {eng: t}
            b.r = {}
        return ins

    def dma(self, q, out, in_, semkey, reads=(), writes=(), **kw):
        reads = [x.buf if isinstance(x, V) else x for x in reads]
        writes = [x.buf if isinstance(x, V) else x for x in writes]
        self._emit_waits(q, reads, writes, skipkey=semkey)
        ins = self.E[q].dma_start(out=out, in_=in_, **kw)
        self.ninstr += 1
        self.cnt[semkey] += 16
        ins.then_inc(self.sem[semkey], 16)
        t = self.cnt[semkey]
        for b in reads:
            if t > b.r.get(semkey, 0):
                b.r[semkey] = t
        for b in writes:
            if semkey in b.w and len(b.w) == 1:
                b.w[semkey] = t
            else:
                b.w = {semkey: t}
            b.r = {}
        return ins

    def wait_all(self, eng):
        e = self.E[eng]
        for k, v in self.snapshot().items():
            if self.waited[eng].get(k, 0) >= v:
                continue
            self.waited[eng][k] = v
            e.wait_ge(self.sem[k], v)


O_HQ, O_HF, O_HI, O_HG, O_AQ, O_AK, O_AV, O_IQ, O_IK, O_IW, O_SZ, O_SX, O_SDT = (
    0, 256, 512, 768, 1024, 1280, 1344, 1408, 1920, 1984, 1992, 2504, 3528)
IWSCALE = float(8 ** -0.5 * 64 ** -0.5)
NIT = 11


def _win2_layout():
    off = {}
    ncol = {}
    cols = []

    def add(name, idx):
        idx = list(idx)
        off[name] = len(cols)
        ncol[name] = len(idx)
        cols.extend(idx)

    def sw(base, n):
        out = []
        for h in range(n // 64):
            b = base + h * 64
            out += list(range(b + 32, b + 64)) + list(range(b, b + 32))
        return out

    for s in range(2):
        add(f"HQ{s}", range(O_HQ + s * 128, O_HQ + (s + 1) * 128))
        add(f"HF{s}", range(O_HF + s * 128, O_HF + (s + 1) * 128))
        add(f"HG{s}", range(O_HG + s * 128, O_HG + (s + 1) * 128))
        add(f"HI{s}", range(O_HI + s * 128, O_HI + (s + 1) * 128))
    for s in range(2):
        add(f"AQ{s}", range(O_AQ + s * 128, O_AQ + (s + 1) * 128))
        add(f"AQW{s}", sw(O_AQ + s * 128, 128))
    add("AK2", list(range(O_AK, O_AK + 64)) * 2)
    add("AKW2", sw(O_AK, 64) * 2)
    for s in range(4):
        add(f"IQ{s}", range(O_IQ + s * 128, O_IQ + (s + 1) * 128))
        add(f"IQW{s}", sw(O_IQ + s * 128, 128))
    add("IK2", list(range(O_IK, O_IK + 64)) * 2)
    add("IKW2", sw(O_IK, 64) * 2)
    add("AVIW", list(range(O_AV, O_AV + 64)) + list(range(O_IW, O_IW + 8)))
    for g in range(2):
        add(f"Z{g}0", range(O_SZ + (2 * g) * 128, O_SZ + (2 * g + 1) * 128))
        add(f"Z{g}1", range(O_SZ + (2 * g + 1) * 128, O_SZ + (2 * g + 2) * 128))
        add(f"XS{g}0", range(O_SX + (2 * g) * 128, O_SX + (2 * g + 1) * 128))
        add(f"XS{g}1", range(O_SX + (2 * g + 1) * 128, O_SX + (2 * g + 2) * 128))
        add(f"B{g}", range(O_SX + 512 + g * 128, O_SX + 512 + (g + 1) * 128))
        add(f"C{g}", range(O_SX + 768 + g * 128, O_SX + 768 + (g + 1) * 128))
    add("SDT", range(O_SDT, O_SDT + 8))
    return np.array(cols, dtype=np.int64), off, ncol


W2COLS, W2OFF, W2N = _win2_layout()
NC2 = len(W2COLS)


def _consts():
    c = {}
    p = np.arange(128)
    c["ident"] = np.eye(128, dtype=np.float32)
    c["ones_d"] = np.full((128, 128), 1.0 / 1024.0, np.float32)
    c["ones_256"] = np.full((128, 128), 1.0 / 256.0, np.float32)
    c["ones_f"] = np.ones((128, 128), np.float32)
    o64 = np.zeros((128, 128), np.float32)
    o64[:64, :64] = 1.0 / 64.0
    o64[64:, 64:] = 1.0 / 64.0
    c["ones_64b"] = o64
    s_, t_ = p[:, None], p[None, :]
    c["bdmask"] = ((s_ // 64 == t_ // 64) & (t_ >= s_)).astype(np.float32)
    c["causT"] = (t_ >= s_).astype(np.float32)
    same = (s_ // 64 == t_ // 64)
    c["maskD2"] = (same & (t_ >= s_) & (s_ % 64 >= 32) & (t_ % 64 >= 32)).astype(np.float32)
    c["maskX"] = (same & (s_ % 64 < 32) & (t_ % 64 >= 32)).astype(np.float32)
    c["causbias"] = np.where(t_ <= s_, 0.0, NEG).astype(np.float32)
    c["Lgt"] = (s_ > t_).astype(np.float32)
    c["Uincl"] = (s_ <= t_).astype(np.float32)
    tt = np.arange(S)
    c["rmask"] = np.broadcast_to((tt % 64 != 0).astype(np.float32)[None, :], (128, S)).copy()
    inv_freq = 1.0 / (10000.0 ** (np.arange(0, 64, 2, dtype=np.float32) / 64.0))
    ang = tt.astype(np.float32)[:, None] * inv_freq[None, :]
    cos = np.cos(ang).astype(np.float32).T
    sin = np.sin(ang).astype(np.float32).T
    c["cosT"] = np.concatenate([cos, cos, cos, cos], axis=0)
    c["sinT"] = np.concatenate([-sin, sin, -sin, sin], axis=0)
    c["pow2"] = np.broadcast_to((2.0 ** -(np.arange(NIT + 1, dtype=np.float32) + 1.0))[None, :], (128, NIT + 1)).copy()
    return c


CONST_SHAPES = dict(ident=[128, 128], ones_d=[128, 128], ones_256=[128, 128], ones_f=[128, 128], ones_64b=[128, 128],
                    bdmask=[128, 128], maskD2=[128, 128], maskX=[128, 128], causT=[128, 128], causbias=[128, 128], Lgt=[128, 128], Uincl=[128, 128],
                    rmask=[128, S], cosT=[128, S], sinT=[128, S], pow2=[128, NIT + 1])

FULL_CFG = dict(nseq=2, layers=(0, 1), ffn1=True, mix=("hg", "dsa", "ssd"), ffn2=True, hostmod=False)


def _fm(v, nchunk):
    v = np.asarray(v, np.float32)
    lead = v.shape[:-1]
    r = v.reshape(lead + (nchunk, 128))
    r = np.moveaxis(r, -1, 0)
    return np.ascontiguousarray(r)


def _shared_inputs(inputs, cfg=FULL_CFG):
    m = {}
    if not cfg["hostmod"]:
        m["w_ada"] = np.ascontiguousarray(inputs["w_ada"])
        m["b_adaT"] = _fm(inputs["b_ada"], 72)
    m["norm_gT"] = _fm(inputs["norm_g"].reshape(DEPTH, 3 * D), 24)
    m["fnorm_gT"] = _fm(inputs["final_norm_g"], 8)
    if cfg["ffn1"] or cfg["ffn2"]:
        m["w_ffn_gu"] = np.ascontiguousarray(inputs["w_ffn_gu"])
        m["w_ffn_down"] = np.ascontiguousarray(inputs["w_ffn_down"])
    if cfg["mix"]:
        m["win2"] = np.ascontiguousarray(inputs["w_in"][:, :, W2COLS])
        m["w_out"] = np.ascontiguousarray(inputs["w_out"])
        m["lblT"] = _fm(inputs["lb_logits"], 2)
        m["hgngT"] = _fm(inputs["hg_norm_g"], 2)
        g64 = inputs["idx_k_norm_g"]
        b64 = inputs["idx_k_norm_b"]
        swp = np.concatenate([np.arange(32, 64), np.arange(0, 32)])
        kn = np.stack([np.tile(g64, (1, 2)), np.tile(b64, (1, 2)), np.tile(g64[:, swp], (1, 2)), np.tile(b64[:, swp], (1, 2))], axis=1)
        m["knT"] = np.ascontiguousarray(kn.transpose(2, 0, 1))
        m["convwT"] = np.ascontiguousarray(inputs["conv_w"].reshape(DEPTH, 4, 8, 128).transpose(3, 0, 2, 1))
        m["convbT"] = _fm(inputs["conv_b"], 8)
        m["ssmgT"] = _fm(inputs["ssm_norm_g"], 4)
        dsk = np.repeat(inputs["d_skip"], 64, axis=1)
        m["dskT"] = _fm(dsk, 4)
        m["dtbB"] = np.ascontiguousarray(np.broadcast_to(inputs["dt_bias"][None], (128, DEPTH, 8)))
        m["alogB"] = np.ascontiguousarray(np.broadcast_to(inputs["a_log"][None], (128, DEPTH, 8)))
    cc = _consts()
    for k in CONST_SHAPES:
        m[k] = cc[k]
    return m


def _host_inputs(inputs, core, cfg=FULL_CFG):
    ns = cfg["nseq"]
    b0 = core * NSEQ
    m = {"x": np.ascontiguousarray(inputs["x"][b0:b0 + ns])}
    if not cfg["hostmod"]:
        c = inputs["c"][b0:b0 + ns]
        m["cT"] = np.ascontiguousarray(c.reshape(ns, 8, 128).transpose(2, 1, 0))
    return m


def build_program(cfg=None):
    cfg = dict(FULL_CFG if cfg is None else cfg)
    NSQ = cfg["nseq"]
    LAYERS = list(cfg["layers"])
    MIX = tuple(cfg["mix"])
    nc = bass.Bass("TRN2", target_bir_lowering=False)

    def din(name, shape, dtype=F32):
        return nc.dram_tensor(name, list(shape), dtype, kind="ExternalInput").ap()

    x_d = din("x", [NSQ, S, D])
    if cfg["hostmod"]:
        modin_d = din("modT_in", [128, DEPTH, 72, NSQ])
    else:
        cT_d = din("cT", [128, 8, NSQ])
        wada_d = din("w_ada", [DEPTH, D, 9 * D])
        badaT_d = din("b_adaT", [128, DEPTH, 72])
    normgT_d = din("norm_gT", [128, DEPTH, 24])
    fngT_d = din("fnorm_gT", [128, 8])
    if cfg["ffn1"] or cfg["ffn2"]:
        wgu_d = din("w_ffn_gu", [DEPTH, 2, D, 2 * DFF])
        wdn_d = din("w_ffn_down", [DEPTH, 2, DFF, D])
    if MIX:
        win2_d = din("win2", [DEPTH, D, NC2])
        wout_d = din("w_out", [DEPTH, D, D])
        lblT_d = din("lblT", [128, DEPTH, 2])
        hgngT_d = din("hgngT", [128, DEPTH, 2])
        knT_d = din("knT", [128, DEPTH, 4])
        convwT_d = din("convwT", [128, DEPTH, 8, 4])
        convbT_d = din("convbT", [128, DEPTH, 8])
        ssmgT_d = din("ssmgT", [128, DEPTH, 4])
        dskT_d = din("dskT", [128, DEPTH, 4])
        dtbB_d = din("dtbB", [128, DEPTH, 8])
        alogB_d = din("alogB", [128, DEPTH, 8])
    cd = {k: din(k, shp) for k, shp in CONST_SHAPES.items()}
    out_d = nc.dram_tensor("out", [NSQ, S, D], F32, kind="ExternalOutput").ap()

    es = ExitStack()
    with es:
        P = Prog(nc, es)
        uid = [0]

        def sb(name, shape, dtype, st=es):
            uid[0] += 1
            return st.enter_context(nc.sbuf_tensor(f"sb{uid[0]}_{name}", list(shape), dtype))

        xT_t = sb("xT", [128, 8, S], F32)
        hT_t = sb("hT", [128, 8, S], BF16)
        RING = 3
        ring_t = [sb(f"ring{i}", [128, 6144], BF16) for i in range(RING)]
        ring_b = [Buf(f"ring{i}") for i in range(RING)]
        ring_sem = [P.dma_sem(f"ring{i}") for i in range(RING)]
        modT_t = sb("modT", [128, DEPTH, 72, NSQ], F32)
        AG_t = sb("AG", [128, DEPTH, NSQ, 6, 8], F32)
        normg_t = sb("normg", [128, DEPTH, 24], F32)
        fng_t = sb("fng", [128, 8], F32)
        eps_t = sb("eps", [128, 1], F32)
        one_t = sb("one", [128, 1], F32)
        ident_t = sb("ident", [128, 128], F32)
        identb_t = sb("identb", [128, 128], BF16)
        onesd_t = sb("onesd", [128, 128], BF16)

        cst = Buf("consts")
        modb = Buf("mod")
        csem = P.dma_sem("const")
        csemp = P.dma_sem("constp")
        misc_sem = P.dma_sem("miscp")
        P.dma("sp", ident_t[:], cd["ident"], csem, writes=[cst])
        P.dma("pool", identb_t[:], cd["ident"], csemp, writes=[cst])
        P.dma("pool", onesd_t[:], cd["ones_d"], csemp, writes=[cst])
        P.dma("sp", normg_t[:], normgT_d, csem, writes=[cst])
        P.dma("sp", fng_t[:], fngT_d, csem, writes=[cst])
        if MIX:
            ones256_t = sb("ones256", [128, 128], BF16)
            ones64b_t = sb("ones64b", [128, 128], BF16)
            onesf_t = sb("onesf", [128, 128], F32)
            bdmask_t = sb("bdmask", [128, 128], BF16)
            maskD2_t = sb("maskD2", [128, 128], BF16)
            maskX_t = sb("maskX", [128, 128], BF16)
            causT_t = sb("causT", [128, 128], BF16)
            causb_t = sb("causb", [128, 128], F32)
            Lgt_t = sb("Lgt", [128, 128], F32)
            Uincl_t = sb("Uincl", [128, 128], F32)
            pow2_t = sb("pow2", [128, NIT + 1], F32)
            lbl_t = sb("lbl", [128, DEPTH, 2], F32)
            lbv_t = sb("lbv", [128, DEPTH, 2], F32)
            oml_t = sb("oml", [128, DEPTH, 2], F32)
            hgng_t = sb("hgng", [128, DEPTH, 2], F32)
            kn_t = sb("kn", [128, DEPTH, 4], F32)
            cw_t = sb("cw", [128, DEPTH, 8, 4], F32)
            cb_t = sb("cb", [128, DEPTH, 8], F32)
            ssmg_t = sb("ssmg", [128, DEPTH, 4], F32)
            dsk_t = sb("dsk", [128, DEPTH, 4], F32)
            dtb_t = sb("dtbias", [128, DEPTH, 8], F32)
            nega_t = sb("nega", [128, DEPTH, 8], F32)
            negthr_t = sb("negthr", [128, 1], F32)
            for tname, tl, q in (("ones_256", ones256_t, "pool"), ("ones_64b", ones64b_t, "pool"), ("ones_f", onesf_t, "sp"),
                                 ("bdmask", bdmask_t, "pool"), ("maskD2", maskD2_t, "pool"), ("maskX", maskX_t, "pool"), ("causT", causT_t, "pool"), ("causbias", causb_t, "sp"),
                                 ("Lgt", Lgt_t, "sp"), ("Uincl", Uincl_t, "sp"), ("pow2", pow2_t, "sp")):
                P.dma(q, tl[:], cd[tname], csemp if q == "pool" else csem, writes=[cst])
            for dsrc, tl in ((lblT_d, lbl_t), (hgngT_d, hgng_t), (knT_d, kn_t), (convwT_d, cw_t), (convbT_d, cb_t),
                             (ssmgT_d, ssmg_t), (dskT_d, dsk_t), (dtbB_d, dtb_t), (alogB_d, nega_t)):
                P.dma("sp", tl[:], dsrc, csem, writes=[cst])
        P.op("dve", lambda e: e.memset(eps_t[:], EPS), writes=[cst])
        P.op("dve", lambda e: e.memset(one_t[:], 1.0), writes=[cst])
        cst.w = P.snapshot()
        if MIX:
            P.op("dve", lambda e: e.memset(negthr_t[:], -1e29), reads=[cst], writes=[cst])
            P.op("dve", lambda e: e.memset(lbv_t[:], 0.0), reads=[cst], writes=[cst])
            P.op("dve", lambda e: e.tensor_tensor(out=lbv_t[:, 1, :], in0=lbl_t[:, 1, :], in1=lbl_t[:, 0, :], op=ALU.subtract),
                 reads=[cst], writes=[cst])
            P.op("act", lambda e: e.activation(out=lbv_t[:, 1, :], in_=lbv_t[:, 1, :], func=AF.Sigmoid), reads=[cst], writes=[cst])
            P.op("dve", lambda e: e.tensor_scalar(out=oml_t[:], in0=lbv_t[:], scalar1=-1.0, scalar2=1.0, op0=ALU.mult, op1=ALU.add),
                 reads=[cst], writes=[cst])
            P.op("act", lambda e: e.activation(out=nega_t[:], in_=nega_t[:], func=AF.Exp), reads=[cst], writes=[cst])
            P.op("dve", lambda e: e.tensor_scalar(out=nega_t[:], in0=nega_t[:], scalar1=-1.0, scalar2=None, op0=ALU.mult),
                 reads=[cst], writes=[cst])

        ps_t = [es.enter_context(nc.psum_tensor(f"ps{i}", [128, 512], F32)) for i in range(8)]
        ps_b = [Buf(f"ps{i}") for i in range(8)]
        ps_rr = [0]

        ps_held = set()

        def psum(hold=False):
            while True:
                i = ps_rr[0] % 8
                ps_rr[0] += 1
                if i not in ps_held:
                    break
            if hold:
                ps_held.add(i)
            return V(ps_t[i][:], ps_b[i])

        def psum_release(v):
            ps_held.discard(ps_b.index(v.buf))

        def bf(pm):
            return pm.ap.bitcast(BF16)

        xb = [[Buf(f"x{c}_{t}") for t in range(4)] for c in range(8)]
        hb = [[Buf(f"h{c}_{t}") for t in range(4)] for c in range(8)]

        def xv(c, t):
            return V(xT_t[:, c, t * 512:(t + 1) * 512], xb[c][t])

        def hv(c, t):
            return V(hT_t[:, c, t * 512:(t + 1) * 512], hb[c][t])

        ring_rr = [0]

        def ring_next():
            i = ring_rr[0] % RING
            ring_rr[0] += 1
            return i

        def load_wset(l, names):
            i = ring_next()
            views = {}
            pos = 0
            for nm in names:
                n = W2N[nm]
                dst = ring_t[i][:, pos * 8:(pos + n) * 8].rearrange("p (k c) -> p k c", c=n)
                src = win2_d[l][:, W2OFF[nm]:W2OFF[nm] + n].rearrange("(k p) c -> p k c", p=128)
                P.dma("pool", dst, src, ring_sem[i], writes=[ring_b[i]])
                views[nm] = dst
                pos += n
            return i, views

        def load_wout(l, row0, nj):
            i = ring_next()
            dst = ring_t[i][:, 0:nj * 1024].rearrange("p (j m) -> p j m", m=1024)
            P.dma("pool", dst, wout_d[l][row0:row0 + nj * 128, :].rearrange("(j p) m -> p j m", p=128), ring_sem[i],
                  writes=[ring_b[i]])
            return i, dst

        def proj_fm(wview, r, t):
            pm = psum()
            for k in range(8):
                P.op("pe", lambda e, k=k: e.matmul(pm.ap, lhsT=wview[:, k, :], rhs=hT_t[:, k, t * 512:(t + 1) * 512],
                                                   start=(k == 0), stop=(k == 7)),
                     reads=[ring_b[r], hb[k][t]], writes=[pm], inc=(k == 7))
            return pm

        def proj_tm(wview, rbuf, tt, n):
            pm = psum()
            for k in range(8):
                P.op("pe", lambda e, k=k: e.matmul(pm.ap[:, 0:n], lhsT=hT_t[:, k, tt * 128:(tt + 1) * 128], rhs=wview[:, k, :],
                                                   start=(k == 0), stop=(k == 7)),
                     reads=[rbuf, hb[k][tt // 4]], writes=[pm], inc=(k == 7))
            return pm

        if cfg["hostmod"]:
            P.dma("sp", modT_t[:], modin_d, P.dma_sem("modin"), writes=[modb])
        else:
            with ExitStack() as ph:
                wa_t = [sb(f"wada{i}", [128, 8, 768], BF16, ph) for i in range(2)]
                cTb_t = sb("cTb", [128, 8, NSQ], BF16, ph)
                wa_b = [P.buf(f"wada{i}") for i in range(2)]
                wa_sem = [P.dma_sem(f"wada{i}") for i in range(2)]
                cT_t = sb("cT", [128, 8, NSQ], F32, ph)
                bada_t = sb("badaT", [128, DEPTH, 72], F32, ph)
                condb = P.buf("cond")
                cond_sem = P.dma_sem("cond")
                P.dma("sp", cT_t[:], cT_d, cond_sem, writes=[condb])
                P.dma("sp", bada_t[:], badaT_d, cond_sem, writes=[condb])
                P.op("act", lambda e: e.activation(out=cTb_t[:], in_=cT_t[:], func=AF.Silu), reads=[condb], writes=[condb])
                npiece = 12
                for l in LAYERS:
                    pm = psum()
                    for pc in range(npiece):
                        i = pc % 2
                        P.dma("pool", wa_t[i][:], wada_d[l][:, pc * 768:(pc + 1) * 768].rearrange("(k p) c -> p k c", p=128),
                              wa_sem[i], writes=[wa_b[i]])
                        for m in range(6):
                            mg = pc * 6 + m
                            for k in range(8):
                                P.op("pe", lambda e, i=i, m=m, k=k, mg=mg: e.matmul(
                                    pm.ap[:, mg * NSQ:(mg + 1) * NSQ], lhsT=wa_t[i][:, k, m * 128:(m + 1) * 128], rhs=cTb_t[:, k, :],
                                    start=(k == 0), stop=(k == 7)),
                                    reads=[wa_b[i], condb], writes=[pm], inc=(k == 7))
                    P.op("dve", lambda e, l=l: e.tensor_tensor(
                        out=modT_t[:, l, :, :], in0=pm.ap[:, 0:72 * NSQ].rearrange("p (m s) -> p m s", s=NSQ),
                        in1=bada_t[:, l, :].unsqueeze(2).to_broadcast([128, 72, NSQ]), op=ALU.add),
                        reads=[pm, condb], writes=[modb])
        for l in LAYERS:
            for s in range(NSQ):
                for j in range(3):
                    P.op("dve", lambda e, l=l, s=s, j=j: e.scalar_tensor_tensor(
                        out=AG_t[:, l, s, j, :], in0=modT_t[:, l, (3 * j + 1) * 8:(3 * j + 2) * 8, s], scalar=1.0,
                        in1=normg_t[:, l, j * 8:(j + 1) * 8], op0=ALU.add, op1=ALU.mult),
                        reads=[modb, cst], writes=[modb])
                    P.op("dve", lambda e, l=l, s=s, j=j: e.tensor_scalar(
                        out=AG_t[:, l, s, 3 + j, :], in0=modT_t[:, l, (3 * j + 2) * 8:(3 * j + 3) * 8, s],
                        scalar1=(1.0 if j == 1 else 0.5), scalar2=None, op0=ALU.mult),
                        reads=[modb], writes=[modb])

        def Avec(l, s, j, c):
            return AG_t[:, l, s, j, c:c + 1]

        def Gvec(l, s, j, c):
            return AG_t[:, l, s, 3 + j, c:c + 1]

        def Bvec(l, s, j, c):
            return modT_t[:, l, 3 * j * 8 + c, s:s + 1]

        def rms_state(ph, tag, C, ones_t):
            return dict(C=C, T=512, ones=ones_t,
                        sq=sb(f"sq_{tag}", [128, C, 512], BF16, ph), rs=sb(f"rs_{tag}", [128, 512], F32, ph),
                        tm=sb(f"tm_{tag}", [128, 2, 512], F32, ph),
                        sqb=P.buf("sq"), rsb=P.buf("rs"), tmb=[P.buf("tm0"), P.buf("tm1")])

        def rms_run(st, srcs, outs, scale_fn, bias_fn):
            C = st["C"]
            T = st["T"]
            sqv = V(st["sq"][:], st["sqb"])
            for c in range(C):
                P.op("act", lambda e, c=c: e.activation(out=st["sq"][:, c, :], in_=srcs[c].ap, func=AF.Square),
                     reads=[srcs[c]], writes=[sqv])
            pm = psum()
            for c in range(C):
                P.op("pe", lambda e, c=c: e.matmul(pm.ap[:, 0:T], lhsT=st["ones"][:], rhs=st["sq"][:, c, :],
                                                   start=(c == 0), stop=(c == C - 1)),
                     reads=[sqv, cst], writes=[pm], inc=(c == C - 1))
            rsv = V(st["rs"][:], st["rsb"])
            P.op("act", lambda e: e.activation(out=st["rs"][:], in_=pm.ap[:, 0:T], func=AF.Ln, bias=eps_t[:], scale=1.0),
                 reads=[pm, cst], writes=[rsv])
            P.op("act", lambda e: e.activation(out=st["rs"][:], in_=st["rs"][:], func=AF.Exp, scale=-0.5), reads=[rsv], writes=[rsv])
            for c in range(C):
                bias = bias_fn(c) if bias_fn is not None else None
                if bias is None:
                    P.op("dve", lambda e, c=c: e.scalar_tensor_tensor(
                        out=outs[c].ap, in0=srcs[c].ap, scalar=scale_fn(c), in1=st["rs"][:], op0=ALU.mult, op1=ALU.mult),
                        reads=[srcs[c], rsv, modb, cst], writes=[outs[c]])
                else:
                    tb = st["tmb"][c % 2]
                    P.op("dve", lambda e, c=c: e.scalar_tensor_tensor(
                        out=st["tm"][:, c % 2, :], in0=srcs[c].ap, scalar=scale_fn(c), in1=st["rs"][:], op0=ALU.mult, op1=ALU.mult),
                        reads=[srcs[c], rsv, modb, cst], writes=[tb])
                    P.op("act", lambda e, c=c, bias=bias: e.activation(
                        out=outs[c].ap, in_=st["tm"][:, c % 2, :], func=AF.Identity, bias=bias, scale=1.0),
                        reads=[tb, modb], writes=[outs[c]])

        def norm_mod(l, s, j):
            with ExitStack() as ph:
                st = rms_state(ph, "nm", 8, onesd_t)
                for t in range(4):
                    rms_run(st, [xv(c, t) for c in range(8)], [hv(c, t) for c in range(8)],
                            lambda c: Avec(l, s, j, c), lambda c: Bvec(l, s, j, c))

        def add_wout(l, s, r2, wo, nj, rhs_fn, rhs_bufs, t0, ntok):
            for m in range(8):
                pw = psum()
                for jj in range(nj):
                    P.op("pe", lambda e, jj=jj, m=m: e.matmul(pw.ap[:, 0:ntok], lhsT=wo[:, jj, m * 128:(m + 1) * 128], rhs=rhs_fn(jj),
                                                             start=(jj == 0), stop=(jj == nj - 1)),
                         reads=[ring_b[r2]] + list(rhs_bufs), writes=[pw], inc=(jj == nj - 1))
                xbuf = xb[m][t0 // 512]
                P.op("dve", lambda e, m=m: e.scalar_tensor_tensor(
                    out=xT_t[:, m, t0:t0 + ntok], in0=pw.ap[:, 0:ntok], scalar=Gvec(l, s, 1, m), in1=xT_t[:, m, t0:t0 + ntok],
                    op0=ALU.mult, op1=ALU.add), reads=[pw, xbuf, modb], writes=[xbuf])

        def ffn(l, s, i):
            j = 0 if i == 0 else 2
            norm_mod(l, s, j)
            with ExitStack() as ph:
                a_t = [sb(f"a{q}", [128, 2, 512], BF16, ph) for q in range(3)]
                a_b = [P.buf(f"a{q}") for q in range(3)]
                sg_t = [sb(f"sg{q}", [128, 512], F32, ph) for q in range(2)]
                sg_b = [P.buf(f"sg{q}") for q in range(2)]
                NG = 11
                wgu = wgu_d[l, i].rearrange("(k p) c -> p k c", p=128)
                wdn = wdn_d[l, i]

                def load(g):
                    r = ring_next()
                    rt = ring_t[r]
                    P.dma("pool", rt[:, 0:2048].rearrange("p (k c) -> p k c", c=256), wgu[:, :, g * 256:(g + 1) * 256],
                          ring_sem[r], writes=[ring_b[r]])
                    P.dma("pool", rt[:, 2048:4096].rearrange("p (k c) -> p k c", c=256),
                          wgu[:, :, DFF + g * 256:DFF + (g + 1) * 256], ring_sem[r], writes=[ring_b[r]])
                    P.dma("pool", rt[:, 4096:6144].rearrange("p (j m) -> p j m", m=1024),
                          wdn[g * 256:(g + 1) * 256, :].rearrange("(j p) m -> p j m", p=128), ring_sem[r], writes=[ring_b[r]])
                    return r
                slots = {0: load(0)}
                slots[1] = load(1)
                items = [(g, t) for g in range(NG) for t in range(4)]
                sgq = [0]

                def emit_gu(n):
                    g, t = items[n]
                    r = slots[g]
                    wg = ring_t[r][:, 0:2048].rearrange("p (k c) -> p k c", c=256)
                    wu = ring_t[r][:, 2048:4096].rearrange("p (k c) -> p k c", c=256)
                    av = a_b[n % 3]
                    for jj in range(2):
                        pg = psum()
                        pu = psum()
                        for k in range(8):
                            P.op("pe", lambda e, k=k, jj=jj: e.matmul(pg.ap, lhsT=wg[:, k, jj * 128:(jj + 1) * 128], rhs=hv(k, t).ap,
                                                                     start=(k == 0), stop=(k == 7)),
                                 reads=[ring_b[r], hv(k, t)], writes=[pg], inc=(k == 7))
                        for k in range(8):
                            P.op("pe", lambda e, k=k, jj=jj: e.matmul(pu.ap, lhsT=wu[:, k, jj * 128:(jj + 1) * 128], rhs=hv(k, t).ap,
                                                                     start=(k == 0), stop=(k == 7)),
                                 reads=[ring_b[r], hv(k, t)], writes=[pu], inc=(k == 7))
                        q = sgq[0] % 2
                        sgq[0] += 1
                        P.op("act", lambda e, q=q: e.activation(out=sg_t[q][:], in_=pg.ap, func=AF.Silu),
                             reads=[pg], writes=[sg_b[q]])
                        P.op("dve", lambda e, q=q, jj=jj: e.tensor_tensor(out=a_t[n % 3][:, jj, :], in0=pu.ap, in1=sg_t[q][:], op=ALU.mult),
                             reads=[pu, sg_b[q]], writes=[av])

                def emit_down(n):
                    g, t = items[n]
                    r = slots[g]
                    wd = ring_t[r][:, 4096:6144].rearrange("p (j m) -> p j m", m=1024)
                    for m in range(8):
                        po = psum()
                        for jj in range(2):
                            P.op("pe", lambda e, jj=jj, m=m: e.matmul(po.ap, lhsT=wd[:, jj, m * 128:(m + 1) * 128], rhs=a_t[n % 3][:, jj, :],
                                                                     start=(jj == 0), stop=(jj == 1)),
                                 reads=[ring_b[r], a_b[n % 3]], writes=[po], inc=(jj == 1))
                        P.op("dve", lambda e, m=m: e.scalar_tensor_tensor(
                            out=xv(m, t).ap, in0=po.ap, scalar=Gvec(l, s, j, m), in1=xv(m, t).ap, op0=ALU.mult, op1=ALU.add),
                            reads=[po, xv(m, t), modb], writes=[xv(m, t)])

                for n in range(len(items)):
                    g, t = items[n]
                    emit_gu(n)
                    if n > 0:
                        emit_down(n - 1)
                    if t == 0 and g + 2 < NG:
                        slots[g + 2] = load(g + 2)
                emit_down(len(items) - 1)

        def mixer_hg(l, s, slot):
            with ExitStack() as ph:
                qt = sb("hg_qt", [128, S], BF16, ph)
                kt = sb("hg_kt", [128, S], BF16, ph)
                ktok = sb("hg_ktok", [128, NT, 128], BF16, ph)
                vtok = sb("hg_vtok", [128, NT, 128], BF16, ph)
                e123 = sb("hg_e", [128, 3, 32], F32, ph)
                bmid = sb("hg_bmid", [128, 32], F32, ph)
                qB = sb("hg_qB", [128, S], BF16, ph)
                kB = sb("hg_kB", [128, S], BF16, ph)
                bh = sb("hg_bh", [128, 64], F32, ph)
                qtb, ktb, ktokb, vtokb, eb = P.buf("qt"), P.buf("kt"), P.buf("ktok"), P.buf("vtok"), P.buf("e")
                qBb, kBb = P.buf("qB"), P.buf("kB")
                r, wv = load_wset(l, [f"HQ{slot}", f"HF{slot}", f"HG{slot}", f"HI{slot}"])
                rb = ring_b[r]
                lbp = lbv_t[:, l, slot:slot + 1]
                omlp = oml_t[:, l, slot:slot + 1]
                with ExitStack() as pa:
                    bb = sb("hg_bb", [128, S], F32, pa)
                    bb2 = sb("hg_bb2", [128, S], F32, pa)
                    bb2b = P.buf("bb2")
                    tA = [sb(f"hg_tA{i}", [128, 512], F32, pa) for i in range(2)]
                    rmask = sb("hg_rmask", [128, S], BF16, pa)
                    bbb, tAb, rmb = P.buf("bb"), [P.buf("tA0"), P.buf("tA1")], P.buf("rmask")
                    P.dma("pool", rmask[:], cd["rmask"], misc_sem, writes=[rmb])
                    for t in range(4):
                        sl = slice(t * 512, (t + 1) * 512)
                        pf = proj_fm(wv[f"HF{slot}"], r, t)
                        P.op("act", lambda e: e.activation(out=tA[0][:], in_=pf.ap, func=AF.Sigmoid), reads=[pf], writes=[tAb[0]])
                        P.op("dve", lambda e: e.tensor_scalar(out=tA[0][:], in0=tA[0][:], scalar1=omlp, scalar2=lbp, op0=ALU.mult, op1=ALU.add),
                             reads=[tAb[0], cst], writes=[tAb[0]])
                        P.op("act", lambda e: e.activation(out=bb[:, sl], in_=tA[0][:], func=AF.Ln), reads=[tAb[0]], writes=[bbb])
                        P.op("dve", lambda e: e.tensor_scalar(out=kt[:, sl], in0=tA[0][:], scalar1=-1.0, scalar2=1.0, op0=ALU.mult, op1=ALU.add),
                             reads=[tAb[0]], writes=[ktb])
                        pq = proj_fm(wv[f"HQ{slot}"], r, t)
                        P.op("act", lambda e: e.activation(out=qt[:, sl], in_=pq.ap, func=AF.Copy), reads=[pq], writes=[qtb])
                        for tt in range(t * 4, t * 4 + 4):
                            pv = proj_tm(wv[f"HI{slot}"], rb, tt, 128)
                            P.op("dve", lambda e, tt=tt: e.tensor_copy(out=vtok[:, tt, :], in_=pv.ap[:, 0:128]), reads=[pv], writes=[vtokb])
                    P.op("dve", lambda e: e.tensor_tensor_scan(out=bb[:], data0=rmask[:], data1=bb[:], initial=0.0, op0=ALU.mult, op1=ALU.add),
                         reads=[bbb, rmb], writes=[bbb])
                    bb3 = bb[:].rearrange("p (c j) -> p c j", j=64)
                    bb4 = bb[:].rearrange("p (c j) -> p c j", j=32)
                    P.op("dve", lambda e: e.tensor_copy(out=bh[:], in_=bb4[:, :, 15]), reads=[bbb], writes=[eb])
                    P.op("dve", lambda e: e.tensor_tensor(out=bb2[:].rearrange("p (c j) -> p c j", j=32), in0=bb4,
                                                          in1=bh[:].unsqueeze(2).to_broadcast([128, 64, 32]), op=ALU.subtract),
                         reads=[bbb, eb], writes=[bb2b])
                    for t in range(4):
                        sl = slice(t * 512, (t + 1) * 512)
                        P.op("act", lambda e: e.activation(out=tA[0][:], in_=bb2[:, sl], func=AF.Exp), reads=[bb2b], writes=[tAb[0]])
                        P.op("dve", lambda e: e.tensor_tensor(out=qB[:, sl], in0=qt[:, sl], in1=tA[0][:], op=ALU.mult), reads=[qtb, tAb[0]], writes=[qBb])
                        P.op("act", lambda e: e.activation(out=tA[1][:], in_=bb2[:, sl], func=AF.Exp, scale=-1.0), reads=[bb2b], writes=[tAb[1]])
                        P.op("dve", lambda e: e.tensor_tensor(out=kB[:, sl], in0=kt[:, sl], in1=tA[1][:], op=ALU.mult), reads=[ktb, tAb[1]], writes=[kBb])
                    P.op("act", lambda e: e.activation(out=e123[:, 0, :], in_=bb3[:, :, 31], func=AF.Exp), reads=[bbb], writes=[eb])
                    P.op("act", lambda e: e.activation(out=e123[:, 2, :], in_=bb3[:, :, 63], func=AF.Exp), reads=[bbb], writes=[eb])
                    P.op("dve", lambda e: e.tensor_copy(out=bmid[:], in_=bb3[:, :, 31]), reads=[bbb], writes=[eb])
                    P.op("dve", lambda e: e.tensor_tensor(out=bb3, in0=bb3, in1=bmid[:].unsqueeze(2).to_broadcast([128, 32, 64]), op=ALU.subtract),
                         reads=[bbb, eb], writes=[bbb])
                    P.op("act", lambda e: e.activation(out=e123[:, 1, :], in_=bb3[:, :, 63], func=AF.Exp), reads=[bbb], writes=[eb])
                    for t in range(4):
                        sl = slice(t * 512, (t + 1) * 512)
                        P.op("act", lambda e: e.activation(out=tA[0][:], in_=bb[:, sl], func=AF.Exp), reads=[bbb], writes=[tAb[0]])
                        P.op("dve", lambda e: e.tensor_tensor(out=qt[:, sl], in0=qt[:, sl], in1=tA[0][:], op=ALU.mult), reads=[qtb, tAb[0]], writes=[qtb])
                        P.op("act", lambda e: e.activation(out=tA[1][:], in_=bb[:, sl], func=AF.Exp, scale=-1.0), reads=[bbb], writes=[tAb[1]])
                        P.op("dve", lambda e: e.tensor_tensor(out=kt[:, sl], in0=kt[:, sl], in1=tA[1][:], op=ALU.mult), reads=[ktb, tAb[1]], writes=[ktb])
                for t4 in range(4):
                    pm = psum()
                    pmb = bf(pm)
                    for i in range(4):
                        tt = t4 * 4 + i
                        P.op("pe", lambda e, i=i, tt=tt: e.transpose(pmb[:, i * 128:(i + 1) * 128], kt[:, tt * 128:(tt + 1) * 128], identb_t[:]),
                             reads=[ktb, cst], writes=[pm], inc=(i == 3))
                    P.op("dve", lambda e, t4=t4: e.tensor_copy(out=ktok[:, t4 * 4:(t4 + 1) * 4, :],
                                                               in_=pmb[:, 0:512].rearrange("p (a b) -> p a b", b=128)),
                         reads=[pm], writes=[ktokb])
                with ExitStack() as pb:
                    kvs = sb("hg_kvs", [128, 64, 32], F32, pb)
                    e3bc = sb("hg_e3bc", [128, 64, 32], F32, pb)
                    smid = sb("hg_smid", [128, 32, 64], BF16, pb)
                    scm = [sb(f"hg_scm{i}", [128, 2, 128], BF16, pb) for i in range(2)]
                    sctmp = sb("hg_sctmp", [128, 2, 32], BF16, pb)
                    sctb = P.buf("sctmp")
                    osb = sb("hg_osb", [128, 512], F32, pb)
                    sgl = sb("hg_sgl", [128, 512], F32, pb)
                    oA = [sb(f"hg_oA{i}", [128, 512], BF16, pb) for i in range(2)]
                    kvsb, e3b, smb, scb, osbb, sglb = P.buf("kvs"), P.buf("e3bc"), P.buf("smid"), [P.buf("scm0"), P.buf("scm1")], P.buf("osb"), P.buf("sgl")
                    oAb = [P.buf("oA0"), P.buf("oA1")]
                    st = rms_state(pb, "hg", 1, ones64b_t)
                    for q_ in range(2):
                        P.op("dve", lambda e, q_=q_: e.memset(scm[q_][:], 0.0), writes=[scb[q_]])
                    for c0 in range(0, 32, 8):
                        pmh = [psum(), psum()]
                        for half in range(2):
                            pm = pmh[half]
                            for idx in range(4):
                                c = c0 + 2 * idx + half
                                tt = c // 2
                                for part in range(2):
                                    last = (idx == 3 and part == 1)
                                    P.op("pe", lambda e, pm=pm, idx=idx, tt=tt, half=half, part=part: e.matmul(
                                        pm.ap[part * 64:(part + 1) * 64, idx * 64:(idx + 1) * 64],
                                        lhsT=ktok[half * 64:(half + 1) * 64, tt, part * 64:(part + 1) * 64],
                                        rhs=vtok[half * 64:(half + 1) * 64, tt, part * 64:(part + 1) * 64], start=True, stop=True),
                                        reads=[ktokb, vtokb], writes=[pm], inc=last)
                            P.op("dve", lambda e, pm=pm, c0=c0, half=half: e.tensor_tensor(
                                out=kvs[:, :, c0 + half:c0 + 8:2].rearrange("p v c -> p c v"), in0=pm.ap[:, 0:256].rearrange("p (c v) -> p c v", v=64),
                                in1=e123[:, 1, c0 + half:c0 + 8:2].unsqueeze(2).to_broadcast([128, 4, 64]), op=ALU.mult),
                                reads=[pm, eb], writes=[kvsb])
                    P.op("dve", lambda e: e.memset(e3bc[:, :, 0:1], 0.0), writes=[e3b])
                    P.op("dve", lambda e: e.tensor_copy(out=e3bc[:, :, 1:32], in_=e123[:, 2, 1:32].unsqueeze(1).to_broadcast([128, 64, 31])),
                         reads=[eb], writes=[e3b])
                    P.op("dve", lambda e: e.tensor_tensor_scan(out=kvs[:].rearrange("p v c -> p (v c)"), data0=e3bc[:].rearrange("p v c -> p (v c)"),
                                                               data1=kvs[:].rearrange("p v c -> p (v c)"), initial=0.0, op0=ALU.mult, op1=ALU.add),
                         reads=[kvsb, e3b], writes=[kvsb])
                    P.op("dve", lambda e: e.memset(smid[:, 0, :], 0.0), writes=[smb])
                    P.op("dve", lambda e: e.tensor_tensor(out=smid[:, 1:32, :], in0=kvs[:, :, 0:31].rearrange("p v c -> p c v"),
                                                          in1=e123[:, 0, 1:32].unsqueeze(2).to_broadcast([128, 31, 64]), op=ALU.mult),
                         reads=[kvsb, eb], writes=[smb])
                    r2, wo = load_wout(l, slot * 128, 1)
                    for t in range(4):
                        pos_ = [psum(hold=True), psum(hold=True)]
                        for i in range(4):
                            tt = t * 4 + i
                            q = tt % 2
                            tsl = slice(tt * 128, (tt + 1) * 128)
                            pscs = [psum(), psum()]
                            for cc in range(2):
                                c = 2 * tt + cc
                                R = slice(cc * 64, (cc + 1) * 64)
                                for part in range(2):
                                    pr = slice(part * 64, (part + 1) * 64)
                                    psc = pscs[part]
                                    P.op("pe", lambda e, psc=psc, c=c, R=R, pr=pr: e.matmul(
                                        psc.ap[R, 0:32], lhsT=kB[pr, c * 64:(c + 1) * 64], rhs=qB[pr, c * 64:c * 64 + 32],
                                        start=True, stop=True), reads=[kBb, qBb], writes=[psc], inc=False)
                                    P.op("pe", lambda e, psc=psc, c=c, R=R, pr=pr: e.matmul(
                                        psc.ap[R, 32:64], lhsT=kB[pr, c * 64:(c + 1) * 64], rhs=qB[pr, c * 64 + 32:c * 64 + 64],
                                        start=True, stop=True), reads=[kBb, qBb], writes=[psc], inc=False)
                                    P.op("pe", lambda e, psc=psc, c=c, R=R, pr=pr: e.matmul(
                                        psc.ap[R, 64:96], lhsT=kt[pr, c * 64:(c + 1) * 64], rhs=qt[pr, c * 64 + 32:c * 64 + 64],
                                        start=True, stop=True), reads=[ktb, qtb], writes=[psc], inc=True)
                            for cc in range(2):
                                R = slice(cc * 64, (cc + 1) * 64)
                                c0_ = cc * 64
                                for part in range(2):
                                    psc = pscs[part]
                                    P.op("dve", lambda e, q=q, R=R, psc=psc, c0_=c0_, part=part: e.tensor_tensor(
                                        out=scm[q][R, part, c0_:c0_ + 32], in0=psc.ap[R, 0:32], in1=bdmask_t[R, c0_:c0_ + 32], op=ALU.mult),
                                        reads=[psc, cst], writes=[scb[q]])
                                    P.op("dve", lambda e, q=q, R=R, psc=psc, c0_=c0_, part=part: e.tensor_tensor(
                                        out=scm[q][R, part, c0_ + 32:c0_ + 64], in0=psc.ap[R, 32:64], in1=maskD2_t[R, c0_ + 32:c0_ + 64], op=ALU.mult),
                                        reads=[psc, cst], writes=[scb[q]])
                                    P.op("dve", lambda e, R=R, psc=psc, c0_=c0_, part=part: e.tensor_tensor(
                                        out=sctmp[R, part, :], in0=psc.ap[R, 64:96], in1=maskX_t[R, c0_ + 32:c0_ + 64], op=ALU.mult),
                                        reads=[psc, cst], writes=[sctb])
                                    P.op("dve", lambda e, q=q, R=R, c0_=c0_, part=part: e.tensor_tensor(
                                        out=scm[q][R, part, c0_ + 32:c0_ + 64], in0=scm[q][R, part, c0_ + 32:c0_ + 64], in1=sctmp[R, part, :], op=ALU.add),
                                        reads=[scb[q], sctb], writes=[scb[q]])
                            for part in range(2):
                                po = pos_[part]
                                P.op("pe", lambda e, po=po, part=part, i=i, tt=tt, q=q: e.matmul(
                                    po.ap[part * 64:(part + 1) * 64, i * 128:(i + 1) * 128], lhsT=vtok[:, tt, part * 64:(part + 1) * 64],
                                    rhs=scm[q][:, part, :], start=True, stop=False),
                                    reads=[vtokb, scb[q]], writes=[po], inc=False)
                                for cc in range(2):
                                    c = 2 * tt + cc
                                    P.op("pe", lambda e, po=po, part=part, i=i, cc=cc, c=c: e.matmul(
                                        po.ap[part * 64:(part + 1) * 64, i * 128 + cc * 64:i * 128 + (cc + 1) * 64],
                                        lhsT=smid[part * 64:(part + 1) * 64, c, :], rhs=qt[part * 64:(part + 1) * 64, c * 64:(c + 1) * 64],
                                        start=False, stop=(cc == 1)),
                                        reads=[smb, qtb], writes=[po], inc=(cc == 1))
                        for part in range(2):
                            pr = slice(part * 64, (part + 1) * 64)
                            P.op("act", lambda e, part=part, pr=pr: e.activation(out=osb[pr, :], in_=pos_[part].ap[pr, :], func=AF.Copy),
                                 reads=[pos_[part]], writes=[osbb])
                            psum_release(pos_[part])
                        ov = V(osb[:], osbb)
                        rms_run(st, [ov], [ov], lambda c: hgng_t[:, l, slot:slot + 1], None)
                        pg = proj_fm(wv[f"HG{slot}"], r, t)
                        P.op("act", lambda e: e.activation(out=sgl[:], in_=pg.ap, func=AF.Silu), reads=[pg], writes=[sglb])
                        oq = t % 2
                        P.op("dve", lambda e, oq=oq: e.tensor_tensor(out=oA[oq][:], in0=osb[:], in1=sgl[:], op=ALU.mult),
                             reads=[osbb, sglb], writes=[oAb[oq]])
                        add_wout(l, s, r2, wo, 1, lambda jj, oq=oq: oA[oq][:], [oAb[oq]], t * 512, 512)

        def mixer_dsa(l, s):
            with ExitStack() as ph:
                kT2 = sb("ds_kT2", [128, S], BF16, ph)
                ikT2 = sb("ds_ikT2", [128, S], BF16, ph)
                vaug = sb("ds_vaug", [128, NT, 65], BF16, ph)
                iwt = sb("ds_iwt", [128, NT, 8], F32, ph)
                qT = sb("ds_qT", [128, 2, S], BF16, ph)
                iqT = sb("ds_iqT", [128, 4, S], BF16, ph)
                kTb, ikTb, vab, iwb, qTb, iqTb = (P.buf("kT2"), P.buf("ikT2"), P.buf("vaug"), P.buf("iwt"), P.buf("qT"), P.buf("iqT"))
                r1, w1 = load_wset(l, ["AQ0", "AQW0", "AQ1", "AQW1", "AK2", "AKW2"])
                r2, w2 = load_wset(l, ["IQ0", "IQW0", "IQ1", "IQW1", "IQ2", "IQW2"])
                r3, w3 = load_wset(l, ["IQ3", "IQW3", "IK2", "IKW2", "AVIW"])
                with ExitStack() as pa:
                    cosT = sb("ds_cos", [128, S], BF16, pa)
                    sinT = sb("ds_sin", [128, S], BF16, pa)
                    tbl = P.buf("ropetab")
                    P.dma("pool", cosT[:], cd["cosT"], misc_sem, writes=[tbl])
                    P.dma("pool", sinT[:], cd["sinT"], misc_sem, writes=[tbl])
                    t1 = sb("ds_t1", [128, 512], F32, pa)
                    t2 = sb("ds_t2", [128, 512], F32, pa)
                    t1b, t2b = P.buf("t1"), P.buf("t2")
                    x1 = sb("ds_x1", [128, 512], F32, pa)
                    x2 = sb("ds_x2", [128, 512], F32, pa)
                    xq = sb("ds_xq", [128, 512], BF16, pa)
                    rsn = sb("ds_rsn", [128, 512], F32, pa)
                    x1b, x2b, xqb, rsnb = P.buf("x1"), P.buf("x2"), P.buf("xq"), P.buf("rsn")

                    def rope_comb(dst_ap, dstbuf, a_v, b_v, t):
                        sl = slice(t * 512, (t + 1) * 512)
                        P.op("dve", lambda e: e.tensor_tensor(out=t1[:], in0=a_v.ap, in1=cosT[:, sl], op=ALU.mult), reads=[a_v, tbl], writes=[t1b])
                        P.op("dve", lambda e: e.tensor_tensor(out=t2[:], in0=b_v.ap, in1=sinT[:, sl], op=ALU.mult), reads=[b_v, tbl], writes=[t2b])
                        P.op("dve", lambda e: e.tensor_tensor(out=dst_ap, in0=t1[:], in1=t2[:], op=ALU.add), reads=[t1b, t2b], writes=[dstbuf])

                    for t in range(4):
                        sl = slice(t * 512, (t + 1) * 512)
                        for sq_ in range(2):
                            pa_ = proj_fm(w1[f"AQ{sq_}"], r1, t)
                            pb_ = proj_fm(w1[f"AQW{sq_}"], r1, t)
                            rope_comb(qT[:, sq_, sl], qTb, pa_, pb_, t)
                        pa_ = proj_fm(w1["AK2"], r1, t)
                        pb_ = proj_fm(w1["AKW2"], r1, t)
                        rope_comb(kT2[:, sl], kTb, pa_, pb_, t)
                        for sq_ in range(4):
                            rr, ww = (r2, w2) if sq_ < 3 else (r3, w3)
                            pa_ = proj_fm(ww[f"IQ{sq_}"], rr, t)
                            pb_ = proj_fm(ww[f"IQW{sq_}"], rr, t)
                            rope_comb(iqT[:, sq_, sl], iqTb, pa_, pb_, t)
                        pa_ = proj_fm(w3["IK2"], r3, t)
                        pb_ = proj_fm(w3["IKW2"], r3, t)
                        P.op("act", lambda e: e.activation(out=x1[:], in_=pa_.ap, func=AF.Copy), reads=[pa_], writes=[x1b])
                        P.op("act", lambda e: e.activation(out=x2[:], in_=pb_.ap, func=AF.Copy), reads=[pb_], writes=[x2b])
                        P.op("act", lambda e: e.activation(out=xq[:], in_=x1[:], func=AF.Copy), reads=[x1b], writes=[xqb])
                        pmn = psum()
                        P.op("pe", lambda e: e.matmul(pmn.ap, lhsT=ones64b_t[:], rhs=xq[:], start=True, stop=True), reads=[xqb, cst], writes=[pmn])
                        P.op("dve", lambda e: e.tensor_tensor(out=x1[:], in0=x1[:], in1=pmn.ap, op=ALU.subtract), reads=[x1b, pmn], writes=[x1b])
                        P.op("dve", lambda e: e.tensor_tensor(out=x2[:], in0=x2[:], in1=pmn.ap, op=ALU.subtract), reads=[x2b, pmn], writes=[x2b])
                        P.op("act", lambda e: e.activation(out=xq[:], in_=x1[:], func=AF.Square), reads=[x1b], writes=[xqb])
                        pvr = psum()
                        P.op("pe", lambda e: e.matmul(pvr.ap, lhsT=ones64b_t[:], rhs=xq[:], start=True, stop=True), reads=[xqb, cst], writes=[pvr])
                        P.op("act", lambda e: e.activation(out=rsn[:], in_=pvr.ap, func=AF.Ln, bias=eps_t[:], scale=1.0), reads=[pvr, cst], writes=[rsnb])
                        P.op("act", lambda e: e.activation(out=rsn[:], in_=rsn[:], func=AF.Exp, scale=-0.5), reads=[rsnb], writes=[rsnb])
                        P.op("dve", lambda e: e.scalar_tensor_tensor(out=x1[:], in0=x1[:], scalar=kn_t[:, l, 0:1], in1=rsn[:], op0=ALU.mult, op1=ALU.mult),
                             reads=[x1b, rsnb, cst], writes=[x1b])
                        P.op("act", lambda e: e.activation(out=x1[:], in_=x1[:], func=AF.Identity, bias=kn_t[:, l, 1:2], scale=1.0), reads=[x1b, cst], writes=[x1b])
                        P.op("dve", lambda e: e.scalar_tensor_tensor(out=x2[:], in0=x2[:], scalar=kn_t[:, l, 2:3], in1=rsn[:], op0=ALU.mult, op1=ALU.mult),
                             reads=[x2b, rsnb, cst], writes=[x2b])
                        P.op("act", lambda e: e.activation(out=x2[:], in_=x2[:], func=AF.Identity, bias=kn_t[:, l, 3:4], scale=1.0), reads=[x2b, cst], writes=[x2b])
                        rope_comb(ikT2[:, sl], ikTb, V(x1[:], x1b), V(x2[:], x2b), t)
                    P.op("dve", lambda e: e.memset(vaug[:, :, 64:65], 1.0), writes=[vab])
                    for tt in range(NT):
                        pv = proj_tm(w3["AVIW"], ring_b[r3], tt, 72)
                        P.op("act", lambda e, tt=tt: e.activation(out=vaug[:, tt, 0:64], in_=pv.ap[:, 0:64], func=AF.Copy), reads=[pv], writes=[vab])
                        P.op("act", lambda e, tt=tt: e.activation(out=iwt[:, tt, :], in_=pv.ap[:, 64:72], func=AF.Copy, scale=IWSCALE),
                             reads=[pv], writes=[iwb])
                with ExitStack() as pb:
                    acc = sb("ds_acc", [128, S], F32, pb)
                    mask = sb("ds_mask", [128, S], BF16, pb)
                    junk = sb("ds_junk", [128, S], F32, pb)
                    junkb = P.buf("junk")
                    rl = [sb(f"ds_rl{i}", [128, 512], F32, pb) for i in range(2)]
                    PT = [sb(f"ds_PT{i}", [128, 4, 128], BF16, pb) for i in range(2)]
                    otok = sb("ds_otok", [128, 4, 64], BF16, pb)
                    oBt = sb("ds_oBt", [128, 2, 128], BF16, pb)
                    sm_ = sb("ds_small", [128, 8], F32, pb)
                    HWt = sb("ds_HW", [128, NIT + 1], F32, pb)
                    HW2 = sb("ds_HW2", [128, NIT + 1], F32, pb)
                    rden = sb("ds_rden", [128, 4], F32, pb)
                    accb, maskb, rlb, PTb, otb, oBb, smb_, rdb = (P.buf("acc"), P.buf("mask"), [P.buf("rl0"), P.buf("rl1")],
                                                                 [P.buf("PT0"), P.buf("PT1")], P.buf("otok"), P.buf("oBt"), P.buf("small"), P.buf("rden"))
                    r4, wo = load_wout(l, 256, 2)
                    rlq = [0]
                    ptq = [0]
                    for j in range(NT):
                        W = 128 * (j + 1)
                        qsl = slice(j * 128, (j + 1) * 128)
                        nkc = (W + 511) // 512
                        for kc in range(nkc):
                            Wc = min(512, W - kc * 512)
                            ksl = slice(kc * 512, kc * 512 + Wc)
                            for h in range(8):
                                part, slot = h % 2, h // 2
                                pl = psum()
                                P.op("pe", lambda e, part=part, slot=slot: e.matmul(
                                    pl.ap[:, 0:Wc], lhsT=iqT[part * 64:(part + 1) * 64, slot, qsl], rhs=ikT2[part * 64:(part + 1) * 64, ksl],
                                    start=True, stop=True), reads=[iqTb, ikTb], writes=[pl])
                                q = rlq[0] % 2
                                rlq[0] += 1
                                P.op("act", lambda e, q=q: e.activation(out=rl[q][:, 0:Wc], in_=pl.ap[:, 0:Wc], func=AF.Relu), reads=[pl], writes=[rlb[q]])
                                if h == 0:
                                    P.op("dve", lambda e, q=q: e.tensor_scalar(out=acc[:, ksl], in0=rl[q][:, 0:Wc], scalar1=iwt[:, j, 0:1], scalar2=None, op0=ALU.mult),
                                         reads=[rlb[q], iwb], writes=[accb])
                                else:
                                    P.op("dve", lambda e, q=q, h=h: e.scalar_tensor_tensor(
                                        out=acc[:, ksl], in0=rl[q][:, 0:Wc], scalar=iwt[:, j, h:h + 1], in1=acc[:, ksl], op0=ALU.mult, op1=ALU.add),
                                        reads=[rlb[q], iwb, accb], writes=[accb])
                        P.op("dve", lambda e: e.tensor_tensor(out=acc[:, qsl], in0=acc[:, qsl], in1=causb_t[:], op=ALU.add), reads=[accb, cst], writes=[accb])
                        if j >= 2:
                            P.op("dve", lambda e: e.tensor_reduce(out=sm_[:, 0:1], in_=acc[:, 0:W - 128], axis=AX.X, op=ALU.min), reads=[accb], writes=[smb_])
                            P.op("dve", lambda e: e.tensor_reduce(out=sm_[:, 1:2], in_=acc[:, 0:W], axis=AX.X, op=ALU.max), reads=[accb, smb_], writes=[smb_])
                            P.op("dve", lambda e: e.tensor_tensor(out=sm_[:, 2:3], in0=sm_[:, 1:2], in1=sm_[:, 0:1], op=ALU.subtract), reads=[smb_], writes=[smb_])
                            P.op("dve", lambda e: e.tensor_scalar(out=HWt[:], in0=pow2_t[:], scalar1=sm_[:, 2:3], scalar2=None, op0=ALU.mult), reads=[smb_, cst], writes=[smb_])
                            P.op("dve", lambda e: e.tensor_scalar(out=HW2[:], in0=HWt[:], scalar1=2.0, scalar2=None, op0=ALU.mult), reads=[smb_], writes=[smb_])
                            P.op("dve", lambda e: e.tensor_tensor(out=sm_[:, 3:4], in0=sm_[:, 0:1], in1=HWt[:, 0:1], op=ALU.add), reads=[smb_], writes=[smb_])
                            for n in range(NIT):
                                P.op("dve", lambda e: e.tensor_scalar(out=junk[:, 0:W], in0=acc[:, 0:W], scalar1=sm_[:, 3:4], scalar2=None,
                                                                      op0=ALU.is_ge, op1=ALU.add, accum_out=sm_[:, 4:5]),
                                     reads=[accb, smb_], writes=[junkb, smb_])
                                P.op("dve", lambda e, n=n: e.tensor_scalar(out=sm_[:, 5:6], in0=sm_[:, 4:5], scalar1=255.5, scalar2=HW2[:, n + 1:n + 2],
                                                                           op0=ALU.is_ge, op1=ALU.mult), reads=[smb_], writes=[smb_])
                                P.op("dve", lambda e, n=n: e.scalar_tensor_tensor(out=sm_[:, 3:4], in0=sm_[:, 5:6], scalar=HWt[:, n + 1:n + 2], in1=sm_[:, 3:4],
                                                                                  op0=ALU.subtract, op1=ALU.add), reads=[smb_], writes=[smb_])
                            P.op("dve", lambda e: e.tensor_tensor(out=sm_[:, 6:7], in0=sm_[:, 3:4], in1=HWt[:, NIT:NIT + 1], op=ALU.subtract), reads=[smb_], writes=[smb_])
                            thr = sm_[:, 6:7]
                        else:
                            thr = negthr_t[:]
                        P.op("dve", lambda e: e.tensor_scalar(out=mask[:, 0:W], in0=acc[:, 0:W], scalar1=thr, scalar2=None, op0=ALU.is_ge),
                             reads=[accb, smb_, cst], writes=[maskb])
                        po = psum(hold=True)
                        po3 = po.ap[:, 0:260].rearrange("p (h e) -> p h e", e=65)
                        first = True
                        for kb in range(j + 1):
                            kbs = slice(kb * 128, (kb + 1) * 128)
                            pmT = psum()
                            pmTb = bf(pmT)
                            P.op("pe", lambda e: e.transpose(pmTb[:, 0:128], mask[:, kbs], identb_t[:]), reads=[maskb, cst], writes=[pmT])
                            psts = [psum(), psum()]
                            q = ptq[0] % 2
                            ptq[0] += 1
                            for part in range(2):
                                for slot in range(2):
                                    P.op("pe", lambda e, part=part, slot=slot: e.matmul(
                                        psts[part].ap[:, slot * 128:(slot + 1) * 128], lhsT=kT2[part * 64:(part + 1) * 64, kbs],
                                        rhs=qT[part * 64:(part + 1) * 64, slot, qsl], start=True, stop=True),
                                        reads=[kTb, qTb], writes=[psts[part]], inc=(slot == 1))
                                P.op("act", lambda e, q=q, part=part: e.activation(
                                    out=PT[q][:, part:4:2, :], in_=psts[part].ap[:, 0:256].rearrange("p (h t) -> p h t", t=128), func=AF.Exp, scale=0.125),
                                    reads=[psts[part]], writes=[PTb[q]])
                            P.op("dve", lambda e, q=q: e.tensor_tensor(out=PT[q][:], in0=PT[q][:], in1=pmTb[:, 0:128].unsqueeze(1).to_broadcast([128, 4, 128]), op=ALU.mult),
                                 reads=[PTb[q], pmT], writes=[PTb[q]])
                            for h in range(4):
                                lastmm = (kb == j and h == 3)
                                P.op("pe", lambda e, h=h, q=q, first=first, lastmm=lastmm: e.matmul(
                                    po3[:, h, :], lhsT=PT[q][:, h, :], rhs=vaug[:, kb, :], start=first, stop=lastmm, skip_group_check=True),
                                    reads=[PTb[q], vab], writes=[po], inc=(h == 3))
                                first = False
                        P.op("dve", lambda e: e.reciprocal(out=rden[:], in_=po3[:, :, 64]), reads=[po], writes=[rdb])
                        P.op("dve", lambda e: e.tensor_tensor(out=otok[:], in0=po3[:, :, 0:64], in1=rden[:].unsqueeze(2).to_broadcast([128, 4, 64]), op=ALU.mult),
                             reads=[po, rdb], writes=[otb])
                        psum_release(po)
                        pmo = psum()
                        pmob = bf(pmo)
                        of = otok[:].rearrange("p h d -> p (h d)")
                        for sl_ in range(2):
                            P.op("pe", lambda e, sl_=sl_: e.transpose(pmob[:, sl_ * 128:(sl_ + 1) * 128], of[:, sl_ * 128:(sl_ + 1) * 128], identb_t[:]),
                                 reads=[otb, cst], writes=[pmo], inc=(sl_ == 1))
                        P.op("dve", lambda e: e.tensor_copy(out=oBt[:], in_=pmob[:, 0:256].rearrange("p (a b) -> p a b", b=128)),
                             reads=[pmo], writes=[oBb])
                        add_wout(l, s, r4, wo, 2, lambda jj: oBt[:, jj, :], [oBb], j * 128, 128)

        def mixer_ssd(l, s):
            with ExitStack() as ph:
                wsdt = sb("ss_wsdt", [128, 8, 8], BF16, ph)
                dtb = sb("ss_dt", [128, NT, 8], F32, ph)
                dA = sb("ss_dA", [128, NT, 8], F32, ph)
                wsb, dtbb = P.buf("wsdt"), P.buf("dt")
                P.dma("pool", wsdt[:], win2_d[l][:, W2OFF["SDT"]:W2OFF["SDT"] + 8].rearrange("(k p) c -> p k c", p=128), misc_sem, writes=[wsb])
                for tt in range(NT):
                    pm = proj_tm(wsdt[:], wsb, tt, 8)
                    P.op("dve", lambda e, tt=tt: e.tensor_tensor(out=dtb[:, tt, :], in0=pm.ap[:, 0:8], in1=dtb_t[:, l, :], op=ALU.add),
                         reads=[pm, cst], writes=[dtbb])
                P.op("act", lambda e: e.activation(out=dtb[:], in_=dtb[:], func=AF.Exp), reads=[dtbb], writes=[dtbb])
                P.op("act", lambda e: e.activation(out=dtb[:], in_=dtb[:], func=AF.Ln, bias=one_t[:], scale=1.0), reads=[dtbb, cst], writes=[dtbb])
                P.op("dve", lambda e: e.tensor_tensor(out=dA[:], in0=dtb[:], in1=nega_t[:, l, :].unsqueeze(1).to_broadcast([128, NT, 8]), op=ALU.mult),
                     reads=[dtbb, cst], writes=[dtbb])
                for g in range(2):
                    with ExitStack() as pg_:
                        zs = sb("ss_zs", [128, 2, S], BF16, pg_)
                        xsT = sb("ss_xsT", [128, 2, S], BF16, pg_)
                        BT = sb("ss_BT", [128, S], BF16, pg_)
                        CT = sb("ss_CT", [128, S], BF16, pg_)
                        yz = sb("ss_yz", [128, 2, S], BF16, pg_)
                        zsb, xsb, BTb, CTb = P.buf("zs"), P.buf("xsT"), P.buf("BT"), P.buf("CT")
                        yzb = [[P.buf(f"yz{i}{t}") for t in range(4)] for i in range(2)]
                        r, wv = load_wset(l, [f"Z{g}0", f"Z{g}1", f"XS{g}0", f"XS{g}1", f"B{g}", f"C{g}"])
                        with ExitStack() as pa:
                            pre = sb("ss_pre", [128, 3 + S], F32, pa)
                            cac = sb("ss_cac", [128, S], F32, pa)
                            preb, cacb = P.buf("pre"), P.buf("cac")
                            for i in range(2):
                                for t in range(4):
                                    pm = proj_fm(wv[f"Z{g}{i}"], r, t)
                                    P.op("act", lambda e, i=i, t=t: e.activation(out=zs[:, i, t * 512:(t + 1) * 512], in_=pm.ap, func=AF.Silu),
                                         reads=[pm], writes=[zsb])
                            blocks = [(f"XS{g}0", 2 * g, xsT[:, 0, :], xsb), (f"XS{g}1", 2 * g + 1, xsT[:, 1, :], xsb),
                                      (f"B{g}", 4 + g, BT[:], BTb), (f"C{g}", 6 + g, CT[:], CTb)]
                            for nm, ch, dst, dstb in blocks:
                                P.op("dve", lambda e: e.memset(pre[:, 0:3], 0.0), writes=[preb])
                                for t in range(4):
                                    pm = proj_fm(wv[nm], r, t)
                                    P.op("act", lambda e, t=t: e.activation(out=pre[:, 3 + t * 512:3 + (t + 1) * 512], in_=pm.ap, func=AF.Copy),
                                         reads=[pm], writes=[preb])
                                P.op("dve", lambda e, ch=ch: e.tensor_scalar(out=cac[:], in0=pre[:, 3:3 + S], scalar1=cw_t[:, l, ch, 3:4], scalar2=cb_t[:, l, ch:ch + 1],
                                                                             op0=ALU.mult, op1=ALU.add), reads=[preb, cst], writes=[cacb])
                                for jt in range(3):
                                    P.op("dve", lambda e, ch=ch, jt=jt: e.scalar_tensor_tensor(out=cac[:], in0=pre[:, jt:jt + S], scalar=cw_t[:, l, ch, jt:jt + 1], in1=cac[:],
                                                                                               op0=ALU.mult, op1=ALU.add), reads=[preb, cacb, cst], writes=[cacb])
                                P.op("act", lambda e, dst=dst: e.activation(out=dst, in_=cac[:], func=AF.Silu), reads=[cacb], writes=[dstb])
                        with ExitStack() as pb:
                            def two(name, shape, dt_):
                                return [sb(f"{name}{q_}", shape, dt_, pb) for q_ in range(2)], [P.buf(f"{name}{q_}") for q_ in range(2)]
                            rhsU2, rhsUb2 = two("ss_rhsU", [128, 4, 128], F32)
                            Eseg2, Esegb2 = two("ss_Eseg", [128, 4, 128], BF16)
                            Ebc2, Ebcb2 = two("ss_Ebc", [128, 4, 128], BF16)
                            smx2, smxb2 = two("ss_smx", [128, 8], F32)
                            CBm2, CBmb2 = two("ss_CBm", [128, 128], BF16)
                            MT2, MTb2 = two("ss_MT", [128, 4, 128], BF16)
                            CsT2, CsTb2 = two("ss_CsT", [128, 4, 128], BF16)
                            xdt2, xdtb2 = two("ss_xdt", [128, 4, 64], BF16)
                            xdtd2, xdtdb2 = two("ss_xdtd", [128, 4, 64], BF16)
                            Btok2, Btokb2 = two("ss_Btok", [128, 128], BF16)
                            ytmp2, ytmpb2 = two("ss_ytmp", [128, 2, 128], F32)
                            prevb162, prevbb2 = two("ss_prevb", [128, 4, 64], BF16)
                            prev = sb("ss_prev", [128, 4, 64], F32, pb)
                            prevb = P.buf("prev")
                            st = rms_state(pb, "ss", 2, ones256_t)
                            P.op("dve", lambda e: e.memset(prev[:], 0.0), writes=[prevb])
                            for c in range(NT):
                                csl = slice(c * 128, (c + 1) * 128)
                                dAc = dA[:, c, 4 * g:4 * g + 4]
                                q_ = c % 2
                                rhsU, rhsUb, Eseg, Esegb, Ebc, Ebcb, smx, smxb = rhsU2[q_], rhsUb2[q_], Eseg2[q_], Esegb2[q_], Ebc2[q_], Ebcb2[q_], smx2[q_], smxb2[q_]
                                CBm, CBmb, MT, MTb, CsT, CsTb = CBm2[q_], CBmb2[q_], MT2[q_], MTb2[q_], CsT2[q_], CsTb2[q_]
                                xdt, xdtb, xdtd, xdtdb, Btok, Btokb, ytmp, ytmpb = xdt2[q_], xdtb2[q_], xdtd2[q_], xdtdb2[q_], Btok2[q_], Btokb2[q_], ytmp2[q_], ytmpb2[q_]
                                prevb16, prevbb = prevb162[q_], prevbb2[q_]
                                prevb16n, prevbbn = prevb162[1 - q_], prevbb2[1 - q_]
                                P.op("dve", lambda e: e.tensor_tensor(out=rhsU[:], in0=Uincl_t[:].unsqueeze(1).to_broadcast([128, 4, 128]),
                                                                      in1=dAc.unsqueeze(2).to_broadcast([128, 4, 128]), op=ALU.mult),
                                     reads=[cst, dtbb], writes=[rhsUb])
                                rf = rhsU[:].rearrange("p a b -> p (a b)")
                                pseg = psum()
                                P.op("pe", lambda e: e.matmul(pseg.ap, lhsT=Lgt_t[:], rhs=rf, start=True, stop=True), reads=[rhsUb, cst], writes=[pseg])
                                pacs = psum()
                                P.op("pe", lambda e: e.matmul(pacs.ap, lhsT=onesf_t[:], rhs=rf, start=True, stop=True), reads=[rhsUb, cst], writes=[pacs])
                                psm = psum()
                                P.op("pe", lambda e: e.matmul(psm.ap[:, 0:4], lhsT=Lgt_t[:], rhs=dAc, start=True, stop=True), reads=[dtbb, cst], writes=[psm], inc=False)
                                P.op("pe", lambda e: e.matmul(psm.ap[:, 4:8], lhsT=onesf_t[:], rhs=dAc, start=True, stop=True), reads=[dtbb, cst], writes=[psm])
                                P.op("act", lambda e: e.activation(out=Eseg[:], in_=pseg.ap.rearrange("p (a b) -> p a b", b=128), func=AF.Exp), reads=[pseg], writes=[Esegb])
                                P.op("act", lambda e: e.activation(out=Ebc[:], in_=pacs.ap.rearrange("p (a b) -> p a b", b=128), func=AF.Exp), reads=[pacs], writes=[Ebcb])
                                P.op("act", lambda e: e.activation(out=smx[:], in_=psm.ap[:, 0:8], func=AF.Exp), reads=[psm], writes=[smxb])
                                pcb = psum()
                                P.op("pe", lambda e: e.matmul(pcb.ap[:, 0:128], lhsT=BT[:, csl], rhs=CT[:, csl], start=True, stop=True), reads=[BTb, CTb], writes=[pcb])
                                P.op("dve", lambda e: e.tensor_tensor(out=CBm[:], in0=pcb.ap[:, 0:128], in1=causT_t[:], op=ALU.mult), reads=[pcb, cst], writes=[CBmb])
                                P.op("dve", lambda e: e.tensor_tensor(out=MT[:], in0=Eseg[:], in1=CBm[:].unsqueeze(1).to_broadcast([128, 4, 128]), op=ALU.mult),
                                     reads=[Esegb, CBmb], writes=[MTb])
                                P.op("dve", lambda e: e.tensor_tensor(out=CsT[:], in0=Ebc[:], in1=CT[:, csl].unsqueeze(1).to_broadcast([128, 4, 128]), op=ALU.mult),
                                     reads=[Ebcb, CTb], writes=[CsTb])
                                pxt = psum()
                                pxtb = bf(pxt)
                                for i in range(2):
                                    P.op("pe", lambda e, i=i: e.transpose(pxtb[:, i * 128:(i + 1) * 128], xsT[:, i, csl], identb_t[:]), reads=[xsb, cst], writes=[pxt], inc=False)
                                P.op("pe", lambda e: e.transpose(pxtb[:, 256:384], BT[:, csl], identb_t[:]), reads=[BTb, cst], writes=[pxt])
                                P.op("dve", lambda e: e.tensor_tensor(out=xdt[:], in0=pxtb[:, 0:256].rearrange("p (a b) -> p a b", b=64),
                                                                      in1=dtb[:, c, 4 * g:4 * g + 4].unsqueeze(2).to_broadcast([128, 4, 64]), op=ALU.mult),
                                     reads=[pxt, dtbb], writes=[xdtb])
                                P.op("dve", lambda e: e.tensor_tensor(out=xdtd[:], in0=xdt[:], in1=smx[:, 0:4].unsqueeze(2).to_broadcast([128, 4, 64]), op=ALU.mult),
                                     reads=[xdtb, smxb], writes=[xdtdb])
                                P.op("dve", lambda e: e.tensor_copy(out=Btok[:], in_=pxtb[:, 256:384]), reads=[pxt], writes=[Btokb])
                                py = psum()
                                for rr in range(4):
                                    i, part = rr // 2, rr % 2
                                    P.op("pe", lambda e, rr=rr, i=i, part=part: e.matmul(
                                        py.ap[part * 64:(part + 1) * 64, i * 128:(i + 1) * 128], lhsT=xdt[:, rr, :], rhs=MT[:, rr, :], start=True, stop=(c == 0)),
                                        reads=[xdtb, MTb], writes=[py], inc=(c == 0))
                                    if c > 0:
                                        P.op("pe", lambda e, rr=rr, i=i, part=part: e.matmul(
                                            py.ap[part * 64:(part + 1) * 64, i * 128:(i + 1) * 128], lhsT=prevb16[:, rr, :], rhs=CsT[:, rr, :], start=False, stop=True),
                                            reads=[prevbb, CsTb], writes=[py])
                                for i in range(2):
                                    P.op("dve", lambda e, i=i: e.scalar_tensor_tensor(out=ytmp[:, i, :], in0=xsT[:, i, csl], scalar=dsk_t[:, l, 2 * g + i:2 * g + i + 1],
                                                                                      in1=py.ap[:, i * 128:(i + 1) * 128], op0=ALU.mult, op1=ALU.add),
                                         reads=[xsb, py, cst], writes=[ytmpb])
                                P.op("dve", lambda e: e.tensor_tensor(out=yz[:, :, csl], in0=ytmp[:], in1=zs[:, :, csl], op=ALU.mult),
                                     reads=[ytmpb, zsb], writes=[yzb[0][c // 4], yzb[1][c // 4]])
                                pstt = psum()
                                P.op("pe", lambda e: e.matmul(pstt.ap[:, 0:256], lhsT=Btok[:], rhs=xdtd[:].rearrange("p a b -> p (a b)"), start=True, stop=True),
                                     reads=[Btokb, xdtdb], writes=[pstt])
                                P.op("dve", lambda e: e.tensor_tensor(out=prev[:], in0=prev[:], in1=smx[:, 4:8].unsqueeze(2).to_broadcast([128, 4, 64]), op=ALU.mult),
                                     reads=[prevb, smxb], writes=[prevb])
                                P.op("dve", lambda e: e.tensor_tensor(out=prev[:], in0=prev[:], in1=pstt.ap[:, 0:256].rearrange("p (a b) -> p a b", b=64), op=ALU.add),
                                     reads=[prevb, pstt], writes=[prevb])
                                P.op("act", lambda e: e.activation(out=prevb16n[:], in_=prev[:], func=AF.Copy), reads=[prevb], writes=[prevbbn])
                            r2, wo = load_wout(l, 512 + g * 256, 2)
                            for t in range(4):
                                tsl = slice(t * 512, (t + 1) * 512)
                                vs = [V(yz[:, i, tsl], yzb[i][t]) for i in range(2)]
                                rms_run(st, vs, vs, lambda i: ssmg_t[:, l, 2 * g + i:2 * g + i + 1], None)
                                add_wout(l, s, r2, wo, 2, lambda jj: yz[:, jj, tsl], [yzb[0][t], yzb[1][t]], t * 512, 512)

        xin_sem = [P.dma_sem(f"xin{i}") for i in range(2)]
        out_sem = [P.dma_sem(f"xout{i}") for i in range(2)]
        for s in range(NSQ):
            with ExitStack() as ph:
                st_t = [sb(f"xin{i}", [128, D], F32, ph) for i in range(2)]
                st_b = [P.buf(f"xin{i}") for i in range(2)]
                for tt in range(NT):
                    i = tt % 2
                    P.dma("sp", st_t[i][:], x_d[s, tt * 128:(tt + 1) * 128, :], xin_sem[i], writes=[st_b[i]])
                    for half in range(2):
                        pm = psum()
                        for cc in range(4):
                            c = half * 4 + cc
                            P.op("pe", lambda e, c=c, cc=cc, i=i: e.transpose(pm.ap[:, cc * 128:(cc + 1) * 128], st_t[i][:, c * 128:(c + 1) * 128], ident_t[:]),
                                 reads=[st_b[i], cst], writes=[pm], inc=(cc == 3))
                        bl = [xb[half * 4 + cc][tt // 4] for cc in range(4)]
                        if half == 0:
                            P.op("act", lambda e, half=half, tt=tt: e.activation(
                                out=xT_t[:, half * 4:(half + 1) * 4, tt * 128:(tt + 1) * 128],
                                in_=pm.ap.rearrange("p (c t) -> p c t", t=128), func=AF.Copy), reads=[pm], writes=bl)
                        else:
                            P.op("dve", lambda e, half=half, tt=tt: e.tensor_copy(
                                out=xT_t[:, half * 4:(half + 1) * 4, tt * 128:(tt + 1) * 128],
                                in_=pm.ap.rearrange("p (c t) -> p c t", t=128)), reads=[pm], writes=bl)
            for l in LAYERS:
                if cfg["ffn1"]:
                    ffn(l, s, 0)
                if MIX:
                    norm_mod(l, s, 1)
                    if "hg" in MIX:
                        for slot in range(2):
                            mixer_hg(l, s, slot)
                    if "dsa" in MIX:
                        mixer_dsa(l, s)
                    if "ssd" in MIX:
                        mixer_ssd(l, s)
                if cfg["ffn2"]:
                    ffn(l, s, 1)
            with ExitStack() as ph:
                st = rms_state(ph, "fin", 8, onesd_t)
                y_t = [sb(f"yfin{i}", [128, 8, 512], F32, ph) for i in range(2)]
                y_b = [P.buf(f"yfin{i}") for i in range(2)]
                o_t = [sb(f"otok{i}", [128, D], F32, ph) for i in range(2)]
                o_b = [P.buf(f"otok{i}") for i in range(2)]
                oq = 0
                for t in range(4):
                    yi = t % 2
                    rms_run(st, [xv(c, t) for c in range(8)], [V(y_t[yi][:, c, :], y_b[yi]) for c in range(8)],
                            lambda c: fng_t[:, c:c + 1], None)
                    for sub in range(4):
                        tt = t * 4 + sub
                        oi = oq % 2
                        oq += 1
                        for half in range(2):
                            pm = psum()
                            for cc in range(4):
                                c = half * 4 + cc
                                P.op("pe", lambda e, c=c, cc=cc, yi=yi, sub=sub: e.transpose(
                                    pm.ap[:, cc * 128:(cc + 1) * 128], y_t[yi][:, c, sub * 128:(sub + 1) * 128], ident_t[:]),
                                    reads=[y_b[yi], cst], writes=[pm], inc=(cc == 3))
                            if half == 0:
                                P.op("act", lambda e, oi=oi: e.activation(out=o_t[oi][:, 0:512], in_=pm.ap, func=AF.Copy),
                                     reads=[pm], writes=[o_b[oi]])
                            else:
                                P.op("dve", lambda e, oi=oi: e.tensor_copy(out=o_t[oi][:, 512:1024], in_=pm.ap),
                                     reads=[pm], writes=[o_b[oi]])
                        P.dma("sp", out_d[s, tt * 128:(tt + 1) * 128, :], o_t[oi][:], out_sem[oi], reads=[o_b[oi]])
        P.wait_all("sp")
        print("instructions emitted:", P.ninstr)
    return nc


def kernel(**inputs):
    inputs = {k: np.asarray(v, dtype=np.float32) for k, v in inputs.items()}
    nc = build_program(FULL_CFG)
    shared = _shared_inputs(inputs, FULL_CFG)
    in_maps = []
    for core in range(8):
        m = dict(shared)
        m.update(_host_inputs(inputs, core, FULL_CFG))
        in_maps.append(m)
    res = run_bass_kernel_spmd(nc, in_maps, core_ids=list(range(8)))
    out = np.concatenate([np.asarray(r["out"]).reshape(NSEQ, S, D) for r in res.results], axis=0)
    return out.astype(np.float32)
```
